# Optimizing a Trainium2 kernel written in Bass

```python
import jax, jax.numpy as jnp
from jax import lax
import numpy as np

D_MODEL = 1024
BATCH = 8
SEQ = 4096
DEPTH = 2
DEC_BATCH = 128
DEC_SEQ = 1
PAST_LEN = 16384
PAGE_SIZE = 128

N_EVEN_LAYERS = (DEPTH + 1) // 2
N_ODD_LAYERS = DEPTH // 2
POOL_WIDTH = D_MODEL // 2
POOL_WINDOWS = (2, 4, 8, 16)
N_POOL_GROUPS = len(POOL_WINDOWS)
POOL_GROUP = POOL_WIDTH // N_POOL_GROUPS
POOL_BUF = max(POOL_WINDOWS) - 1
MLA_HEADS = 8
QK_NOPE = 64
QK_ROPE = 32
V_HEAD = 64
Q_LORA = D_MODEL // 4
KV_LORA = D_MODEL // 8
MLA_ROW = KV_LORA + QK_ROPE
MLA_SCALE = (QK_NOPE + QK_ROPE) ** -0.5
ROPE_THETA = 10000.0
Q_BLOCK = 128
D_IN0 = POOL_WIDTH + Q_LORA + KV_LORA + QK_ROPE
D_OUT0 = POOL_WIDTH + MLA_HEADS * V_HEAD
D_RNN = D_MODEL
LRU_HEADS = 8
LRU_BLOCK = D_RNN // LRU_HEADS
CONV_WIDTH = 4
LRU_C = 8.0
D_FF = 11 * D_MODEL // 4
N_EXPERTS = 8
TOP_K = 2
D_EXPERT = D_FF // 2
PLE_DIM = 256
NORM_EPS = 1e-6

kernel_name = "hybrid_pool_mla_rglru_moe_step"


def _rmsnorm(x, g):
    xf = x.astype(jnp.float32)
    y = xf * lax.rsqrt(jnp.mean(xf * xf, axis=-1, keepdims=True) + NORM_EPS)
    return y.astype(x.dtype) * g


def _rope_tables(pos):
    inv = ROPE_THETA ** (-jnp.arange(0, QK_ROPE, 2, dtype=jnp.float32) / QK_ROPE)
    ang = pos.astype(jnp.float32)[:, None] * inv[None, :]
    return jnp.cos(ang), jnp.sin(ang)


def _apply_rope(x, cos, sin):
    half = QK_ROPE // 2
    xf = x.astype(jnp.float32)
    x1, x2 = xf[..., :half], xf[..., half:]
    out = jnp.concatenate([x1 * cos - x2 * sin, x2 * cos + x1 * sin], axis=-1)
    return out.astype(x.dtype)


def _pool_mixer(u, buf, pos, w_pool, scale):
    bsz, t, _ = u.shape
    ext = jnp.concatenate([buf, u], axis=1)
    cs = lax.cumsum(ext.astype(jnp.float32), axis=1)
    cs = jnp.concatenate([jnp.zeros((bsz, 1, POOL_WIDTH), jnp.float32), cs], axis=1)
    end = cs[:, POOL_BUF + 1:POOL_BUF + 1 + t]
    means = []
    for g, w in enumerate(POOL_WINDOWS):
        sl = slice(g * POOL_GROUP, (g + 1) * POOL_GROUP)
        start = cs[:, POOL_BUF + 1 - w:POOL_BUF + 1 - w + t, sl]
        cnt = jnp.minimum(pos + 1, w).astype(jnp.float32)[None, :, None]
        means.append((end[..., sl] - start) / cnt)
    pooled = jnp.concatenate(means, axis=-1).astype(u.dtype) - u
    mixed = jnp.einsum("btgc,gcd->btgd", pooled.reshape(bsz, t, N_POOL_GROUPS, POOL_GROUP), w_pool)
    y = mixed.reshape(bsz, t, POOL_WIDTH) * scale
    return y, ext[:, -POOL_BUF:]


def _causal_block_attention(q, kv):
    bsz, t, h, c = q.shape
    nb = t // Q_BLOCK
    qb = q.reshape(bsz, nb, Q_BLOCK, h, c).transpose(1, 0, 2, 3, 4)
    k_pos = jnp.arange(t)
    lat = kv[..., :KV_LORA]

    def one_block(args):
        q_blk, blk = args
        s = jnp.einsum("bqhc,bsc->bhqs", q_blk, kv).astype(jnp.float32)
        q_pos = blk * Q_BLOCK + jnp.arange(Q_BLOCK)
        s = jnp.where(k_pos[None, :] <= q_pos[:, None], s, -jnp.inf)
        p = jax.nn.softmax(s, axis=-1).astype(kv.dtype)
        return jnp.einsum("bhqs,bsc->bqhc", p, lat)

    o = lax.map(one_block, (qb, jnp.arange(nb)))
    return o.transpose(1, 0, 2, 3, 4).reshape(bsz, t, h, KV_LORA)


def _decode_attention(q, past, rows):
    t = q.shape[1]
    n_past = past.shape[1]
    s_past = jnp.einsum("bthc,bsc->bhts", q, past).astype(jnp.float32)
    s_new = jnp.einsum("bthc,bsc->bhts", q, rows).astype(jnp.float32)
    causal = jnp.tril(jnp.ones((t, t), dtype=bool))
    s_new = jnp.where(causal, s_new, -jnp.inf)
    p = jax.nn.softmax(jnp.concatenate([s_past, s_new], axis=-1), axis=-1).astype(q.dtype)
    return (jnp.einsum("bhts,bsc->bthc", p[..., :n_past], past[..., :KV_LORA])
            + jnp.einsum("bhts,bsc->bthc", p[..., n_past:], rows[..., :KV_LORA]))


def _pool_mla_mixer(xn, pos, pool_buf, past, prm, j):
    bsz, t, _ = xn.shape
    proj = xn @ prm["w_in0"][j]
    o1 = POOL_WIDTH
    o2 = o1 + Q_LORA
    o3 = o2 + KV_LORA
    u, c_q, c_kv, k_r = proj[..., :o1], proj[..., o1:o2], proj[..., o2:o3], proj[..., o3:]
    pool_y, new_pool = _pool_mixer(u, pool_buf, pos, prm["pool_w"][j], prm["pool_scale"][j])
    q = (_rmsnorm(c_q, prm["q_norm"][j]) @ prm["w_uq"][j]).reshape(bsz, t, MLA_HEADS, QK_NOPE + QK_ROPE)
    q_nope, q_rope = q[..., :QK_NOPE], q[..., QK_NOPE:]
    cos, sin = _rope_tables(pos)
    q_rope = _apply_rope(q_rope, cos[None, :, None, :], sin[None, :, None, :])
    k_rope = _apply_rope(k_r, cos[None], sin[None])
    rows = jnp.concatenate([_rmsnorm(c_kv, prm["kv_norm"][j]), k_rope], axis=-1)
    q_lat = jnp.einsum("bthn,chn->bthc", q_nope, prm["w_uk"][j])
    q_cat = jnp.concatenate([q_lat, q_rope], axis=-1) * MLA_SCALE
    if past is None:
        o_lat = _causal_block_attention(q_cat, rows)
    else:
        o_lat = _decode_attention(q_cat, past, rows)
    o = jnp.einsum("bthc,chv->bthv", o_lat, prm["w_uv"][j]).reshape(bsz, t, MLA_HEADS * V_HEAD)
    y = jnp.concatenate([pool_y, o], axis=-1) @ prm["w_out0"][j]
    return y, new_pool, rows


def _linear_scan(a, b, h0):
    b = b.at[:, 0].add(a[:, 0] * h0)

    def combine(lhs, rhs):
        a_l, b_l = lhs
        a_r, b_r = rhs
        return a_l * a_r, a_r * b_l + b_r

    _, h = lax.associative_scan(combine, (a, b), axis=1)
    return h


def _rglru_mixer(xn, conv_buf, h0, prm, j):
    bsz, t, _ = xn.shape
    proj = xn @ prm["w_in1"][j]
    xb, gb = proj[..., :D_RNN], proj[..., D_RNN:]
    ext = jnp.concatenate([conv_buf, xb], axis=1)
    w = prm["conv_w"][j]
    xc = prm["conv_b"][j] + ext[:, 0:t] * w[0]
    for k in range(1, CONV_WIDTH):
        xc = xc + ext[:, k:k + t] * w[k]
    xh = xc.reshape(bsz, t, LRU_HEADS, LRU_BLOCK)
    r = jax.nn.sigmoid(jnp.einsum("bthi,hij->bthj", xh, prm["w_rg"][j]).reshape(bsz, t, D_RNN) + prm["b_rg"][j])
    ig = jax.nn.sigmoid(jnp.einsum("bthi,hij->bthj", xh, prm["w_ig"][j]).reshape(bsz, t, D_RNN) + prm["b_ig"][j])
    log_a = (-LRU_C * r.astype(jnp.float32)) * jax.nn.softplus(-prm["lru_lambda"][j].astype(jnp.float32))
    a = jnp.exp(log_a)
    mult = jnp.sqrt(-jnp.expm1(2.0 * log_a))
    bt = mult * (ig * xc).astype(jnp.float32)
    h = _linear_scan(a, bt, h0.astype(jnp.float32))
    y = h.astype(xn.dtype) * jax.nn.gelu(gb)
    return y @ prm["w_out1"][j], ext[:, -(CONV_WIDTH - 1):], h[:, -1].astype(xn.dtype)


def _swiglu(h, w_g, w_u, w_d):
    return (jax.nn.silu(h @ w_g) * (h @ w_u)) @ w_d


def _moe_swiglu(h, w_router, w_g, w_u, w_d):
    logits = (h @ w_router).astype(jnp.float32)
    top_v, top_i = lax.top_k(logits, TOP_K)
    top_w = jax.nn.softmax(top_v, axis=-1)
    combine = jnp.einsum("btk,btke->bte", top_w, jax.nn.one_hot(top_i, N_EXPERTS, dtype=jnp.float32)).astype(h.dtype)
    out = jnp.zeros_like(h)
    for e in range(N_EXPERTS):
        out = out + combine[..., e:e + 1] * _swiglu(h, w_g[e], w_u[e], w_d[e])
    return out


def _per_layer_embedding(x, p_l, g_norm, w_proj, w_gate):
    gate = jax.nn.sigmoid(_rmsnorm(x, g_norm) @ w_gate)
    return gate * (p_l @ w_proj)


def _run_group(x, p, pos, pool_buf, conv_buf, lru_h, cache_mla, page_table, prm):
    rows_out, pool_out, conv_out, h_out = [], [], [], []
    for layer in range(DEPTH):
        j = layer // 2
        xn = _rmsnorm(x, prm["norm_mix"][layer])
        if layer % 2 == 0:
            past = None
            if cache_mla is not None:
                past = cache_mla[j, page_table].reshape(x.shape[0], -1, MLA_ROW)
            mix, new_pool, rows = _pool_mla_mixer(xn, pos, pool_buf[j], past, prm, j)
            rows_out.append(rows)
            pool_out.append(new_pool)
            x = x + mix
            x = x + _swiglu(_rmsnorm(x, prm["norm_ffn"][layer]), prm["w_ffn_gate"][j],
                            prm["w_ffn_up"][j], prm["w_ffn_down"][j])
        else:
            mix, new_conv, new_h = _rglru_mixer(xn, conv_buf[j], lru_h[j], prm, j)
            conv_out.append(new_conv)
            h_out.append(new_h)
            x = x + mix
            x = x + _moe_swiglu(_rmsnorm(x, prm["norm_ffn"][layer]), prm["w_router"][j],
                                prm["w_exp_gate"][j], prm["w_exp_up"][j], prm["w_exp_down"][j])
        x = x + _per_layer_embedding(x, p[layer], prm["norm_ple"][layer],
                                     prm["w_ple_proj"][layer], prm["w_ple_gate"][layer])
    y = _rmsnorm(x, prm["norm_final"])
    return y, jnp.stack(rows_out), jnp.stack(pool_out), jnp.stack(conv_out), jnp.stack(h_out)


def setup_inputs(seed: int = 0) -> dict:
    key = jax.random.key(seed)
    ks = iter(jax.random.split(key, 64))
    f32 = jnp.float32
    n_pages = PAST_LEN // PAGE_SIZE
    n_pool = (DEC_BATCH * n_pages * 5) // 4
    ne, no = N_EVEN_LAYERS, N_ODD_LAYERS

    def nrm(shape, scale):
        return jax.random.normal(next(ks), shape, f32) * scale

    def gain(shape):
        return 1.0 + nrm(shape, 0.05)

    inp = {}
    inp["x_prompt"] = nrm((BATCH, SEQ, D_MODEL), 1.0)
    inp["x_sample"] = nrm((DEC_BATCH, DEC_SEQ, D_MODEL), 1.0)
    inp["p_prompt"] = nrm((DEPTH, BATCH, SEQ, PLE_DIM), 1.0)
    inp["p_sample"] = nrm((DEPTH, DEC_BATCH, DEC_SEQ, PLE_DIM), 1.0)
    inp["cache_mla"] = nrm((ne, n_pool, PAGE_SIZE, MLA_ROW), 1.0)
    inp["state_pool"] = nrm((ne, DEC_BATCH, POOL_BUF, POOL_WIDTH), 1.0)
    inp["state_conv"] = nrm((no, DEC_BATCH, CONV_WIDTH - 1, D_RNN), 1.0)
    inp["state_lru"] = nrm((no, DEC_BATCH, D_RNN), 0.5)
    perm = jax.random.permutation(next(ks), n_pool)[:DEC_BATCH * n_pages]
    inp["page_table"] = perm.astype(jnp.int32).reshape(DEC_BATCH, n_pages)
    inp["norm_mix"] = gain((DEPTH, D_MODEL))
    inp["norm_ffn"] = gain((DEPTH, D_MODEL))
    inp["norm_ple"] = gain((DEPTH, D_MODEL))
    inp["norm_final"] = gain((D_MODEL,))
    inp["w_in0"] = nrm((ne, D_MODEL, D_IN0), D_MODEL ** -0.5)
    inp["pool_w"] = nrm((ne, N_POOL_GROUPS, POOL_GROUP, POOL_GROUP), POOL_GROUP ** -0.5)
    inp["pool_scale"] = 1.0 + nrm((ne, POOL_WIDTH), 0.1)
    inp["q_norm"] = gain((ne, Q_LORA))
    inp["kv_norm"] = gain((ne, KV_LORA))
    inp["w_uq"] = nrm((ne, Q_LORA, MLA_HEADS * (QK_NOPE + QK_ROPE)), Q_LORA ** -0.5)
    inp["w_uk"] = nrm((ne, KV_LORA, MLA_HEADS, QK_NOPE), KV_LORA ** -0.5)
    inp["w_uv"] = nrm((ne, KV_LORA, MLA_HEADS, V_HEAD), KV_LORA ** -0.5)
    inp["w_out0"] = nrm((ne, D_OUT0, D_MODEL), D_OUT0 ** -0.5)
    inp["w_in1"] = nrm((no, D_MODEL, 2 * D_RNN), D_MODEL ** -0.5)
    inp["conv_w"] = nrm((no, CONV_WIDTH, D_RNN), CONV_WIDTH ** -0.5)
    inp["conv_b"] = nrm((no, D_RNN), 0.01)
    inp["w_rg"] = nrm((no, LRU_HEADS, LRU_BLOCK, LRU_BLOCK), LRU_BLOCK ** -0.5)
    inp["b_rg"] = nrm((no, D_RNN), 0.01)
    inp["w_ig"] = nrm((no, LRU_HEADS, LRU_BLOCK, LRU_BLOCK), LRU_BLOCK ** -0.5)
    inp["b_ig"] = nrm((no, D_RNN), 0.01)
    a0 = jax.random.uniform(next(ks), (no, D_RNN), f32, 0.9, 0.999)
    inp["lru_lambda"] = jnp.log(a0) - jnp.log1p(-a0)
    inp["w_out1"] = nrm((no, D_RNN, D_MODEL), D_RNN ** -0.5)
    inp["w_ffn_gate"] = nrm((ne, D_MODEL, D_FF), D_MODEL ** -0.5)
    inp["w_ffn_up"] = nrm((ne, D_MODEL, D_FF), D_MODEL ** -0.5)
    inp["w_ffn_down"] = nrm((ne, D_FF, D_MODEL), D_FF ** -0.5)
    inp["w_router"] = nrm((no, D_MODEL, N_EXPERTS), D_MODEL ** -0.5)
    inp["w_exp_gate"] = nrm((no, N_EXPERTS, D_MODEL, D_EXPERT), D_MODEL ** -0.5)
    inp["w_exp_up"] = nrm((no, N_EXPERTS, D_MODEL, D_EXPERT), D_MODEL ** -0.5)
    inp["w_exp_down"] = nrm((no, N_EXPERTS, D_EXPERT, D_MODEL), D_EXPERT ** -0.5)
    inp["w_ple_proj"] = nrm((DEPTH, PLE_DIM, D_MODEL), PLE_DIM ** -0.5)
    inp["w_ple_gate"] = nrm((DEPTH, D_MODEL, D_MODEL), D_MODEL ** -0.5)
    return inp


def reference(x_prompt, x_sample, p_prompt, p_sample, cache_mla, state_pool, state_conv, state_lru,
              page_table, norm_mix, norm_ffn, norm_ple, norm_final, w_in0, pool_w, pool_scale,
              q_norm, kv_norm, w_uq, w_uk, w_uv, w_out0, w_in1, conv_w, conv_b, w_rg, b_rg,
              w_ig, b_ig, lru_lambda, w_out1, w_ffn_gate, w_ffn_up, w_ffn_down, w_router,
              w_exp_gate, w_exp_up, w_exp_down, w_ple_proj, w_ple_gate):
    prm = dict(norm_mix=norm_mix, norm_ffn=norm_ffn, norm_ple=norm_ple, norm_final=norm_final,
               w_in0=w_in0, pool_w=pool_w, pool_scale=pool_scale, q_norm=q_norm, kv_norm=kv_norm,
               w_uq=w_uq, w_uk=w_uk, w_uv=w_uv, w_out0=w_out0, w_in1=w_in1, conv_w=conv_w,
               conv_b=conv_b, w_rg=w_rg, b_rg=b_rg, w_ig=w_ig, b_ig=b_ig, lru_lambda=lru_lambda,
               w_out1=w_out1, w_ffn_gate=w_ffn_gate, w_ffn_up=w_ffn_up, w_ffn_down=w_ffn_down,
               w_router=w_router, w_exp_gate=w_exp_gate, w_exp_up=w_exp_up,
               w_exp_down=w_exp_down, w_ple_proj=w_ple_proj, w_ple_gate=w_ple_gate)
    bsz, seq = x_prompt.shape[0], x_prompt.shape[1]
    dt = x_prompt.dtype
    past_len = page_table.shape[1] * PAGE_SIZE
    pos_prompt = jnp.arange(seq, dtype=jnp.int32)
    pos_sample = past_len + jnp.arange(x_sample.shape[1], dtype=jnp.int32)
    zero_pool = jnp.zeros((N_EVEN_LAYERS, bsz, POOL_BUF, POOL_WIDTH), dt)
    zero_conv = jnp.zeros((N_ODD_LAYERS, bsz, CONV_WIDTH - 1, D_RNN), dt)
    zero_h = jnp.zeros((N_ODD_LAYERS, bsz, D_RNN), dt)
    y_prompt, rows_p, pool_p, conv_p, h_p = _run_group(
        x_prompt, p_prompt, pos_prompt, zero_pool, zero_conv, zero_h, None, None, prm)
    y_sample, rows_s, pool_s, conv_s, h_s = _run_group(
        x_sample, p_sample, pos_sample, state_pool, state_conv, state_lru, cache_mla, page_table, prm)
    return (y_prompt, y_sample, rows_p, rows_s, pool_p, pool_s, conv_p, conv_s, h_p, h_s)
```

```python
import contextlib
import math
import numpy as np
import concourse.bass as bass
import concourse.mybir as mybir
from concourse.bass_utils import run_bass_kernel_spmd

F32 = mybir.dt.float32
BF16 = mybir.dt.bfloat16
I32 = mybir.dt.int32
AF = mybir.ActivationFunctionType
ALU = mybir.AluOpType
AX = mybir.AxisListType

D = 1024
SEQ = 4096
NCORE = 8
NSAMP = 16
TW = 512
W = TW + NSAMP
NT = SEQ // TW
PAST_PAGES = 128
PAGE = 128
ROWW = 160
NPOOLPG = 20480
SCALE = float((64 + 32) ** -0.5)
EPS = 1e-6
DEXP = 1408
NEXP = 8
GCH = 16
NGCH = PAGE // GCH
SLOT = 4096
NSLOT = 4
ENGS = ("pe", "act", "dve", "pool", "sp")


class Op:
    __slots__ = ("idx", "eng", "fn", "dma", "key", "deps", "inc", "cnt")

    def __init__(self, idx, eng, fn, dma, key):
        self.idx = idx
        self.eng = eng
        self.fn = fn
        self.dma = dma
        self.key = key
        self.deps = {}
        self.inc = False
        self.cnt = 0


class Prog:
    def __init__(self, nc):
        self.nc = nc
        self.ops = []
        self.lastw = {}
        self.readers = {}
        self.stack = contextlib.ExitStack()
        self.nbank = 0
        self.live = []
        self.scr = None

    def sb(self, name, shape, dtype):
        return self.stack.enter_context(self.nc.sbuf_tensor(name, list(shape), dtype))

    def ps(self, name, shape, dtype):
        return self.stack.enter_context(self.nc.psum_tensor(name, list(shape), dtype))

    def alias(self, new, olds):
        lst = self.readers.setdefault(new, [])
        for o in olds:
            lst.extend(self.readers.get(o, []))
            if o in self.lastw:
                lst.append(self.lastw[o])

    def carve(self, tok, off, shape, dtype, p0=0, p1=128):
        esz = 4 if dtype in (F32, I32) else 2
        nel = 1
        for s in shape:
            nel *= s
        nbytes = nel * esz
        assert off % 4 == 0 and nbytes % 4 == 0, (tok, off, nbytes)
        assert off + nbytes <= self.scr_bytes, (tok, off, nbytes, self.scr_bytes)
        ap = self.scr[p0:p1, off // 4:(off + nbytes) // 4]
        if dtype != F32:
            ap = ap.bitcast(dtype)
        if len(shape) == 2:
            ap = ap.rearrange("p (a b) -> p a b", a=shape[0])
        elif len(shape) == 3:
            ap = ap.rearrange("p (a b c) -> p a b c", a=shape[0], b=shape[1])
        lo, hi = off, off + nbytes
        same = [e for e in self.live if e[2] == tok]
        if same and same[0][0] == lo and same[0][1] == hi:
            return ap
        olds = [e for e in self.live if e[0] < hi and lo < e[1]]
        if olds:
            self.alias(tok, [e[2] for e in olds if e[2] != tok])
            keep = [e for e in self.live if not (e[0] < hi and lo < e[1])]
            for (lo_o, hi_o, tok_o) in olds:
                if tok_o == tok:
                    continue
                if lo_o < lo:
                    keep.append((lo_o, lo, tok_o))
                if hi_o > hi:
                    keep.append((hi, hi_o, tok_o))
            self.live = keep
        self.live.append((lo, hi, tok))
        return ap

    def add(self, eng, meth, *args, reads=(), writes=(), dma=False, key=None, **kw):
        op = Op(len(self.ops), eng, (meth, args, kw), dma, key)
        for t in reads:
            w = self.lastw.get(t)
            if w is not None:
                op.deps[w] = True
            if t.startswith("bank"):
                for r in self.readers.get(t, ()):
                    if self.ops[r].eng != eng:
                        op.deps[r] = True
                        self.rar = getattr(self, "rar", 0) + 1
        for t in writes:
            w = self.lastw.get(t)
            if w is not None and w not in op.deps:
                op.deps[w] = False
            for r in self.readers.get(t, ()):
                if r not in op.deps:
                    op.deps[r] = False
        for t in reads:
            lst = self.readers.setdefault(t, [])
            if not dma:
                lst[:] = [r for r in lst if self.ops[r].dma or self.ops[r].eng != eng]
            lst.append(op.idx)
        for t in writes:
            self.lastw[t] = op.idx
            self.readers[t] = []
        self.ops.append(op)
        return op

    def emit(self):
        nc = self.nc
        ops = self.ops
        for op in ops:
            for d, raw in op.deps.items():
                dop = ops[d]
                if dop.dma:
                    continue
                if dop.eng == op.eng and not op.dma:
                    if dop.eng == "pe":
                        continue
                dop.inc = True
        esem = {e: self.stack.enter_context(nc.semaphore("sem_" + e)) for e in ENGS}
        ecnt = {e: 0 for e in ENGS}
        ksem = {}
        kcnt = {}
        for op in ops:
            if op.dma:
                k = op.key
                if k not in ksem:
                    ksem[k] = self.stack.enter_context(nc.semaphore("dk%d" % len(ksem)))
                    kcnt[k] = 0
                kcnt[k] += 16
                op.cnt = kcnt[k]
            elif op.inc:
                ecnt[op.eng] += 1
                op.cnt = ecnt[op.eng]
        self.stats = dict(nops=len(ops), nsem=len(ksem) + len(ENGS), ecnt=dict(ecnt))
        per_eng = {e: [op for op in ops if op.eng == e] for e in ENGS}

        def run(eng_name, eng):
            waited = {}
            for op in per_eng[eng_name]:
                for d, raw in op.deps.items():
                    dop = ops[d]
                    if dop.dma:
                        s = ksem[dop.key]
                    else:
                        if dop.eng == op.eng and not op.dma:
                            if dop.eng == "pe":
                                continue
                        s = esem[dop.eng]
                    v = dop.cnt
                    if waited.get(s.name, 0) >= v:
                        continue
                    eng.wait_ge(s, v)
                    waited[s.name] = v
                meth, args, kw = op.fn
                ins = getattr(eng, meth)(*args, **kw)
                if op.dma:
                    ins.then_inc(ksem[op.key], 16)
                elif op.inc:
                    ins.then_inc(esem[op.eng], 1)
            if eng_name == "sp":
                for k, v in kcnt.items():
                    if waited.get(ksem[k].name, 0) >= v:
                        continue
                    eng.wait_ge(ksem[k], v)

        with nc.Block() as block:
            @block.tensor
            def _(e):
                run("pe", e)

            @block.scalar
            def _(e):
                run("act", e)

            @block.vector
            def _(e):
                run("dve", e)

            @block.gpsimd
            def _(e):
                run("pool", e)

            @block.sync
            def _(e):
                run("sp", e)


def _blk(Wm, k0, nk, m0, mw):
    a = Wm[k0 * 128:(k0 + nk) * 128, m0:m0 + mw]
    return np.ascontiguousarray(a.reshape(nk, 128, mw).transpose(1, 0, 2)).reshape(128, nk * mw)


def _split3(i):
    return i * 512, min(512, DEXP - i * 512)


def weight_units(inp=None):
    u = []

    def add(name, nel, fn):
        u.append((name, nel, fn))

    add("in0a", 8 * 512, lambda i: _blk(i["w_in0"][0], 0, 8, 0, 512))
    add("in0b", 8 * 416, lambda i: _blk(i["w_in0"][0], 0, 8, 512, 416))
    add("pool", 512, lambda i: np.ascontiguousarray(i["pool_w"][0].transpose(1, 0, 2)).reshape(128, 512))

    def uq(i):
        w = i["w_uq"][0].reshape(256, 8, 96)
        w2 = np.concatenate([w[:, :, :64].reshape(256, 512), w[:, :, 64:].reshape(256, 256)], axis=1)
        return _blk(w2, 0, 2, 0, 768)
    add("uq", 2 * 768, uq)

    def uk(i):
        w = np.ascontiguousarray(i["w_uk"][0].transpose(2, 1, 0)).reshape(64, 1024)
        return np.concatenate([w, np.zeros((64, 1024), np.float32)], axis=0)
    add("uk", 1024, uk)
    add("uv", 512, lambda i: np.ascontiguousarray(i["w_uv"][0].reshape(128, 512)))
    for j in range(2):
        add("out0%d" % j, 4096, lambda i, j=j: _blk(i["w_out0"][0], 0, 8, j * 512, 512))
    for pe in range(2):
        for j in range(3):
            m0, mw = _split3(j)
            add("fg%d%d" % (pe, j), 8 * mw, lambda i, pe=pe, m0=m0, mw=mw: _blk(i["w_ffn_gate"][0], 0, 8, pe * DEXP + m0, mw))
            add("fu%d%d" % (pe, j), 8 * mw, lambda i, pe=pe, m0=m0, mw=mw: _blk(i["w_ffn_up"][0], 0, 8, pe * DEXP + m0, mw))
        for j in range(4):
            add("fd%d%d" % (pe, j), 11 * 256, lambda i, pe=pe, j=j: _blk(i["w_ffn_down"][0], pe * 11, 11, j * 256, 256))
    for l in range(2):
        for j in range(2):
            add("pg%d%d" % (l, j), 4096, lambda i, l=l, j=j: _blk(i["w_ple_gate"][l], 0, 8, j * 512, 512))
        add("pp%d" % l, 2048, lambda i, l=l: _blk(i["w_ple_proj"][l], 0, 2, 0, 1024))
    for c in range(8):
        def in1(i, c=c):
            w = i["w_in1"][0]
            w2 = np.concatenate([w[:, c * 128:(c + 1) * 128], w[:, 1024 + c * 128:1024 + (c + 1) * 128]], axis=1)
            return _blk(w2, 0, 8, 0, 256)
        add("in1%d" % c, 2048, in1)

    def rgig(i):
        a = i["w_rg"][0].transpose(1, 0, 2)
        b = i["w_ig"][0].transpose(1, 0, 2)
        return np.ascontiguousarray(np.stack([a, b], axis=2)).reshape(128, 2048)
    add("rgig", 2048, rgig)
    for j in range(2):
        add("out1%d" % j, 4096, lambda i, j=j: _blk(i["w_out1"][0], 0, 8, j * 512, 512))
    for e in range(NEXP):
        for j in range(3):
            m0, mw = _split3(j)
            add("eg%d%d" % (e, j), 8 * mw, lambda i, e=e, m0=m0, mw=mw: _blk(i["w_exp_gate"][0, e], 0, 8, m0, mw))
            add("eu%d%d" % (e, j), 8 * mw, lambda i, e=e, m0=m0, mw=mw: _blk(i["w_exp_up"][0, e], 0, 8, m0, mw))
        for j in range(4):
            add("ed%d%d" % (e, j), 11 * 256, lambda i, e=e, j=j: _blk(i["w_exp_down"][0, e], 0, 11, j * 256, 256))
    offs = {}
    off = 0
    for name, nel, fn in u:
        offs[name] = (off, nel)
        off += nel
    return u, offs, off


def cst_layout():
    names = [("identf", 128), ("mask", 128), ("nmix", 16), ("nffn", 16), ("nple", 16), ("nfin", 8),
             ("pscale", 4), ("qnorm", 2), ("kvnorm", 1), ("convw", 32), ("convb", 8), ("brg", 8), ("big", 8),
             ("lam", 8), ("wr", 64), ("r32t", 32), ("invc", 64)]
    offs = {}
    off = 0
    for n, c in names:
        offs[n] = (off, c)
        off += c
    return offs, off


def _fm(v):
    return np.ascontiguousarray(np.asarray(v, np.float32).reshape(-1, 128).T)


def build_cst(inp):
    offs, tot = cst_layout()
    c = np.zeros((128, tot), np.float32)

    def put(name, arr):
        o, n = offs[name]
        arr = np.asarray(arr, np.float32)
        c[:arr.shape[0], o:o + arr.shape[1]] = arr
    put("identf", np.eye(128, dtype=np.float32))
    q = np.arange(128)
    put("mask", np.where(q[None, :] <= q[:, None], 0.0, -30000.0).astype(np.float32))
    put("nmix", np.concatenate([_fm(inp["norm_mix"][0]), _fm(inp["norm_mix"][1])], axis=1))
    put("nffn", np.concatenate([_fm(inp["norm_ffn"][0]), _fm(inp["norm_ffn"][1])], axis=1))
    put("nple", np.concatenate([_fm(inp["norm_ple"][0]), _fm(inp["norm_ple"][1])], axis=1))
    put("nfin", _fm(inp["norm_final"]))
    put("pscale", _fm(inp["pool_scale"][0]))
    put("qnorm", _fm(inp["q_norm"][0]))
    put("kvnorm", _fm(inp["kv_norm"][0]))
    put("convw", np.concatenate([_fm(inp["conv_w"][0][k]) for k in range(4)], axis=1))
    put("convb", _fm(inp["conv_b"][0]))
    put("brg", _fm(inp["b_rg"][0]))
    put("big", _fm(inp["b_ig"][0]))
    put("lam", _fm(inp["lru_lambda"][0]))
    put("wr", np.ascontiguousarray(inp["w_router"][0].reshape(8, 128, 8).transpose(1, 0, 2)).reshape(128, 64))
    R = np.zeros((32, 32), np.float32)
    for i in range(16):
        R[i, 16 + i] = -1.0
        R[16 + i, i] = 1.0
    put("r32t", R.T)
    invc = np.zeros((128, 64), np.float32)
    for g, w in enumerate((2, 4, 8, 16)):
        for t in range(16):
            invc[:, g * 16 + t] = 1.0 / min(t + 1, w)
    put("invc", invc)
    return c


def build_rope():
    inv = (np.float32(10000.0) ** (-np.arange(0, 32, 2, dtype=np.float32) / np.float32(32))).astype(np.float32)
    pos = np.concatenate([np.arange(SEQ, dtype=np.float32), np.full(NSAMP, PAST_PAGES * PAGE, np.float32)])
    ang = (pos[:, None] * inv[None, :]).astype(np.float32)
    cos = np.cos(ang).astype(np.float32).T
    sin = np.sin(ang).astype(np.float32).T
    r = np.zeros((32, 2, SEQ + NSAMP), np.float32)
    r[0:16, 0] = cos
    r[16:32, 0] = cos
    r[0:16, 1] = sin
    r[16:32, 1] = sin
    return r


class _Stop(Exception):
    pass


def build_program(n_tiles=NT, samples=True, stop=0):
    def chk(stage):
        if stage == stop:
            raise _Stop()

    nc = bass.Bass("TRN2", target_bir_lowering=False)
    units, woffs, wtot = weight_units()
    coffs, ctot = cst_layout()
    last_tile = n_tiles - 1
    do_s = samples

    def din(name, shape, dt=F32):
        return nc.dram_tensor(name, list(shape), dt, kind="ExternalInput").ap()

    def dout(name, shape, dt=F32):
        return nc.dram_tensor(name, list(shape), dt, kind="ExternalOutput").ap()

    xp = din("xp", [SEQ, D])
    pp = din("pp", [2, SEQ, 256])
    wflat = din("wflat", [128, wtot])
    cst = din("cst", [128, ctot])
    rope = din("rope", [32, 2, SEQ + NSAMP])
    y_p = dout("y_p", [SEQ, D])
    rows_p = dout("rows_p", [SEQ, ROWW])
    pool_p = dout("pool_p", [15, 512])
    conv_p = dout("conv_p", [3, D])
    h_p = dout("h_p", [1, D])
    if do_s:
        xs = din("xs", [NSAMP, D])
        psm = din("psm", [2, NSAMP, 256])
        cache = din("cache", [NPOOLPG * NGCH, GCH * ROWW])
        spool = din("spool", [NSAMP, 15, 512])
        sconv = din("sconv", [NSAMP, 3, D])
        slru = din("slru", [NSAMP, D])
        ptab = din("ptab", [NSAMP, PAST_PAGES], I32)
        y_s = dout("y_s", [NSAMP, D])
        rows_s = dout("rows_s", [NSAMP, ROWW])
        pool_s = dout("pool_s", [NSAMP, 15, 512])
        conv_s = dout("conv_s", [NSAMP, 3, D])
        h_s = dout("h_s", [NSAMP, D])

    P = Prog(nc)
    with P.stack:
        X = P.sb("X", [128, 8, W], F32)
        XN = P.sb("XN", [128, 8, W], BF16)
        KLT = P.sb("KLT", [128, SEQ], BF16)
        KRT = P.sb("KRT", [32, SEQ], BF16)
        VT = P.sb("VT", [128, SEQ // 128, 128], BF16)
        UB = P.sb("UB", [128, 4, 16 + W], F32)
        CH = P.sb("CH", [128, 8, 4], F32)
        HS = P.sb("HS", [128, 8], F32)
        PT2 = P.sb("PT2", [128, 2, 2, W], BF16)
        CST = P.sb("CST", [128, ctot], F32)
        ROPE = P.sb("ROPE", [32, 2, W], F32)
        identb = P.sb("identb", [128, 128], BF16)
        maskb = P.sb("maskb", [128, 128], BF16)
        onesb = P.sb("onesb", [128, 128], BF16)
        onesf = P.sb("onesf", [128, 128], F32)
        GWR = P.sb("GWR", [128, 8, 8], F32)
        NSP = P.sb("NSP", [128, 16], F32)
        SQ = [P.sb("SQ%d" % i, [128, 512], BF16) for i in range(2)]
        NT1 = P.sb("NT1", [128, 512], F32)
        RSTD = P.sb("RSTD", [128, W], F32)
        slots = [P.sb("wslot%d" % i, [128, SLOT], BF16) for i in range(NSLOT)]
        SCR_BYTES = 72 * 1024
        P.scr = P.sb("SCR", [128, SCR_BYTES // 4], F32)
        P.scr_bytes = SCR_BYTES
        banks = [P.ps("bank%d" % i, [128, 512], F32) for i in range(8)]
        if do_s:
            IDX = P.sb("IDX", [128, NSAMP, NGCH], I32)
            PTT = P.sb("PTT", [128, NSAMP], I32)
            EXT = P.sb("EXT", [128, 4, NSAMP, 16], F32)
            CEX = P.sb("CEX", [128, 8, NSAMP, 4], F32)
            H0S = P.sb("H0S", [128, 8, NSAMP], F32)
            NXB = P.sb("NXB", [128, 8, NSAMP], F32)
            NU = P.sb("NU", [128, 4, NSAMP], F32)
            SEL = P.sb("SEL", [16, NSAMP, 8], F32)
            ROWS_S = P.sb("ROWS_S", [16, ROWW], F32)
            RLB = P.sb("RLB", [128, NSAMP], BF16)
            KROB = P.sb("KROB", [32, NSAMP], BF16)

        def C(name):
            o, c = coffs[name]
            return CST[:, o:o + c]

        identf = C("identf")

        def nb():
            i = P.nbank % 8
            P.nbank += 1
            return banks[i], "bank%d" % i

        def bfview(bank):
            return bank[:, 0:512].bitcast(BF16)

        def mm(out, lhsT, rhs, start, stop, reads, writes):
            P.add("pe", "matmul", out, lhsT, rhs, start=start, stop=stop, reads=reads, writes=writes)

        def tr(out, in_, ident, reads, writes):
            P.add("pe", "transpose", out, in_, ident, reads=reads, writes=writes)

        def act(out, in_, func, reads, writes, **kw):
            P.add("act", "activation", out, in_, func, reads=reads, writes=writes, **kw)

        def dve(meth, *args, reads, writes, **kw):
            P.add("dve", meth, *args, reads=reads, writes=writes, **kw)

        def dma(eng, out, in_, reads, writes, key, **kw):
            P.add(eng, "dma_start", out=out, in_=in_, reads=reads, writes=writes, dma=True, key=key, **kw)

        def act_copy(out, in_, reads, writes, scale=None):
            if scale is None:
                act(out, in_, AF.Copy, reads, writes)
            else:
                act(out, in_, AF.Copy, reads, writes, scale=scale)

        def dve_copy(out, in_, reads, writes):
            dve("tensor_copy", out, in_, reads=reads, writes=writes)

        def mmgroup(bank, btok, msz, n, pairs, reads):
            k = len(pairs)
            for i, (l, r) in enumerate(pairs):
                mm(bank[0:msz, 0:n], l, r, i == 0, i == k - 1, reads, [btok])

        wstate = {"u": 0}

        def wget(name):
            off, nel = woffs[name]
            s = wstate["u"] % NSLOT
            wstate["u"] += 1
            tok = "wslot%d" % s
            dma("pool", slots[s][:, 0:nel], wflat[:, off:off + nel], [], [tok], tok)
            return slots[s], tok

        def rmsnorm(src, src_tok, nch, dfeat, gain, dst, dst_tok, sts):
            for (c0, n) in sts:
                bank, btok = nb()
                for c in range(nch):
                    sq, sqt = SQ[c % 2], "SQ%d" % (c % 2)
                    act(sq[:, 0:n], src(c, c0, n), AF.Square, [src_tok], [sqt])
                    mm(bank[:, 0:n], onesb[:], sq[:, 0:n], c == 0, c == nch - 1, [sqt, "onesb"], [btok])
                dve("tensor_scalar", NT1[:, 0:n], bank[:, 0:n], 1.0 / dfeat, EPS, ALU.mult, ALU.add,
                    reads=[btok], writes=["NT1"])
                act(NT1[:, 0:n], NT1[:, 0:n], AF.Sqrt, ["NT1"], ["NT1"])
                dve("reciprocal", RSTD[:, c0:c0 + n], NT1[:, 0:n], reads=["NT1"], writes=["RSTD"])
                for c in range(nch):
                    dve("scalar_tensor_tensor", dst(c, c0, n), src(c, c0, n), gain(c), RSTD[:, c0:c0 + n],
                        ALU.mult, ALU.mult, reads=[src_tok, "RSTD", "CST"], writes=[dst_tok])

        def Xc(c, c0, n):
            return X[:, c, c0:c0 + n]

        def XNc(c, c0, n):
            return XN[:, c, c0:c0 + n]

        def out_tok16(srcs, dst_ap, tag):
            nch = len(srcs)
            stg = P.carve("OTS_" + tag, SCR_BYTES - 4096, [D], F32, 0, NSAMP)
            for h0 in range(0, nch, 4):
                bank, btok = nb()
                cnt = min(4, nch - h0)
                for ci in range(cnt):
                    tr(bank[0:NSAMP, ci * 128:(ci + 1) * 128], srcs[h0 + ci][0], identf, [srcs[h0 + ci][1], "CST"], [btok])
                dve_copy(stg[:, h0 * 128:(h0 + cnt) * 128], bank[0:NSAMP, 0:cnt * 128], [btok], ["OTS_" + tag])
            dma("sp", dst_ap, stg[:, 0:nch * 128], ["OTS_" + tag], [], "OTS_" + tag)

        dma("sp", CST[:], cst, [], ["CST"], "CST")
        dve_copy(identb[:], identf, ["CST"], ["identb"])
        dve_copy(maskb[:], C("mask"), ["CST"], ["maskb"])
        dve("memset", onesb[:], 1.0, reads=[], writes=["onesb"])
        dve("memset", onesf[:], 1.0, reads=[], writes=["onesf"])
        dve("memset", UB[:], 0.0, reads=[], writes=["UB"])
        dve("memset", CH[:], 0.0, reads=[], writes=["CH"])
        dve("memset", HS[:], 0.0, reads=[], writes=["HS"])
        for k in range(8):
            dve("tensor_scalar", GWR[:, k, :], C("wr")[:, k * 8:(k + 1) * 8], C("nffn")[:, 8 + k:9 + k], None, ALU.mult,
                reads=["CST"], writes=["GWR"])
        act(NSP[:, 0:8], C("lam"), AF.Exp, ["CST"], ["NSP"], scale=-1.0)
        act(NSP[:, 0:8], NSP[:, 0:8], AF.Ln, ["NSP"], ["NSP"], bias=1.0)
        dve("tensor_scalar", NSP[:, 8:16], NSP[:, 0:8], -16.0, None, ALU.mult, reads=["NSP"], writes=["NSP"])
        dve("tensor_scalar", NSP[:, 0:8], NSP[:, 0:8], -8.0, None, ALU.mult, reads=["NSP"], writes=["NSP"])

        if do_s:
            ST0 = P.carve("ST0", 0, [512], F32, 0, 120)
            ST1 = P.carve("ST1", 2048, [512], F32, 0, 120)
            spv = spool.rearrange("b r f -> (b r) f")
            dma("sp", ST0, spv[0:120, :], [], ["ST0"], "ST0")
            dma("sp", ST1, spv[120:240, :], [], ["ST1"], "ST1")
            for g in range(4):
                bank, btok = nb()
                tr(bank[:, 0:120], ST0[:, g * 128:(g + 1) * 128], identf[0:120, 0:120], ["ST0", "CST"], [btok])
                tr(bank[:, 120:240], ST1[:, g * 128:(g + 1) * 128], identf[0:120, 0:120], ["ST1", "CST"], [btok])
                dve_copy(EXT[:, g, :, 0:15], bank[:, 0:240].rearrange("p (b r) -> p b r", b=NSAMP), [btok], ["EXT"])
            ST2 = P.carve("ST2", 4096, [D], F32, 0, 48)
            dma("sp", ST2, sconv.rearrange("b r f -> (b r) f"), [], ["ST2"], "ST2")
            for c in range(8):
                bank, btok = nb()
                tr(bank[:, 0:48], ST2[:, c * 128:(c + 1) * 128], identf[0:48, 0:48], ["ST2", "CST"], [btok])
                dve_copy(CEX[:, c, :, 0:3], bank[:, 0:48].rearrange("p (b r) -> p b r", b=NSAMP), [btok], ["CEX"])
            ST3 = P.carve("ST3", 8192, [D], F32, 0, NSAMP)
            dma("sp", ST3, slru, [], ["ST3"], "ST3")
            bank, btok = nb()
            for c in range(8):
                tr(bank[:, c * 16:(c + 1) * 16], ST3[:, c * 128:(c + 1) * 128], identf[0:NSAMP, 0:NSAMP], ["ST3", "CST"], [btok])
            dve_copy(H0S[:], bank[:, 0:128].rearrange("p (c b) -> p c b", c=8), [btok], ["H0S"])
            dma("sp", PTT[:], ptab.rearrange("b j -> j b"), [], ["PTT"], "PTT", allow_slow_non_contiguous=True)
            for rc in range(NGCH):
                dve("tensor_scalar", IDX[:, :, rc], PTT[:], NGCH, rc, ALU.mult, ALU.add, reads=["PTT"], writes=["IDX"])
            dve_copy(SEL[:], identf[0:16, 0:16].unsqueeze(2).broadcast_to([16, NSAMP, 8]), ["CST"], ["SEL"])
            dma("sp", pool_s[:, 0:14, :], spool[:, 1:15, :], [], [], "pool_s_cp")
            dma("sp", conv_s[:, 0:2, :], sconv[:, 1:3, :], [], [], "conv_s_cp")

        A_QR = 0
        A_QL = A_QR + 8 * W * 2
        A_PY = A_QL + 8 * W * 2
        A_OT = A_PY + 4 * W * 2
        A_F = A_OT + 4 * W * 2

        def swiglu_expert(sts, gname, uname, dname, e_idx, CALL):
            H = [P.carve("H%d" % j, j * W * 2, [W], BF16) for j in range(11)]
            o = 11 * W * 2
            SG = [P.carve("SG%d" % i, o + i * 2048, [512], F32) for i in range(2)]
            o += 4096
            SG2 = [P.carve("SGB%d" % i, o + i * 2048, [512], F32) for i in range(2)]
            cnt = 0
            for j3 in range(3):
                m0, mw = _split3(j3)
                wg, wgtok = wget(gname + str(j3))
                wu, wutok = wget(uname + str(j3))
                wgv = wg[:, 0:8 * mw].rearrange("p (k m) -> p k m", k=8)
                wuv_ = wu[:, 0:8 * mw].rearrange("p (k m) -> p k m", k=8)
                for ci in range(mw // 128):
                    jc = j3 * 4 + ci
                    for (c0, n) in sts:
                        bg, bgtok = nb()
                        mmgroup(bg, bgtok, 128, n, [(wgv[:, k, ci * 128:(ci + 1) * 128], XNc(k, c0, n)) for k in range(8)],
                                [wgtok, "XN"])
                        bu, butok = nb()
                        mmgroup(bu, butok, 128, n, [(wuv_[:, k, ci * 128:(ci + 1) * 128], XNc(k, c0, n)) for k in range(8)],
                                [wutok, "XN"])
                        sg, sgtok = SG[cnt % 2], "SG%d" % (cnt % 2)
                        sg2, sg2tok = SG2[cnt % 2], "SGB%d" % (cnt % 2)
                        cnt += 1
                        act(sg[:, 0:n], bg[:, 0:n], AF.Silu, [bgtok], [sgtok])
                        if e_idx is None:
                            dve("tensor_tensor", H[jc][:, c0:c0 + n], sg[:, 0:n], bu[:, 0:n], ALU.mult,
                                reads=[sgtok, butok], writes=["H%d" % jc])
                        else:
                            dve("tensor_tensor", sg2[:, 0:n], sg[:, 0:n], bu[:, 0:n], ALU.mult,
                                reads=[sgtok, butok], writes=[sg2tok])
                            dve("tensor_tensor", H[jc][:, c0:c0 + n], sg2[:, 0:n], CALL[:, e_idx, c0:c0 + n], ALU.mult,
                                reads=[sg2tok, "CALL"], writes=["H%d" % jc])
            htoks = ["H%d" % j for j in range(11)]
            for j4 in range(4):
                wd, wdtok = wget(dname + str(j4))
                wdv = wd[:, 0:11 * 256].rearrange("p (k m) -> p k m", k=11)
                for mi in range(2):
                    m = j4 * 2 + mi
                    for (c0, n) in sts:
                        bank, btok = nb()
                        mmgroup(bank, btok, 128, n, [(wdv[:, k, mi * 128:(mi + 1) * 128], H[k][:, c0:c0 + n]) for k in range(11)],
                                [wdtok] + htoks)
                        dve("tensor_tensor", X[:, m, c0:c0 + n], X[:, m, c0:c0 + n], bank[:, 0:n], ALU.add,
                            reads=["X", btok], writes=["X"])

        def ple(sts, l):
            rmsnorm(Xc, "X", 8, D, lambda c: C("nple")[:, l * 8 + c:l * 8 + c + 1], XNc, "XN", sts)
            o = 11 * W * 2
            SG = [P.carve("SG%d" % i, o + i * 2048, [512], F32) for i in range(2)]
            wp, wptok = wget("pp%d" % l)
            wpv = wp[:, 0:2048].rearrange("p (k m) -> p k m", k=2)
            cnt = 0
            for j in range(2):
                wg, wgtok = wget("pg%d%d" % (l, j))
                wgv = wg[:, 0:4096].rearrange("p (k m) -> p k m", k=8)
                for mi in range(4):
                    m = j * 4 + mi
                    for (c0, n) in sts:
                        bg, bgtok = nb()
                        mmgroup(bg, bgtok, 128, n, [(wgv[:, k, mi * 128:(mi + 1) * 128], XNc(k, c0, n)) for k in range(8)],
                                [wgtok, "XN"])
                        bp, bptok = nb()
                        mmgroup(bp, bptok, 128, n, [(wpv[:, k, m * 128:(m + 1) * 128], PT2[:, l, k, c0:c0 + n]) for k in range(2)],
                                [wptok, "PT2"])
                        sg, sgtok = SG[cnt % 2], "SG%d" % (cnt % 2)
                        cnt += 1
                        act(sg[:, 0:n], bg[:, 0:n], AF.Sigmoid, [bgtok], [sgtok])
                        dve("tensor_tensor", sg[:, 0:n], sg[:, 0:n], bp[:, 0:n], ALU.mult, reads=[sgtok, bptok], writes=[sgtok])
                        dve("tensor_tensor", X[:, m, c0:c0 + n], X[:, m, c0:c0 + n], sg[:, 0:n], ALU.add,
                            reads=["X", sgtok], writes=["X"])

        def router(sts):
            o = 11 * W * 2 + 8192
            CALL = P.carve("CALL", o, [8, W], BF16)
            o += 8 * W * 2
            LG = P.carve("LG", o, [8], F32)
            TOP = P.carve("TOP", o + 32, [8], F32)
            CB = P.carve("CB", o + 64, [8], F32)
            CB2 = P.carve("CB2", o + 96, [8], F32)
            WV = P.carve("WV", o + 128, [8], F32)
            o += 160
            DG = [P.carve("DG%d" % i, o + i * 512, [128], F32) for i in range(2)]
            cnt = 0
            for (c0, n) in sts:
                for tb in range((n + 127) // 128):
                    nt = min(128, n - tb * 128)
                    tc0 = c0 + tb * 128
                    bank, btok = nb()
                    for k in range(8):
                        mm(bank[0:nt, 0:8], X[:, k, tc0:tc0 + nt], GWR[:, k, :], k == 0, k == 7, ["X", "GWR"], [btok])
                    b2, b2tok = nb()
                    tr(b2[0:nt, 0:128], RSTD[:, tc0:tc0 + nt], identf, ["RSTD", "CST"], [b2tok])
                    dve_copy(WV[0:nt, 3:4], b2[0:nt, 0:1], [b2tok], ["WV"])
                    dve("tensor_scalar", LG[0:nt, :], bank[0:nt, 0:8], WV[0:nt, 3:4], None, ALU.mult,
                        reads=[btok, "WV"], writes=["LG"])
                    dve("max", TOP[0:nt, :], LG[0:nt, :], reads=["LG"], writes=["TOP"])
                    dve("tensor_tensor", WV[0:nt, 0:1], TOP[0:nt, 1:2], TOP[0:nt, 0:1], ALU.subtract,
                        reads=["TOP", "WV"], writes=["WV"])
                    act(WV[0:nt, 1:2], WV[0:nt, 0:1], AF.Sigmoid, ["WV"], ["WV"])
                    act(WV[0:nt, 2:3], WV[0:nt, 0:1], AF.Sigmoid, ["WV"], ["WV"], scale=-1.0)
                    dve("tensor_scalar", CB[0:nt, :], LG[0:nt, :], TOP[0:nt, 0:1], WV[0:nt, 2:3], ALU.is_equal, ALU.mult,
                        reads=["LG", "TOP", "WV"], writes=["CB"])
                    dve("tensor_scalar", CB2[0:nt, :], LG[0:nt, :], TOP[0:nt, 1:2], WV[0:nt, 1:2], ALU.is_equal, ALU.mult,
                        reads=["LG", "TOP", "WV"], writes=["CB2"])
                    dve("tensor_tensor", CB[0:nt, :], CB[0:nt, :], CB2[0:nt, :], ALU.add, reads=["CB", "CB2"], writes=["CB"])
                    for e_ in range(NEXP):
                        dg, dgtok = DG[cnt % 2], "DG%d" % (cnt % 2)
                        cnt += 1
                        dve("tensor_scalar", dg[0:nt, 0:nt], identf[0:nt, 0:nt], CB[0:nt, e_:e_ + 1], None, ALU.mult,
                            reads=["CST", "CB"], writes=[dgtok])
                        bc, bctok = nb()
                        mm(bc[:, 0:nt], onesf[0:nt, :], dg[0:nt, 0:nt], True, True, ["onesf", dgtok], [bctok])
                        act_copy(CALL[:, e_, tc0:tc0 + nt], bc[:, 0:nt], [bctok], ["CALL"])
            return CALL

        def rglru(t, sts):
            YL = P.carve("YL", 0, [8, W], BF16)
            o = 8 * W * 2
            RGIG = P.carve("RGIG", o, [8, 2, 128], BF16)
            o += 4096
            off, nel = woffs["rgig"]
            dma("pool", RGIG.rearrange("p a b c -> p (a b c)"), wflat[:, off:off + nel], [], ["RGIG"], "RGIG")
            names = ["XB", "GG", "TT", "XC", "RR", "IG", "AA", "BT", "HH"]
            tmp = []
            for par in range(2):
                d = {}
                for nm in names:
                    wd_ = (4 + W) if nm == "XB" else W
                    d[nm] = (P.carve("%s%d" % (nm, par), o, [wd_], F32), "%s%d" % (nm, par))
                    o += wd_ * 4
                d["XCB"] = (P.carve("XCB%d" % par, o, [W], BF16), "XCB%d" % par)
                o += W * 2
                tmp.append(d)
            def rg_a(c):
                T_ = tmp[c % 2]
                XB, xbt = T_["XB"]
                GG, ggt = T_["GG"]
                TT, ttt = T_["TT"]
                XC, xct = T_["XC"]
                RR, rrt = T_["RR"]
                IG, igt = T_["IG"]
                AA, aat = T_["AA"]
                BT, btt = T_["BT"]
                HH, hht = T_["HH"]
                XCB, xcbt = T_["XCB"]

                wt, wtok = wget("in1%d" % c)
                wv = wt[:, 0:2048].rearrange("p (k m) -> p k m", k=8)
                for (c0, n) in sts:
                    is_s = c0 >= TW
                    bx, bxtok = nb()
                    mmgroup(bx, bxtok, 128, n, [(wv[:, k, 0:128], XNc(k, c0, n)) for k in range(8)], [wtok, "XN"])
                    bg, bgtok = nb()
                    mmgroup(bg, bgtok, 128, n, [(wv[:, k, 128:256], XNc(k, c0, n)) for k in range(8)], [wtok, "XN"])
                    act_copy(GG[:, c0:c0 + n], bg[:, 0:n], [bgtok], [ggt])
                    act(TT[:, c0:c0 + n], GG[:, c0:c0 + n], AF.Square, [ggt], [ttt])
                    dve("tensor_scalar", TT[:, c0:c0 + n], TT[:, c0:c0 + n], 0.044715, 1.0, ALU.mult, ALU.add,
                        reads=[ttt], writes=[ttt])
                    dve("tensor_tensor", TT[:, c0:c0 + n], TT[:, c0:c0 + n], GG[:, c0:c0 + n], ALU.mult, reads=[ttt, ggt], writes=[ttt])
                    act(TT[:, c0:c0 + n], TT[:, c0:c0 + n], AF.Sigmoid, [ttt], [ttt], scale=1.5957691216057308)
                    dve("tensor_tensor", GG[:, c0:c0 + n], TT[:, c0:c0 + n], GG[:, c0:c0 + n], ALU.mult, reads=[ttt, ggt], writes=[ggt])
                    cw = C("convw")
                    if not is_s:
                        act_copy(XB[:, 0:4], CH[:, c, :], ["CH"], [xbt])
                        act_copy(XB[:, 4:4 + n], bx[:, 0:n], [bxtok], [xbt])
                        dve_copy(CH[:, c, :], XB[:, n:n + 4], [xbt], ["CH"])
                        dve("tensor_scalar", XC[:, 0:n], XB[:, 1:1 + n], cw[:, c:c + 1], C("convb")[:, c:c + 1],
                            ALU.mult, ALU.add, reads=[xbt, "CST"], writes=[xct])
                        for k in range(1, 4):
                            dve("scalar_tensor_tensor", XC[:, 0:n], XB[:, 1 + k:1 + k + n], cw[:, k * 8 + c:k * 8 + c + 1],
                                XC[:, 0:n], ALU.mult, ALU.add, reads=[xbt, xct, "CST"], writes=[xct])
                    else:
                        act_copy(CEX[:, c, :, 3], bx[:, 0:n], [bxtok], ["CEX"])
                        act_copy(NXB[:, c, :], bx[:, 0:n], [bxtok], ["NXB"])
                        dve("tensor_scalar", XC[:, c0:c0 + n], CEX[:, c, :, 0], cw[:, c:c + 1], C("convb")[:, c:c + 1],
                            ALU.mult, ALU.add, reads=["CEX", "CST"], writes=[xct])
                        for k in range(1, 4):
                            dve("scalar_tensor_tensor", XC[:, c0:c0 + n], CEX[:, c, :, k], cw[:, k * 8 + c:k * 8 + c + 1],
                                XC[:, c0:c0 + n], ALU.mult, ALU.add, reads=["CEX", xct, "CST"], writes=[xct])
                    act_copy(XCB[:, c0:c0 + n], XC[:, c0:c0 + n], [xct], [xcbt])

            def rg_b(c):
                T_ = tmp[c % 2]
                XB, xbt = T_["XB"]
                GG, ggt = T_["GG"]
                TT, ttt = T_["TT"]
                XC, xct = T_["XC"]
                RR, rrt = T_["RR"]
                IG, igt = T_["IG"]
                AA, aat = T_["AA"]
                BT, btt = T_["BT"]
                HH, hht = T_["HH"]
                XCB, xcbt = T_["XCB"]

                for (c0, n) in sts:
                    is_s = c0 >= TW
                    br, brtok = nb()
                    mm(br[:, 0:n], RGIG[:, c, 0, :], XCB[:, c0:c0 + n], True, True, ["RGIG", xcbt], [brtok])
                    bi_, bitok = nb()
                    mm(bi_[:, 0:n], RGIG[:, c, 1, :], XCB[:, c0:c0 + n], True, True, ["RGIG", xcbt], [bitok])
                    act(RR[:, c0:c0 + n], br[:, 0:n], AF.Sigmoid, [brtok, "CST"], [rrt], bias=C("brg")[:, c:c + 1])
                    act(IG[:, c0:c0 + n], bi_[:, 0:n], AF.Sigmoid, [bitok, "CST"], [igt], bias=C("big")[:, c:c + 1])
                    act(AA[:, c0:c0 + n], RR[:, c0:c0 + n], AF.Exp, [rrt, "NSP"], [aat], scale=NSP[:, c:c + 1])
                    act(BT[:, c0:c0 + n], RR[:, c0:c0 + n], AF.Exp, [rrt, "NSP"], [btt], scale=NSP[:, 8 + c:9 + c])
                    act(BT[:, c0:c0 + n], BT[:, c0:c0 + n], AF.Sqrt, [btt], [btt], scale=-1.0, bias=1.0)
                    dve("tensor_tensor", IG[:, c0:c0 + n], IG[:, c0:c0 + n], XC[:, c0:c0 + n], ALU.mult, reads=[igt, xct], writes=[igt])
                    dve("tensor_tensor", BT[:, c0:c0 + n], BT[:, c0:c0 + n], IG[:, c0:c0 + n], ALU.mult, reads=[btt, igt], writes=[btt])
                    if not is_s:
                        dve("tensor_tensor_scan", HH[:, 0:n], AA[:, 0:n], BT[:, 0:n], HS[:, c:c + 1], ALU.mult, ALU.add,
                            reads=[aat, btt, "HS"], writes=[hht])
                        dve_copy(HS[:, c:c + 1], HH[:, n - 1:n], [hht], ["HS"])
                    else:
                        dve("tensor_tensor", HH[:, c0:c0 + n], AA[:, c0:c0 + n], H0S[:, c, :], ALU.mult, reads=[aat, "H0S"], writes=[hht])
                        dve("tensor_tensor", HH[:, c0:c0 + n], HH[:, c0:c0 + n], BT[:, c0:c0 + n], ALU.add, reads=[hht, btt], writes=[hht])
                        dve_copy(H0S[:, c, :], HH[:, c0:c0 + n], [hht], ["H0S"])
                    dve("tensor_tensor", YL[:, c, c0:c0 + n], HH[:, c0:c0 + n], GG[:, c0:c0 + n], ALU.mult, reads=[hht, ggt], writes=["YL"])

            rg_a(0)
            for c in range(8):
                if c + 1 < 8:
                    rg_a(c + 1)
                rg_b(c)
            for j in range(2):
                wt, wtok = wget("out1%d" % j)
                wv = wt[:, 0:4096].rearrange("p (k m) -> p k m", k=8)
                for mi in range(4):
                    m = j * 4 + mi
                    for (c0, n) in sts:
                        bank, btok = nb()
                        mmgroup(bank, btok, 128, n, [(wv[:, k, mi * 128:(mi + 1) * 128], YL[:, k, c0:c0 + n]) for k in range(8)],
                                [wtok, "YL"])
                        dve("tensor_tensor", X[:, m, c0:c0 + n], X[:, m, c0:c0 + n], bank[:, 0:n], ALU.add,
                            reads=["X", btok], writes=["X"])
            if t == last_tile:
                for c in range(8):
                    dma("sp", conv_p[:, c * 128:(c + 1) * 128].rearrange("r f -> f r"), CH[:, c, 1:4], ["CH"], [], "conv_p",
                        allow_slow_non_contiguous=True)
                dma("sp", h_p.rearrange("o (c f) -> f (o c)", f=128), HS[:], ["HS"], [], "h_p", allow_slow_non_contiguous=True)
                if do_s:
                    out_tok16([(NXB[:, c, :], "NXB") for c in range(8)], conv_s[:, 2, :], "cs")
                    out_tok16([(H0S[:, c, :], "H0S") for c in range(8)], h_s, "hs")

        def decode_attention(QL, QR, RL, KRO, OT, wuv, wuvtok):
            o = A_F
            G = [P.carve("G%d" % i, o + i * GCH * ROWW * 4, [GCH, ROWW], F32) for i in range(2)]
            o += 2 * GCH * ROWW * 4
            KTL = [P.carve("KTL%d" % i, o + i * GCH * 256, [GCH, 128], BF16) for i in range(2)]
            o += 2 * GCH * 256
            KTR = [P.carve("KTR%d" % i, o + i * GCH * 256, [GCH, 128], BF16, 0, 32) for i in range(2)]
            o += 2 * GCH * 256
            STB = P.carve("STB", o, [GCH, 8], F32)
            o += GCH * 32
            PSB = P.carve("PSB", o, [GCH, 8], F32)
            o += GCH * 32
            MXJ = P.carve("MXJ", o, [8], F32)
            MBC = P.carve("MBC", o + 32, [8], F32)
            o += 64
            QLS = P.carve("QLS", o, [NSAMP, 8], BF16)
            o += NSAMP * 16
            QRS = P.carve("QRS", o, [NSAMP, 8], BF16, 0, 32)
            o += NSAMP * 16
            MOLD = P.carve("MOLD", o, [NSAMP], F32, 0, 8)
            o += 64
            SMALL = P.carve("SMALL", o, [8], F32, 0, 8)
            o += 32
            DIAG = P.carve("DIAG", o, [8], F32, 0, 8)
            o += 32
            OACC = P.carve("OACC", o, [132], F32, 0, 8)
            o += 528
            OL1 = P.carve("OL1", o, [128], F32, 0, 8)
            o += 512
            OLST = P.carve("OLST", o, [8, NSAMP], BF16)
            o += 256
            OTOKS = P.carve("OTOKS", o, [512], BF16, 0, NSAMP)
            o += 1024
            dve_copy(QLS, QL[:, :, TW:W].rearrange("p h b -> p b h"), ["QL"], ["QLS"])
            dve_copy(QRS, QR[:, :, TW:W].rearrange("p h b -> p b h"), ["QR"], ["QRS"])
            bn, bntok = nb()
            for b in range(NSAMP):
                mm(bn[0:8, b:b + 1], QLS[:, b, :], RLB[:, b:b + 1], True, False, ["QLS", "RLB"], [bntok])
                mm(bn[0:8, b:b + 1], QRS[:, b, :], KROB[:, b:b + 1], False, True, ["QRS", "KROB"], [bntok])
            dve_copy(MOLD, bn[0:8, 0:NSAMP], [bntok], ["MOLD"])
            gcnt = 0
            for b in range(NSAMP):
                bv0, bv0tok = nb()
                mm(bv0[0:8, 0:128], SEL[:, b, :], ROWS_S[:, 0:128], True, True, ["SEL", "ROWS_S"], [bv0tok])
                dve_copy(OACC[:, 0:128], bv0[0:8, 0:128], [bv0tok], ["OACC"])
                dve("memset", OACC[:, 128:129], 1.0, reads=[], writes=["OACC"])
                for rc in range(NGCH):
                    gp = gcnt % 2
                    gcnt += 1
                    g, gtok = G[gp], "G%d" % gp
                    ktl, ktltok = KTL[gp], "KTL%d" % gp
                    ktr, ktrtok = KTR[gp], "KTR%d" % gp
                    P.add("pool", "indirect_dma_start", out=g.rearrange("p r c -> p (r c)"), out_offset=None, in_=cache,
                          in_offset=bass.IndirectOffsetOnAxis(ap=IDX[:, b, rc:rc + 1], axis=0),
                          reads=["IDX"], writes=[gtok], dma=True, key=gtok)
                    for q4 in range(GCH // 4):
                        ba, batok = nb()
                        bb, bbtok = nb()
                        for i in range(4):
                            r = q4 * 4 + i
                            tr(ba[:, i * 128:(i + 1) * 128], g[:, r, 0:128], identf, [gtok, "CST"], [batok])
                        for i in range(4):
                            r = q4 * 4 + i
                            tr(bb[0:32, i * 128:(i + 1) * 128], g[:, r, 128:160], identf, [gtok, "CST"], [bbtok])
                        act_copy(ktl[:, q4 * 4:(q4 + 1) * 4, :], ba[:, 0:512].rearrange("p (a b) -> p a b", a=4), [batok], [ktltok])
                        dve_copy(ktr[:, q4 * 4:(q4 + 1) * 4, :], bb[0:32, 0:512].rearrange("p (a b) -> p a b", a=4), [bbtok], [ktrtok])
                    bs, bstok = nb()
                    for r in range(GCH):
                        mm(bs[:, r * 8:(r + 1) * 8], ktl[:, r, :], QLS[:, b, :], True, False, [ktltok, "QLS"], [bstok])
                        mm(bs[:, r * 8:(r + 1) * 8], ktr[:, r, :], QRS[:, b, :], False, True, [ktrtok, "QRS"], [bstok])
                    bsv = bs[:, 0:GCH * 8].rearrange("p (r h) -> p r h", h=8)
                    dve("tensor_reduce", MXJ, bsv.rearrange("p r h -> p h r"), AX.X, ALU.max, reads=[bstok], writes=["MXJ"])
                    bm, bmtok = nb()
                    tr(bm[0:8, 0:128], MXJ, identf, ["MXJ", "CST"], [bmtok])
                    dve("tensor_reduce", SMALL[:, 0:1], bm[0:8, 0:128], AX.X, ALU.max, reads=[bmtok], writes=["SMALL"])
                    dve("tensor_tensor", SMALL[:, 1:2], MOLD[:, b:b + 1], SMALL[:, 0:1], ALU.max, reads=["MOLD", "SMALL"], writes=["SMALL"])
                    dve("tensor_tensor", SMALL[:, 2:3], MOLD[:, b:b + 1], SMALL[:, 1:2], ALU.subtract, reads=["MOLD", "SMALL"], writes=["SMALL"])
                    act(SMALL[:, 2:3], SMALL[:, 2:3], AF.Exp, ["SMALL"], ["SMALL"])
                    dve_copy(MOLD[:, b:b + 1], SMALL[:, 1:2], ["SMALL"], ["MOLD"])
                    dve("tensor_scalar", DIAG, identf[0:8, 0:8], SMALL[:, 1:2], None, ALU.mult, reads=["CST", "SMALL"], writes=["DIAG"])
                    bd, bdtok = nb()
                    mm(bd[:, 0:8], onesf[0:8, :], DIAG, True, True, ["onesf", "DIAG"], [bdtok])
                    act_copy(MBC, bd[:, 0:8], [bdtok], ["MBC"])
                    dve("tensor_tensor", STB, bsv, MBC.unsqueeze(1).broadcast_to([128, GCH, 8]), ALU.subtract,
                        reads=[bstok, "MBC"], writes=["STB"])
                    act(PSB, STB, AF.Exp, ["STB"], ["PSB"])
                    dve("memset", g[:, :, 128:129], 1.0, reads=[], writes=[gtok])
                    bo, botok = nb()
                    for r in range(GCH):
                        mm(bo[0:8, 0:129], PSB[:, r, :], g[:, r, 0:129], r == 0, r == GCH - 1, ["PSB", gtok], [botok])
                    dve("scalar_tensor_tensor", OACC[:, 0:129], OACC[:, 0:129], SMALL[:, 2:3], bo[0:8, 0:129], ALU.mult, ALU.add,
                        reads=["OACC", "SMALL", botok], writes=["OACC"])
                dve("reciprocal", SMALL[:, 3:4], OACC[:, 128:129], reads=["OACC", "SMALL"], writes=["SMALL"])
                dve("tensor_scalar", OL1, OACC[:, 0:128], SMALL[:, 3:4], None, ALU.mult, reads=["OACC", "SMALL"], writes=["OL1"])
                bt_, bttok = nb()
                tr(bt_[:, 0:8], OL1, identf[0:8, 0:8], ["OL1", "CST"], [bttok])
                act_copy(OLST[:, :, b], bt_[:, 0:8], [bttok], ["OLST"])
            bv, bvtok = nb()
            for h in range(8):
                mm(bv[0:NSAMP, h * 64:(h + 1) * 64], OLST[:, h, :], wuv[:, h * 64:(h + 1) * 64], True, True, ["OLST", wuvtok], [bvtok])
            dve_copy(OTOKS, bv[0:NSAMP, 0:512], [bvtok], ["OTOKS"])
            b2, b2tok = nb()
            b2v = bfview(b2)
            for j in range(4):
                tr(b2v[:, j * 16:(j + 1) * 16], OTOKS[:, j * 128:(j + 1) * 128], identb[0:NSAMP, 0:NSAMP], ["OTOKS", "identb"], [b2tok])
            act_copy(OT[:, :, TW:W], b2v[:, 0:64].rearrange("p (j n) -> p j n", j=4), [b2tok], ["OT"])

        try:
          for t in range(n_tiles):
              sts = [(0, TW)]
              if do_s and t == last_tile:
                  sts.append((TW, NSAMP))
              g0 = t * TW
              XS = P.carve("XS", A_F, [4, D], F32)
              PS_ = P.carve("PS_", A_F + 16384, [2, 4, 256], F32)
              for b in range(4):
                  dma("sp", XS[:, b, :], xp[g0 + b * 128:g0 + (b + 1) * 128, :], [], ["XS"], "XS%d" % b)
              for l in range(2):
                  dma("sp", PS_[:, l, :, :], pp[l, g0:g0 + TW, :].rearrange("(b p) f -> p b f", p=128), [], ["PS_"], "PS_%d" % l)
              dma("sp", ROPE[:, :, 0:TW], rope[:, :, g0:g0 + TW], [], ["ROPE"], "ROPEa")
              if do_s and t == last_tile:
                  dma("sp", ROPE[:, :, TW:W], rope[:, :, SEQ:SEQ + NSAMP], [], ["ROPE"], "ROPEb")
              for c in range(8):
                  bank, btok = nb()
                  for b in range(4):
                      tr(bank[:, b * 128:(b + 1) * 128], XS[:, b, c * 128:(c + 1) * 128], identf, ["XS", "CST"], [btok])
                  if c % 2 == 0:
                      act_copy(X[:, c, 0:TW], bank[:, 0:TW], [btok], ["X"])
                  else:
                      dve_copy(X[:, c, 0:TW], bank[:, 0:TW], [btok], ["X"])
              for l in range(2):
                  for k in range(2):
                      bank, btok = nb()
                      for b in range(4):
                          tr(bank[:, b * 128:(b + 1) * 128], PS_[:, l, b, k * 128:(k + 1) * 128], identf, ["PS_", "CST"], [btok])
                      act_copy(PT2[:, l, k, 0:TW], bank[:, 0:TW], [btok], ["PT2"])
              if do_s and t == last_tile:
                  XSS = P.carve("XSS", A_F + 24576, [D], F32, 0, NSAMP)
                  PSS = P.carve("PSS", A_F + 28672, [2, 256], F32, 0, NSAMP)
                  dma("sp", XSS, xs, [], ["XSS"], "XSS")
                  dma("sp", PSS, psm.rearrange("l b f -> b l f"), [], ["PSS"], "PSS")
                  bank, btok = nb()
                  for c in range(8):
                      tr(bank[:, c * 16:(c + 1) * 16], XSS[:, c * 128:(c + 1) * 128], identf[0:NSAMP, 0:NSAMP], ["XSS", "CST"], [btok])
                  dve_copy(X[:, :, TW:W], bank[:, 0:128].rearrange("p (c n) -> p c n", c=8), [btok], ["X"])
                  bank, btok = nb()
                  for l in range(2):
                      for k in range(2):
                          j = l * 2 + k
                          tr(bank[:, j * 16:(j + 1) * 16], PSS[:, l, k * 128:(k + 1) * 128], identf[0:NSAMP, 0:NSAMP], ["PSS", "CST"], [btok])
                  dve_copy(PT2[:, :, :, TW:W], bank[:, 0:64].rearrange("p (l k n) -> p l k n", l=2, k=2), [btok], ["PT2"])

              chk(1)
              rmsnorm(Xc, "X", 8, D, lambda c: C("nmix")[:, c:c + 1], XNc, "XN", sts)
              chk(2)
              CQ = P.carve("CQ", A_F, [2, W], F32)
              CKV = P.carve("CKV", A_F + 2 * W * 4, [W], F32)
              KR = P.carve("KR", A_F + 3 * W * 4, [W], F32, 0, 32)
              RL = P.carve("RL", A_F + 4 * W * 4, [W], F32)
              KRO = P.carve("KRO", A_F + 5 * W * 4, [W], F32, 0, 32)
              o1 = A_F + 6 * W * 4
              CQN = P.carve("CQN", o1, [2, W], BF16)
              o1 += 2 * W * 2
              PL = P.carve("PL", o1, [4, W], BF16)
              o1 += 4 * W * 2
              TA = P.carve("TA", o1, [16 + TW], F32)
              o1 += (16 + TW) * 4
              TB = P.carve("TB", o1, [16 + TW], F32)
              o1 += (16 + TW) * 4
              QN = [P.carve("QN%d" % i, o1 + i * W * 2, [W], BF16, 0, 64) for i in range(2)]
              o1 += 2 * W * 2
              QRR = P.carve("QRR", o1, [W], F32, 0, 32)
              o1 += W * 4
              T1 = P.carve("T1", o1, [TW], F32, 0, 32)
              o1 += TW * 4
              T2 = P.carve("T2", o1, [TW], F32, 0, 32)
              o1 += TW * 4
              ROWST = P.carve("ROWST", o1, [4, ROWW], F32)
              o1 += 4 * ROWW * 4
              QR = P.carve("QR", A_QR, [8, W], BF16, 0, 32)
              QL = P.carve("QL", A_QL, [8, W], BF16)
              PY = P.carve("PY", A_PY, [4, W], BF16)
              OT = P.carve("OT", A_OT, [4, W], BF16)

              wt, wtok = wget("in0a")
              wv = wt[:, 0:4096].rearrange("p (k m) -> p k m", k=8)
              for g in range(4):
                  for (c0, n) in sts:
                      bank, btok = nb()
                      mmgroup(bank, btok, 128, n, [(wv[:, k, g * 128:(g + 1) * 128], XNc(k, c0, n)) for k in range(8)], [wtok, "XN"])
                      act_copy(UB[:, g, 16 + c0:16 + c0 + n], bank[:, 0:n], [btok], ["UB"])
              wt, wtok = wget("in0b")
              wv = wt[:, 0:8 * 416].rearrange("p (k m) -> p k m", k=8)
              for (c0, n) in sts:
                  for j in range(2):
                      bank, btok = nb()
                      mmgroup(bank, btok, 128, n, [(wv[:, k, j * 128:(j + 1) * 128], XNc(k, c0, n)) for k in range(8)], [wtok, "XN"])
                      dve_copy(CQ[:, j, c0:c0 + n], bank[:, 0:n], [btok], ["CQ"])
                  bank, btok = nb()
                  mmgroup(bank, btok, 128, n, [(wv[:, k, 256:384], XNc(k, c0, n)) for k in range(8)], [wtok, "XN"])
                  act_copy(CKV[:, c0:c0 + n], bank[:, 0:n], [btok], ["CKV"])
                  bank, btok = nb()
                  mmgroup(bank, btok, 32, n, [(wv[:, k, 384:416], XNc(k, c0, n)) for k in range(8)], [wtok, "XN"])
                  dve_copy(KR[:, c0:c0 + n], bank[0:32, 0:n], [btok], ["KR"])

              chk(3)
              for g, wdw in enumerate((2, 4, 8, 16)):
                  U = UB[:, g, :]
                  cur, curtok = U, "UB"
                  valid = 1
                  d = 1
                  bufs = [(TA, "TA"), (TB, "TB")]
                  bi = 0
                  while d < wdw:
                      dst, dtok = bufs[bi]
                      bi ^= 1
                      lo = valid + d
                      dve("tensor_tensor", dst[:, lo:16 + TW], cur[:, lo:16 + TW], cur[:, lo - d:16 + TW - d], ALU.add,
                          reads=[curtok], writes=[dtok])
                      cur, curtok = dst, dtok
                      valid = lo
                      d *= 2
                  dve("scalar_tensor_tensor", PL[:, g, 0:TW], cur[:, 16:16 + TW], 1.0 / wdw, U[:, 16:16 + TW], ALU.mult, ALU.subtract,
                      reads=[curtok, "UB"], writes=["PL"])
                  if t == 0:
                      dve("tensor_tensor", NT1[:, 0:16], cur[:, 16:32], C("invc")[:, g * 16:(g + 1) * 16], ALU.mult,
                          reads=[curtok, "CST"], writes=["NT1"])
                      dve("tensor_tensor", PL[:, g, 0:16], NT1[:, 0:16], U[:, 16:32], ALU.subtract, reads=["NT1", "UB"], writes=["PL"])
              chk(31)
              if do_s and t == last_tile:
                  for g in range(4):
                      dve_copy(EXT[:, g, :, 15], UB[:, g, 16 + TW:16 + W], ["UB"], ["EXT"])
                      dve_copy(NU[:, g, :], UB[:, g, 16 + TW:16 + W], ["UB"], ["NU"])
                  for g, wdw in enumerate((2, 4, 8, 16)):
                      dve("tensor_reduce", NT1[:, 0:NSAMP], EXT[:, g, :, 16 - wdw:16], AX.X, ALU.add, reads=["EXT"], writes=["NT1"])
                      dve("scalar_tensor_tensor", PL[:, g, TW:W], NT1[:, 0:NSAMP], 1.0 / wdw, UB[:, g, 16 + TW:16 + W],
                          ALU.mult, ALU.subtract, reads=["NT1", "UB"], writes=["PL"])
              if t == last_tile:
                  for g in range(4):
                      dma("sp", pool_p[:, g * 128:(g + 1) * 128].rearrange("r f -> f r"), UB[:, g, 16 + TW - 15:16 + TW],
                          ["UB"], [], "pool_p", allow_slow_non_contiguous=True)
                  if do_s:
                      out_tok16([(NU[:, g, :], "NU") for g in range(4)], pool_s[:, 14, :], "ps")
              else:
                  dve_copy(UB[:, :, 0:16], UB[:, :, TW:TW + 16], ["UB"], ["UB"])
              chk(32)
              wt, wtok = wget("pool")
              wv = wt[:, 0:512].rearrange("p (g m) -> p g m", g=4)
              for g in range(4):
                  for (c0, n) in sts:
                      bank, btok = nb()
                      mmgroup(bank, btok, 128, n, [(wv[:, g, :], PL[:, g, c0:c0 + n])], [wtok, "PL"])
                      dve("tensor_scalar", PY[:, g, c0:c0 + n], bank[:, 0:n], C("pscale")[:, g:g + 1], None, ALU.mult, reads=[btok, "CST"], writes=["PY"])

              chk(4)
              rmsnorm(lambda c, c0, n: CQ[:, c, c0:c0 + n], "CQ", 2, 256, lambda c: C("qnorm")[:, c:c + 1],
                      lambda c, c0, n: CQN[:, c, c0:c0 + n], "CQN", sts)
              wq, wqtok = wget("uq")
              wqv = wq[:, 0:1536].rearrange("p (k m) -> p k m", k=2)
              wk, wktok = wget("uk")
              wkv = wk[:, 0:1024].rearrange("p (h c) -> p h c", h=8)
              r32t = C("r32t")[0:32, :]
              for h in range(8):
                  for (c0, n) in sts:
                      qn, qntok = QN[h % 2], "QN%d" % (h % 2)
                      bank, btok = nb()
                      mmgroup(bank, btok, 64, n, [(wqv[:, k, h * 64:(h + 1) * 64], CQN[:, k, c0:c0 + n]) for k in range(2)], [wqtok, "CQN"])
                      act_copy(qn[:, c0:c0 + n], bank[0:64, 0:n], [btok], [qntok])
                      bank, btok = nb()
                      mmgroup(bank, btok, 128, n, [(wkv[0:64, h, :], qn[:, c0:c0 + n])], [wktok, qntok])
                      act_copy(QL[:, h, c0:c0 + n], bank[:, 0:n], [btok], ["QL"], scale=SCALE)
                      bank, btok = nb()
                      mmgroup(bank, btok, 32, n, [(wqv[:, k, 512 + h * 32:512 + (h + 1) * 32], CQN[:, k, c0:c0 + n]) for k in range(2)],
                              [wqtok, "CQN"])
                      dve_copy(QRR[:, c0:c0 + n], bank[0:32, 0:n], [btok], ["QRR"])
                      bank, btok = nb()
                      mmgroup(bank, btok, 32, n, [(r32t, QRR[:, c0:c0 + n])], ["CST", "QRR"])
                      dve("scalar_tensor_tensor", T1[:, 0:n], QRR[:, c0:c0 + n], SCALE, ROPE[:, 0, c0:c0 + n], ALU.mult, ALU.mult,
                          reads=["QRR", "ROPE"], writes=["T1"])
                      dve("scalar_tensor_tensor", T2[:, 0:n], bank[0:32, 0:n], SCALE, ROPE[:, 1, c0:c0 + n], ALU.mult, ALU.mult,
                          reads=[btok, "ROPE"], writes=["T2"])
                      dve("tensor_tensor", QR[:, h, c0:c0 + n], T1[:, 0:n], T2[:, 0:n], ALU.add, reads=["T1", "T2"], writes=["QR"])

              chk(5)
              rmsnorm(lambda c, c0, n: CKV[:, c0:c0 + n], "CKV", 1, 128, lambda c: C("kvnorm")[:, 0:1],
                      lambda c, c0, n: RL[:, c0:c0 + n], "RL", sts)
              for (c0, n) in sts:
                  bank, btok = nb()
                  mmgroup(bank, btok, 32, n, [(r32t, KR[:, c0:c0 + n])], ["CST", "KR"])
                  dve("tensor_tensor", T1[:, 0:n], KR[:, c0:c0 + n], ROPE[:, 0, c0:c0 + n], ALU.mult, reads=["KR", "ROPE"], writes=["T1"])
                  dve("tensor_tensor", T2[:, 0:n], bank[0:32, 0:n], ROPE[:, 1, c0:c0 + n], ALU.mult, reads=[btok, "ROPE"], writes=["T2"])
                  dve("tensor_tensor", KRO[:, c0:c0 + n], T1[:, 0:n], T2[:, 0:n], ALU.add, reads=["T1", "T2"], writes=["KRO"])
              chk(51)
              act_copy(KLT[:, g0:g0 + TW], RL[:, 0:TW], ["RL"], ["KLT"])
              act_copy(KRT[:, g0:g0 + TW], KRO[:, 0:TW], ["KRO"], ["KRT"])
              chk(52)
              for b in range(4):
                  bank, btok = nb()
                  tr(bank[:, 0:128], RL[:, b * 128:(b + 1) * 128], identf, ["RL", "CST"], [btok])
                  tr(bank[:, 128:160], KRO[:, b * 128:(b + 1) * 128], identf[0:32, 0:32], ["KRO", "CST"], [btok])
                  dve_copy(ROWST[:, b, :], bank[:, 0:ROWW], [btok], ["ROWST"])
                  act_copy(VT[:, t * 4 + b, :], ROWST[:, b, 0:128], ["ROWST"], ["VT"])
              chk(53)
              dma("sp", rows_p[g0:g0 + TW, :].rearrange("(b p) c -> p b c", p=128), ROWST, ["ROWST"], [], "rows_p")
              if do_s and t == last_tile:
                  bank, btok = nb()
                  tr(bank[0:NSAMP, 0:128], RL[:, TW:W], identf, ["RL", "CST"], [btok])
                  tr(bank[0:NSAMP, 128:160], KRO[:, TW:W], identf[0:32, 0:32], ["KRO", "CST"], [btok])
                  dve_copy(ROWS_S[:], bank[0:NSAMP, 0:ROWW], [btok], ["ROWS_S"])
                  dve_copy(RLB[:], RL[:, TW:W], ["RL"], ["RLB"])
                  dve_copy(KROB[:], KRO[:, TW:W], ["KRO"], ["KROB"])
                  dma("sp", rows_s, ROWS_S[:], ["ROWS_S"], [], "rows_s")

              chk(6)
              o2 = A_F
              S_ = [P.carve("S%d" % i, o2 + i * SEQ * 4, [SEQ], F32) for i in range(2)]
              o2 += 2 * SEQ * 4
              PM = [P.carve("PM%d" % i, o2 + i * 2048, [1024], BF16) for i in range(2)]
              o2 += 4096
              PTS = [P.carve("PTS%d" % i, o2 + i * 2048, [8, 128], BF16) for i in range(2)]
              o2 += 4096
              OLT = [P.carve("OLT%d" % i, o2 + i * 256, [128], BF16) for i in range(2)]
              o2 += 512
              OTOK = P.carve("OTOK", o2, [512], BF16)
              o2 += 1024
              SM = [P.carve("SM%d" % i, o2 + i * 64, [16], F32) for i in range(2)]
              o2 += 128
              wuv, wuvtok = wget("uv")
              segc = 0
              items = [(qb, h) for qb in range(4) for h in range(8)]
              segc_box = [0]

              def att_a(idx):
                  qb, h = items[idx]
                  nk = (t * 4 + qb + 1) * 128
                  qc0 = qb * 128
                  par = idx % 2
                  S, stok = S_[par], "S%d" % par
                  sm, smtok = SM[par], "SM%d" % par
                  nchk = (nk + 511) // 512
                  for ck in range(nchk):
                      k0 = ck * 512
                      kw = min(512, nk - k0)
                      last = ck == nchk - 1
                      bank, btok = nb()
                      mm(bank[:, 0:kw], QL[:, h, qc0:qc0 + 128], KLT[:, k0:k0 + kw], True, False, ["QL", "KLT"], [btok])
                      mm(bank[:, 0:kw], QR[:, h, qc0:qc0 + 128], KRT[:, k0:k0 + kw], False, not last, ["QR", "KRT"], [btok])
                      if last:
                          mm(bank[:, kw - 128:kw], identb[:], maskb[:], False, True, ["identb", "maskb"], [btok])
                      dve("tensor_scalar", S[:, k0:k0 + kw], bank[:, 0:kw], 1.0, None, ALU.mult, ALU.max,
                          accum_out=sm[:, ck:ck + 1], reads=[btok], writes=[stok, smtok])
                  dve("tensor_reduce", sm[:, 8:9], sm[:, 0:nchk], AX.X, ALU.max, negate=True, reads=[smtok], writes=[smtok])

              def att_b(idx):
                  qb, h = items[idx]
                  nk = (t * 4 + qb + 1) * 128
                  qc0 = qb * 128
                  par = idx % 2
                  S, stok = S_[par], "S%d" % par
                  sm, smtok = SM[par], "SM%d" % par
                  nseg = (nk + 1023) // 1024
                  bo, botok = nb()
                  nkb = nk // 128
                  for sg in range(nseg):
                      s0 = sg * 1024
                      sw = min(1024, nk - s0)
                      sp_ = segc_box[0] % 2
                      segc_box[0] += 1
                      pm, pmtok = PM[sp_], "PM%d" % sp_
                      pts, ptstok = PTS[sp_], "PTS%d" % sp_
                      act(pm[:, 0:sw], S[:, s0:s0 + sw], AF.Exp, [stok, smtok], [pmtok, smtok],
                          bias=sm[:, 8:9], accum_out=sm[:, 9 + sg:10 + sg])
                      bt_, bttok = nb()
                      btv = bfview(bt_)
                      nbl = sw // 128
                      for j in range(nbl):
                          tr(btv[:, j * 128:(j + 1) * 128], pm[:, j * 128:(j + 1) * 128], identb[:], [pmtok, "identb"], [bttok])
                      ptsf = pts.rearrange("p a b -> p (a b)")
                      if sp_ == 0:
                          act_copy(ptsf[:, 0:sw], btv[:, 0:sw], [bttok], [ptstok])
                      else:
                          dve_copy(ptsf[:, 0:sw], btv[:, 0:sw], [bttok], [ptstok])
                      for j in range(nbl):
                          kb = sg * 8 + j
                          mm(bo[:, 0:128], VT[:, kb, :], pts[:, j, :], kb == 0, kb == nkb - 1, ["VT", ptstok], [botok])
                  olt, olttok = OLT[par], "OLT%d" % par
                  act_copy(olt, bo[:, 0:128], [botok], [olttok])
                  bv, bvtok = nb()
                  mm(bv[:, 0:64], olt, wuv[:, h * 64:(h + 1) * 64], True, True, [olttok, wuvtok], [bvtok])
                  dve("tensor_reduce", sm[:, 13:14], sm[:, 9:9 + nseg], AX.X, ALU.add, reads=[smtok], writes=[smtok])
                  dve("reciprocal", sm[:, 14:15], sm[:, 13:14], reads=[smtok], writes=[smtok])
                  dve("tensor_scalar", OTOK[:, h * 64:(h + 1) * 64], bv[:, 0:64], sm[:, 14:15], None, ALU.mult,
                      reads=[bvtok, smtok], writes=["OTOK"])
                  if h == 7:
                      bt_, bttok = nb()
                      btv = bfview(bt_)
                      for j in range(4):
                          tr(btv[:, j * 128:(j + 1) * 128], OTOK[:, j * 128:(j + 1) * 128], identb[:], ["OTOK", "identb"], [bttok])
                      act_copy(OT[:, :, qc0:qc0 + 128], btv[:, 0:512].rearrange("p (j n) -> p j n", j=4), [bttok], ["OT"])

              att_a(0)
              for idx in range(len(items)):
                  if idx + 1 < len(items):
                      att_a(idx + 1)
                  att_b(idx)

              if do_s and t == last_tile:
                  decode_attention(QL, QR, RL, KRO, OT, wuv, wuvtok)

              chk(7)
              for j in range(2):
                  wt, wtok = wget("out0%d" % j)
                  wv = wt[:, 0:4096].rearrange("p (k m) -> p k m", k=8)
                  for mi in range(4):
                      m = j * 4 + mi
                      for (c0, n) in sts:
                          bank, btok = nb()
                          pairs = [(wv[:, k, mi * 128:(mi + 1) * 128], PY[:, k, c0:c0 + n]) for k in range(4)]
                          pairs += [(wv[:, 4 + k, mi * 128:(mi + 1) * 128], OT[:, k, c0:c0 + n]) for k in range(4)]
                          mmgroup(bank, btok, 128, n, pairs, [wtok, "PY", "OT"])
                          dve("tensor_tensor", X[:, m, c0:c0 + n], X[:, m, c0:c0 + n], bank[:, 0:n], ALU.add,
                              reads=["X", btok], writes=["X"])

              chk(8)
              rmsnorm(Xc, "X", 8, D, lambda c: C("nffn")[:, c:c + 1], XNc, "XN", sts)
              for pe_ in range(2):
                  swiglu_expert(sts, "fg%d" % pe_, "fu%d" % pe_, "fd%d" % pe_, None, None)
              chk(9)
              ple(sts, 0)
              chk(10)
              rmsnorm(Xc, "X", 8, D, lambda c: C("nmix")[:, 8 + c:9 + c], XNc, "XN", sts)
              rglru(t, sts)
              chk(11)
              rmsnorm(Xc, "X", 8, D, lambda c: C("nffn")[:, 8 + c:9 + c], XNc, "XN", sts)
              chk(12)
              CALL = router(sts)
              chk(13)
              for e_ in range(NEXP):
                  swiglu_expert(sts, "eg%d" % e_, "eu%d" % e_, "ed%d" % e_, e_, CALL)
              chk(14)
              ple(sts, 1)
              chk(15)
              rmsnorm(Xc, "X", 8, D, lambda c: C("nfin")[:, c:c + 1], Xc, "X", sts)
              YS = [P.carve("YS%d" % i, i * 4096, [D], F32) for i in range(2)]
              for b in range(4):
                  ys, ystok = YS[b % 2], "YS%d" % (b % 2)
                  for hf in range(2):
                      bank, btok = nb()
                      for ci in range(4):
                          c = hf * 4 + ci
                          tr(bank[:, ci * 128:(ci + 1) * 128], X[:, c, b * 128:(b + 1) * 128], identf, ["X", "CST"], [btok])
                      if hf == 0:
                          act_copy(ys[:, 0:512], bank[:, 0:512], [btok], [ystok])
                      else:
                          dve_copy(ys[:, 512:1024], bank[:, 0:512], [btok], [ystok])
                  dma("sp", y_p[g0 + b * 128:g0 + (b + 1) * 128, :], ys, [ystok], [], ystok)
              if do_s and t == last_tile:
                  out_tok16([(X[:, c, TW:W], "X") for c in range(8)], y_s, "ys")

        except _Stop:
            pass
        P.emit()
    return nc, P


_CACHE = {}


def _prep_common(inp):
    units, woffs, wtot = weight_units()
    wflat = np.empty((128, wtot), np.float32)
    for name, nel, fn in units:
        off, _ = woffs[name]
        wflat[:, off:off + nel] = fn(inp)
    return wflat, build_cst(inp), build_rope()


def kernel(**inp):
    inp = {k: np.asarray(v) for k, v in inp.items()}
    n_tiles = int(inp.pop("_n_tiles", NT))
    samples = bool(inp.pop("_samples", True))
    key = (n_tiles, samples)
    if key not in _CACHE:
        _CACHE[key] = build_program(n_tiles, samples)
    nc, P = _CACHE[key]
    wflat, cstv, ropev = _prep_common(inp)
    in_maps = []
    for c in range(NCORE):
        m = {
            "xp": np.ascontiguousarray(inp["x_prompt"][c]),
            "pp": np.ascontiguousarray(inp["p_prompt"][:, c]),
            "wflat": wflat, "cst": cstv, "rope": ropev,
        }
        if samples:
            sl = slice(c * NSAMP, (c + 1) * NSAMP)
            m.update({
                "xs": np.ascontiguousarray(inp["x_sample"][sl, 0]),
                "psm": np.ascontiguousarray(inp["p_sample"][:, sl, 0]),
                "cache": inp["cache_mla"][0].reshape(NPOOLPG * NGCH, GCH * ROWW),
                "spool": np.ascontiguousarray(inp["state_pool"][0, sl]),
                "sconv": np.ascontiguousarray(inp["state_conv"][0, sl]),
                "slru": np.ascontiguousarray(inp["state_lru"][0, sl]),
                "ptab": np.ascontiguousarray(inp["page_table"][sl]).astype(np.int32),
            })
        in_maps.append(m)
    res = run_bass_kernel_spmd(nc, in_maps, core_ids=list(range(NCORE)))
    r = res.results
    f32 = np.float32
    y_prompt = np.stack([r[c]["y_p"] for c in range(NCORE)]).astype(f32)
    rows_prompt = np.stack([r[c]["rows_p"] for c in range(NCORE)])[None].astype(f32)
    pool_prompt = np.stack([r[c]["pool_p"] for c in range(NCORE)])[None].astype(f32)
    conv_prompt = np.stack([r[c]["conv_p"] for c in range(NCORE)])[None].astype(f32)
    h_prompt = np.stack([r[c]["h_p"][0] for c in range(NCORE)])[None].astype(f32)
    if samples:
        y_sample = np.concatenate([r[c]["y_s"] for c in range(NCORE)])[:, None, :].astype(f32)
        rows_sample = np.concatenate([r[c]["rows_s"] for c in range(NCORE)])[None, :, None, :].astype(f32)
        pool_sample = np.concatenate([r[c]["pool_s"] for c in range(NCORE)])[None].astype(f32)
        conv_sample = np.concatenate([r[c]["conv_s"] for c in range(NCORE)])[None].astype(f32)
        h_sample = np.concatenate([r[c]["h_s"] for c in range(NCORE)])[None].astype(f32)
    else:
        y_sample = np.zeros((128, 1, D), f32)
        rows_sample = np.zeros((1, 128, 1, ROWW), f32)
        pool_sample = np.zeros((1, 128, 15, 512), f32)
        conv_sample = np.zeros((1, 128, 3, D), f32)
        h_sample = np.zeros((1, 128, D), f32)
    return (y_prompt, y_sample, rows_prompt, rows_sample, pool_prompt, pool_sample,
            conv_prompt, conv_sample, h_prompt, h_sample)
```

```python
import contextlib
import math
import numpy as np
import concourse.bass as bass
import concourse.mybir as mybir
from concourse.bass_utils import run_bass_kernel_spmd

F32 = mybir.dt.float32
BF16 = mybir.dt.bfloat16
I32 = mybir.dt.int32
AF = mybir.ActivationFunctionType
ALU = mybir.AluOpType
AX = mybir.AxisListType

D = 1024
SEQ = 4096
NCORE = 8
NSAMP = 16
TW = 512
W = TW + NSAMP
NT = SEQ // TW
PAST_PAGES = 128
PAGE = 128
ROWW = 160
NPOOLPG = 20480
SCALE = float((64 + 32) ** -0.5)
EPS = 1e-6
DEXP = 1408
NEXP = 8
GCH = 16
NGCH = PAGE // GCH
SLOT = 4096
NSLOT = 4
ENGS = ("pe", "act", "dve", "pool", "sp")


class Op:
    __slots__ = ("idx", "eng", "fn", "dma", "key", "deps", "inc", "cnt")

    def __init__(self, idx, eng, fn, dma, key):
        self.idx = idx
        self.eng = eng
        self.fn = fn
        self.dma = dma
        self.key = key
        self.deps = {}
        self.inc = False
        self.cnt = 0


class Prog:
    def __init__(self, nc):
        self.nc = nc
        self.ops = []
        self.lastw = {}
        self.readers = {}
        self.stack = contextlib.ExitStack()
        self.nbank = 0
        self.live = []
        self.scr = None

    def sb(self, name, shape, dtype):
        return self.stack.enter_context(self.nc.sbuf_tensor(name, list(shape), dtype))

    def ps(self, name, shape, dtype):
        return self.stack.enter_context(self.nc.psum_tensor(name, list(shape), dtype))

    def alias(self, new, olds):
        lst = self.readers.setdefault(new, [])
        for o in olds:
            lst.extend(self.readers.get(o, []))
            if o in self.lastw:
                lst.append(self.lastw[o])

    def carve(self, tok, off, shape, dtype, p0=0, p1=128):
        esz = 4 if dtype in (F32, I32) else 2
        nel = 1
        for s in shape:
            nel *= s
        nbytes = nel * esz
        assert off % 4 == 0 and nbytes % 4 == 0, (tok, off, nbytes)
        assert off + nbytes <= self.scr_bytes, (tok, off, nbytes, self.scr_bytes)
        ap = self.scr[p0:p1, off // 4:(off + nbytes) // 4]
        if dtype != F32:
            ap = ap.bitcast(dtype)
        if len(shape) == 2:
            ap = ap.rearrange("p (a b) -> p a b", a=shape[0])
        elif len(shape) == 3:
            ap = ap.rearrange("p (a b c) -> p a b c", a=shape[0], b=shape[1])
        lo, hi = off, off + nbytes
        same = [e for e in self.live if e[2] == tok]
        if same and same[0][0] == lo and same[0][1] == hi:
            return ap
        olds = [e for e in self.live if e[0] < hi and lo < e[1]]
        if olds:
            self.alias(tok, [e[2] for e in olds if e[2] != tok])
            keep = [e for e in self.live if not (e[0] < hi and lo < e[1])]
            for (lo_o, hi_o, tok_o) in olds:
                if tok_o == tok:
                    continue
                if lo_o < lo:
                    keep.append((lo_o, lo, tok_o))
                if hi_o > hi:
                    keep.append((hi, hi_o, tok_o))
            self.live = keep
        self.live.append((lo, hi, tok))
        return ap

    def add(self, eng, meth, *args, reads=(), writes=(), dma=False, key=None, **kw):
        op = Op(len(self.ops), eng, (meth, args, kw), dma, key)
        for t in reads:
            w = self.lastw.get(t)
            if w is not None:
                op.deps[w] = True
            if t.startswith("bank"):
                for r in self.readers.get(t, ()):
                    if self.ops[r].eng != eng:
                        op.deps[r] = True
                        self.rar = getattr(self, "rar", 0) + 1
        for t in writes:
            w = self.lastw.get(t)
            if w is not None and w not in op.deps:
                op.deps[w] = False
            for r in self.readers.get(t, ()):
                if r not in op.deps:
                    op.deps[r] = False
        for t in reads:
            lst = self.readers.setdefault(t, [])
            if not dma:
                lst[:] = [r for r in lst if self.ops[r].dma or self.ops[r].eng != eng]
            lst.append(op.idx)
        for t in writes:
            self.lastw[t] = op.idx
            self.readers[t] = []
        self.ops.append(op)
        return op

    def emit(self):
        nc = self.nc
        ops = self.ops
        for op in ops:
            for d, raw in op.deps.items():
                dop = ops[d]
                if dop.dma:
                    continue
                if dop.eng == op.eng and not op.dma:
                    if dop.eng == "pe":
                        continue
                dop.inc = True
        esem = {e: self.stack.enter_context(nc.semaphore("sem_" + e)) for e in ENGS}
        ecnt = {e: 0 for e in ENGS}
        ksem = {}
        kcnt = {}
        for op in ops:
            if op.dma:
                k = op.key
                if k not in ksem:
                    ksem[k] = self.stack.enter_context(nc.semaphore("dk%d" % len(ksem)))
                    kcnt[k] = 0
                kcnt[k] += 16
                op.cnt = kcnt[k]
            elif op.inc:
                ecnt[op.eng] += 1
                op.cnt = ecnt[op.eng]
        self.stats = dict(nops=len(ops), nsem=len(ksem) + len(ENGS), ecnt=dict(ecnt))
        per_eng = {e: [op for op in ops if op.eng == e] for e in ENGS}

        def run(eng_name, eng):
            waited = {}
            for op in per_eng[eng_name]:
                for d, raw in op.deps.items():
                    dop = ops[d]
                    if dop.dma:
                        s = ksem[dop.key]
                    else:
                        if dop.eng == op.eng and not op.dma:
                            if dop.eng == "pe":
                                continue
                        s = esem[dop.eng]
                    v = dop.cnt
                    if waited.get(s.name, 0) >= v:
                        continue
                    eng.wait_ge(s, v)
                    waited[s.name] = v
                meth, args, kw = op.fn
                ins = getattr(eng, meth)(*args, **kw)
                if op.dma:
                    ins.then_inc(ksem[op.key], 16)
                elif op.inc:
                    ins.then_inc(esem[op.eng], 1)
            if eng_name == "sp":
                for k, v in kcnt.items():
                    if waited.get(ksem[k].name, 0) >= v:
                        continue
                    eng.wait_ge(ksem[k], v)

        with nc.Block() as block:
            @block.tensor
            def _(e):
                run("pe", e)

            @block.scalar
            def _(e):
                run("act", e)

            @block.vector
            def _(e):
                run("dve", e)

            @block.gpsimd
            def _(e):
                run("pool", e)

            @block.sync
            def _(e):
                run("sp", e)


def _blk(Wm, k0, nk, m0, mw):
    a = Wm[k0 * 128:(k0 + nk) * 128, m0:m0 + mw]
    return np.ascontiguousarray(a.reshape(nk, 128, mw).transpose(1, 0, 2)).reshape(128, nk * mw)


def _split3(i):
    return i * 512, min(512, DEXP - i * 512)


def weight_units(inp=None):
    u = []

    def add(name, nel, fn):
        u.append((name, nel, fn))

    add("in0a", 8 * 512, lambda i: _blk(i["w_in0"][0], 0, 8, 0, 512))
    add("in0b", 8 * 416, lambda i: _blk(i["w_in0"][0], 0, 8, 512, 416))
    add("pool", 512, lambda i: np.ascontiguousarray(i["pool_w"][0].transpose(1, 0, 2)).reshape(128, 512))

    def uq(i):
        w = i["w_uq"][0].reshape(256, 8, 96)
        w2 = np.concatenate([w[:, :, :64].reshape(256, 512), w[:, :, 64:].reshape(256, 256)], axis=1)
        return _blk(w2, 0, 2, 0, 768)
    add("uq", 2 * 768, uq)

    def uk(i):
        w = np.ascontiguousarray(i["w_uk"][0].transpose(2, 1, 0)).reshape(64, 1024)
        return np.concatenate([w, np.zeros((64, 1024), np.float32)], axis=0)
    add("uk", 1024, uk)
    add("uv", 512, lambda i: np.ascontiguousarray(i["w_uv"][0].reshape(128, 512)))
    for j in range(2):
        add("out0%d" % j, 4096, lambda i, j=j: _blk(i["w_out0"][0], 0, 8, j * 512, 512))
    for pe in range(2):
        for j in range(3):
            m0, mw = _split3(j)
            add("fg%d%d" % (pe, j), 8 * mw, lambda i, pe=pe, m0=m0, mw=mw: _blk(i["w_ffn_gate"][0], 0, 8, pe * DEXP + m0, mw))
            add("fu%d%d" % (pe, j), 8 * mw, lambda i, pe=pe, m0=m0, mw=mw: _blk(i["w_ffn_up"][0], 0, 8, pe * DEXP + m0, mw))
        for j in range(4):
            add("fd%d%d" % (pe, j), 11 * 256, lambda i, pe=pe, j=j: _blk(i["w_ffn_down"][0], pe * 11, 11, j * 256, 256))
    for l in range(2):
        for j in range(2):
            add("pg%d%d" % (l, j), 4096, lambda i, l=l, j=j: _blk(i["w_ple_gate"][l], 0, 8, j * 512, 512))
        add("pp%d" % l, 2048, lambda i, l=l: _blk(i["w_ple_proj"][l], 0, 2, 0, 1024))
    for c in range(8):
        def in1(i, c=c):
            w = i["w_in1"][0]
            w2 = np.concatenate([w[:, c * 128:(c + 1) * 128], w[:, 1024 + c * 128:1024 + (c + 1) * 128]], axis=1)
            return _blk(w2, 0, 8, 0, 256)
        add("in1%d" % c, 2048, in1)

    def rgig(i):
        a = i["w_rg"][0].transpose(1, 0, 2)
        b = i["w_ig"][0].transpose(1, 0, 2)
        return np.ascontiguousarray(np.stack([a, b], axis=2)).reshape(128, 2048)
    add("rgig", 2048, rgig)
    for j in range(2):
        add("out1%d" % j, 4096, lambda i, j=j: _blk(i["w_out1"][0], 0, 8, j * 512, 512))
    for e in range(NEXP):
        for j in range(3):
            m0, mw = _split3(j)
            add("eg%d%d" % (e, j), 8 * mw, lambda i, e=e, m0=m0, mw=mw: _blk(i["w_exp_gate"][0, e], 0, 8, m0, mw))
            add("eu%d%d" % (e, j), 8 * mw, lambda i, e=e, m0=m0, mw=mw: _blk(i["w_exp_up"][0, e], 0, 8, m0, mw))
        for j in range(4):
            add("ed%d%d" % (e, j), 11 * 256, lambda i, e=e, j=j: _blk(i["w_exp_down"][0, e], 0, 11, j * 256, 256))
    offs = {}
    off = 0
    for name, nel, fn in u:
        offs[name] = (off, nel)
        off += nel
    return u, offs, off


def cst_layout():
    names = [("identf", 128), ("mask", 128), ("nmix", 16), ("nffn", 16), ("nple", 16), ("nfin", 8),
             ("pscale", 4), ("qnorm", 2), ("kvnorm", 1), ("convw", 32), ("convb", 8), ("brg", 8), ("big", 8),
             ("lam", 8), ("wr", 64), ("r32t", 32), ("invc", 64)]
    offs = {}
    off = 0
    for n, c in names:
        offs[n] = (off, c)
        off += c
    return offs, off


def _fm(v):
    return np.ascontiguousarray(np.asarray(v, np.float32).reshape(-1, 128).T)


def build_cst(inp):
    offs, tot = cst_layout()
    c = np.zeros((128, tot), np.float32)

    def put(name, arr):
        o, n = offs[name]
        arr = np.asarray(arr, np.float32)
        c[:arr.shape[0], o:o + arr.shape[1]] = arr
    put("identf", np.eye(128, dtype=np.float32))
    q = np.arange(128)
    put("mask", np.where(q[None, :] <= q[:, None], 0.0, -30000.0).astype(np.float32))
    put("nmix", np.concatenate([_fm(inp["norm_mix"][0]), _fm(inp["norm_mix"][1])], axis=1))
    put("nffn", np.concatenate([_fm(inp["norm_ffn"][0]), _fm(inp["norm_ffn"][1])], axis=1))
    put("nple", np.concatenate([_fm(inp["norm_ple"][0]), _fm(inp["norm_ple"][1])], axis=1))
    put("nfin", _fm(inp["norm_final"]))
    put("pscale", _fm(inp["pool_scale"][0]))
    put("qnorm", _fm(inp["q_norm"][0]))
    put("kvnorm", _fm(inp["kv_norm"][0]))
    put("convw", np.concatenate([_fm(inp["conv_w"][0][k]) for k in range(4)], axis=1))
    put("convb", _fm(inp["conv_b"][0]))
    put("brg", _fm(inp["b_rg"][0]))
    put("big", _fm(inp["b_ig"][0]))
    put("lam", _fm(inp["lru_lambda"][0]))
    put("wr", np.ascontiguousarray(inp["w_router"][0].reshape(8, 128, 8).transpose(1, 0, 2)).reshape(128, 64))
    R = np.zeros((32, 32), np.float32)
    for i in range(16):
        R[i, 16 + i] = -1.0
        R[16 + i, i] = 1.0
    put("r32t", R.T)
    invc = np.zeros((128, 64), np.float32)
    for g, w in enumerate((2, 4, 8, 16)):
        for t in range(16):
            invc[:, g * 16 + t] = 1.0 / min(t + 1, w)
    put("invc", invc)
    return c


def build_rope():
    inv = (np.float32(10000.0) ** (-np.arange(0, 32, 2, dtype=np.float32) / np.float32(32))).astype(np.float32)
    pos = np.concatenate([np.arange(SEQ, dtype=np.float32), np.full(NSAMP, PAST_PAGES * PAGE, np.float32)])
    ang = (pos[:, None] * inv[None, :]).astype(np.float32)
    cos = np.cos(ang).astype(np.float32).T
    sin = np.sin(ang).astype(np.float32).T
    r = np.zeros((32, 2, SEQ + NSAMP), np.float32)
    r[0:16, 0] = cos
    r[16:32, 0] = cos
    r[0:16, 1] = sin
    r[16:32, 1] = sin
    return r


class _Stop(Exception):
    pass


def build_program(n_tiles=NT, samples=True, stop=0):
    def chk(stage):
        if stage == stop:
            raise _Stop()

    nc = bass.Bass("TRN2", target_bir_lowering=False)
    units, woffs, wtot = weight_units()
    coffs, ctot = cst_layout()
    last_tile = n_tiles - 1
    do_s = samples

    def din(name, shape, dt=F32):
        return nc.dram_tensor(name, list(shape), dt, kind="ExternalInput").ap()

    def dout(name, shape, dt=F32):
        return nc.dram_tensor(name, list(shape), dt, kind="ExternalOutput").ap()

    xp = din("xp", [SEQ, D])
    pp = din("pp", [2, SEQ, 256])
    wflat = din("wflat", [128, wtot])
    cst = din("cst", [128, ctot])
    rope = din("rope", [32, 2, SEQ + NSAMP])
    y_p = dout("y_p", [SEQ, D])
    rows_p = dout("rows_p", [SEQ, ROWW])
    pool_p = dout("pool_p", [15, 512])
    conv_p = dout("conv_p", [3, D])
    h_p = dout("h_p", [1, D])
    if do_s:
        xs = din("xs", [NSAMP, D])
        psm = din("psm", [2, NSAMP, 256])
        cache = din("cache", [NPOOLPG * NGCH, GCH * ROWW])
        spool = din("spool", [NSAMP, 15, 512])
        sconv = din("sconv", [NSAMP, 3, D])
        slru = din("slru", [NSAMP, D])
        ptab = din("ptab", [NSAMP, PAST_PAGES], I32)
        y_s = dout("y_s", [NSAMP, D])
        rows_s = dout("rows_s", [NSAMP, ROWW])
        pool_s = dout("pool_s", [NSAMP, 15, 512])
        conv_s = dout("conv_s", [NSAMP, 3, D])
        h_s = dout("h_s", [NSAMP, D])

    P = Prog(nc)
    with P.stack:
        X = P.sb("X", [128, 8, W], F32)
        XN = P.sb("XN", [128, 8, W], BF16)
        KLT = P.sb("KLT", [128, SEQ], BF16)
        KRT = P.sb("KRT", [32, SEQ], BF16)
        VT = P.sb("VT", [128, SEQ // 128, 128], BF16)
        UB = P.sb("UB", [128, 4, 16 + W], F32)
        CH = P.sb("CH", [128, 8, 4], F32)
        HS = P.sb("HS", [128, 8], F32)
        PT2 = P.sb("PT2", [128, 2, 2, W], BF16)
        CST = P.sb("CST", [128, ctot], F32)
        ROPE = P.sb("ROPE", [32, 2, W], F32)
        identb = P.sb("identb", [128, 128], BF16)
        maskb = P.sb("maskb", [128, 128], BF16)
        onesb = P.sb("onesb", [128, 128], BF16)
        onesf = P.sb("onesf", [128, 128], F32)
        GWR = P.sb("GWR", [128, 8, 8], F32)
        NSP = P.sb("NSP", [128, 16], F32)
        SQ = [P.sb("SQ%d" % i, [128, 512], BF16) for i in range(2)]
        NT1 = P.sb("NT1", [128, 512], F32)
        RSTD = P.sb("RSTD", [128, W], F32)
        slots = [P.sb("wslot%d" % i, [128, SLOT], BF16) for i in range(NSLOT)]
        SCR_BYTES = 88 * 1024
        P.scr = P.sb("SCR", [128, SCR_BYTES // 4], F32)
        P.scr_bytes = SCR_BYTES
        banks = [P.ps("bank%d" % i, [128, 512], F32) for i in range(8)]
        if do_s:
            IDX = P.sb("IDX", [128, NSAMP, NGCH], I32)
            PTT = P.sb("PTT", [128, NSAMP], I32)
            EXT = P.sb("EXT", [128, 4, NSAMP, 16], F32)
            CEX = P.sb("CEX", [128, 8, NSAMP, 4], F32)
            H0S = P.sb("H0S", [128, 8, NSAMP], F32)
            NXB = P.sb("NXB", [128, 8, NSAMP], F32)
            NU = P.sb("NU", [128, 4, NSAMP], F32)
            SEL = P.sb("SEL", [16, NSAMP, 8], F32)
            ROWS_S = P.sb("ROWS_S", [16, ROWW], F32)
            RLB = P.sb("RLB", [128, NSAMP], BF16)
            KROB = P.sb("KROB", [32, NSAMP], BF16)

        def C(name):
            o, c = coffs[name]
            return CST[:, o:o + c]

        identf = C("identf")

        P.rr = list(range(8))

        def nb():
            i = P.rr[P.nbank % len(P.rr)]
            P.nbank += 1
            return banks[i], "bank%d" % i

        def bfview(bank):
            return bank[:, 0:512].bitcast(BF16)

        def mm(out, lhsT, rhs, start, stop, reads, writes):
            P.add("pe", "matmul", out, lhsT, rhs, start=start, stop=stop, reads=reads, writes=writes)

        def tr(out, in_, ident, reads, writes):
            P.add("pe", "transpose", out, in_, ident, reads=reads, writes=writes)

        def act(out, in_, func, reads, writes, **kw):
            P.add("act", "activation", out, in_, func, reads=reads, writes=writes, **kw)

        def dve(meth, *args, reads, writes, **kw):
            P.add("dve", meth, *args, reads=reads, writes=writes, **kw)

        def dma(eng, out, in_, reads, writes, key, **kw):
            P.add(eng, "dma_start", out=out, in_=in_, reads=reads, writes=writes, dma=True, key=key, **kw)

        def act_copy(out, in_, reads, writes, scale=None):
            if scale is None:
                act(out, in_, AF.Copy, reads, writes)
            else:
                act(out, in_, AF.Copy, reads, writes, scale=scale)

        def dve_copy(out, in_, reads, writes):
            dve("tensor_copy", out, in_, reads=reads, writes=writes)

        def mmgroup(bank, btok, msz, n, pairs, reads):
            k = len(pairs)
            for i, (l, r) in enumerate(pairs):
                mm(bank[0:msz, 0:n], l, r, i == 0, i == k - 1, reads, [btok])

        wstate = {"u": 0}

        def wget(name):
            off, nel = woffs[name]
            s = wstate["u"] % NSLOT
            wstate["u"] += 1
            tok = "wslot%d" % s
            dma("pool", slots[s][:, 0:nel], wflat[:, off:off + nel], [], [tok], tok)
            return slots[s], tok

        def rmsnorm(src, src_tok, nch, dfeat, gain, dst, dst_tok, sts):
            for (c0, n) in sts:
                bank, btok = nb()
                for c in range(nch):
                    sq, sqt = SQ[c % 2], "SQ%d" % (c % 2)
                    act(sq[:, 0:n], src(c, c0, n), AF.Square, [src_tok], [sqt])
                    mm(bank[:, 0:n], onesb[:], sq[:, 0:n], c == 0, c == nch - 1, [sqt, "onesb"], [btok])
                dve("tensor_scalar", NT1[:, 0:n], bank[:, 0:n], 1.0 / dfeat, EPS, ALU.mult, ALU.add,
                    reads=[btok], writes=["NT1"])
                act(NT1[:, 0:n], NT1[:, 0:n], AF.Sqrt, ["NT1"], ["NT1"])
                dve("reciprocal", RSTD[:, c0:c0 + n], NT1[:, 0:n], reads=["NT1"], writes=["RSTD"])
                for c in range(nch):
                    dve("scalar_tensor_tensor", dst(c, c0, n), src(c, c0, n), gain(c), RSTD[:, c0:c0 + n],
                        ALU.mult, ALU.mult, reads=[src_tok, "RSTD", "CST"], writes=[dst_tok])

        def Xc(c, c0, n):
            return X[:, c, c0:c0 + n]

        def XNc(c, c0, n):
            return XN[:, c, c0:c0 + n]

        def out_tok16(srcs, dst_ap, tag):
            nch = len(srcs)
            stg = P.carve("OTS_" + tag, SCR_BYTES - 4096, [D], F32, 0, NSAMP)
            for h0 in range(0, nch, 4):
                bank, btok = nb()
                cnt = min(4, nch - h0)
                for ci in range(cnt):
                    tr(bank[0:NSAMP, ci * 128:(ci + 1) * 128], srcs[h0 + ci][0], identf, [srcs[h0 + ci][1], "CST"], [btok])
                dve_copy(stg[:, h0 * 128:(h0 + cnt) * 128], bank[0:NSAMP, 0:cnt * 128], [btok], ["OTS_" + tag])
            dma("sp", dst_ap, stg[:, 0:nch * 128], ["OTS_" + tag], [], "OTS_" + tag)

        dma("sp", CST[:], cst, [], ["CST"], "CST")
        dve_copy(identb[:], identf, ["CST"], ["identb"])
        dve_copy(maskb[:], C("mask"), ["CST"], ["maskb"])
        dve("memset", onesb[:], 1.0, reads=[], writes=["onesb"])
        dve("memset", onesf[:], 1.0, reads=[], writes=["onesf"])
        dve("memset", UB[:], 0.0, reads=[], writes=["UB"])
        dve("memset", CH[:], 0.0, reads=[], writes=["CH"])
        dve("memset", HS[:], 0.0, reads=[], writes=["HS"])
        for k in range(8):
            dve("tensor_scalar", GWR[:, k, :], C("wr")[:, k * 8:(k + 1) * 8], C("nffn")[:, 8 + k:9 + k], None, ALU.mult,
                reads=["CST"], writes=["GWR"])
        act(NSP[:, 0:8], C("lam"), AF.Exp, ["CST"], ["NSP"], scale=-1.0)
        act(NSP[:, 0:8], NSP[:, 0:8], AF.Ln, ["NSP"], ["NSP"], bias=1.0)
        dve("tensor_scalar", NSP[:, 8:16], NSP[:, 0:8], -16.0, None, ALU.mult, reads=["NSP"], writes=["NSP"])
        dve("tensor_scalar", NSP[:, 0:8], NSP[:, 0:8], -8.0, None, ALU.mult, reads=["NSP"], writes=["NSP"])

        if do_s:
            ST0 = P.carve("ST0", 0, [512], F32, 0, 120)
            ST1 = P.carve("ST1", 2048, [512], F32, 0, 120)
            spv = spool.rearrange("b r f -> (b r) f")
            dma("sp", ST0, spv[0:120, :], [], ["ST0"], "ST0")
            dma("sp", ST1, spv[120:240, :], [], ["ST1"], "ST1")
            for g in range(4):
                bank, btok = nb()
                tr(bank[:, 0:120], ST0[:, g * 128:(g + 1) * 128], identf[0:120, 0:120], ["ST0", "CST"], [btok])
                tr(bank[:, 120:240], ST1[:, g * 128:(g + 1) * 128], identf[0:120, 0:120], ["ST1", "CST"], [btok])
                dve_copy(EXT[:, g, :, 0:15], bank[:, 0:240].rearrange("p (b r) -> p b r", b=NSAMP), [btok], ["EXT"])
            ST2 = P.carve("ST2", 4096, [D], F32, 0, 48)
            dma("sp", ST2, sconv.rearrange("b r f -> (b r) f"), [], ["ST2"], "ST2")
            for c in range(8):
                bank, btok = nb()
                tr(bank[:, 0:48], ST2[:, c * 128:(c + 1) * 128], identf[0:48, 0:48], ["ST2", "CST"], [btok])
                dve_copy(CEX[:, c, :, 0:3], bank[:, 0:48].rearrange("p (b r) -> p b r", b=NSAMP), [btok], ["CEX"])
            ST3 = P.carve("ST3", 8192, [D], F32, 0, NSAMP)
            dma("sp", ST3, slru, [], ["ST3"], "ST3")
            bank, btok = nb()
            for c in range(8):
                tr(bank[:, c * 16:(c + 1) * 16], ST3[:, c * 128:(c + 1) * 128], identf[0:NSAMP, 0:NSAMP], ["ST3", "CST"], [btok])
            dve_copy(H0S[:], bank[:, 0:128].rearrange("p (c b) -> p c b", c=8), [btok], ["H0S"])
            dma("sp", PTT[:], ptab.rearrange("b j -> j b"), [], ["PTT"], "PTT", allow_slow_non_contiguous=True)
            for rc in range(NGCH):
                dve("tensor_scalar", IDX[:, :, rc], PTT[:], NGCH, rc, ALU.mult, ALU.add, reads=["PTT"], writes=["IDX"])
            dve_copy(SEL[:], identf[0:16, 0:16].unsqueeze(2).broadcast_to([16, NSAMP, 8]), ["CST"], ["SEL"])
            dma("sp", pool_s[:, 0:14, :], spool[:, 1:15, :], [], [], "pool_s_cp")
            dma("sp", conv_s[:, 0:2, :], sconv[:, 1:3, :], [], [], "conv_s_cp")

        A_QR = 0
        A_QL = A_QR + 8 * W * 2
        A_PY = A_QL + 8 * W * 2
        A_OT = A_PY + 4 * W * 2
        A_F = A_OT + 4 * W * 2

        def swiglu_expert(sts, gname, uname, dname, e_idx, CALL):
            H = [P.carve("H%d" % j, j * W * 2, [W], BF16) for j in range(11)]
            o = 11 * W * 2
            SG = [P.carve("SG%d" % i, o + i * 2048, [512], F32) for i in range(2)]
            o += 4096
            SG2 = [P.carve("SGB%d" % i, o + i * 2048, [512], F32) for i in range(2)]
            cnt = 0
            for j3 in range(3):
                m0, mw = _split3(j3)
                wg, wgtok = wget(gname + str(j3))
                wu, wutok = wget(uname + str(j3))
                wgv = wg[:, 0:8 * mw].rearrange("p (k m) -> p k m", k=8)
                wuv_ = wu[:, 0:8 * mw].rearrange("p (k m) -> p k m", k=8)
                for ci in range(mw // 128):
                    jc = j3 * 4 + ci
                    for (c0, n) in sts:
                        bg, bgtok = nb()
                        mmgroup(bg, bgtok, 128, n, [(wgv[:, k, ci * 128:(ci + 1) * 128], XNc(k, c0, n)) for k in range(8)],
                                [wgtok, "XN"])
                        bu, butok = nb()
                        mmgroup(bu, butok, 128, n, [(wuv_[:, k, ci * 128:(ci + 1) * 128], XNc(k, c0, n)) for k in range(8)],
                                [wutok, "XN"])
                        sg, sgtok = SG[cnt % 2], "SG%d" % (cnt % 2)
                        sg2, sg2tok = SG2[cnt % 2], "SGB%d" % (cnt % 2)
                        cnt += 1
                        act(sg[:, 0:n], bg[:, 0:n], AF.Silu, [bgtok], [sgtok])
                        if e_idx is None:
                            dve("tensor_tensor", H[jc][:, c0:c0 + n], sg[:, 0:n], bu[:, 0:n], ALU.mult,
                                reads=[sgtok, butok], writes=["H%d" % jc])
                        else:
                            dve("tensor_tensor", sg2[:, 0:n], sg[:, 0:n], bu[:, 0:n], ALU.mult,
                                reads=[sgtok, butok], writes=[sg2tok])
                            dve("tensor_tensor", H[jc][:, c0:c0 + n], sg2[:, 0:n], CALL[:, e_idx, c0:c0 + n], ALU.mult,
                                reads=[sg2tok, "CALL"], writes=["H%d" % jc])
            htoks = ["H%d" % j for j in range(11)]
            for j4 in range(4):
                wd, wdtok = wget(dname + str(j4))
                wdv = wd[:, 0:11 * 256].rearrange("p (k m) -> p k m", k=11)
                for mi in range(2):
                    m = j4 * 2 + mi
                    for (c0, n) in sts:
                        bank, btok = nb()
                        mmgroup(bank, btok, 128, n, [(wdv[:, k, mi * 128:(mi + 1) * 128], H[k][:, c0:c0 + n]) for k in range(11)],
                                [wdtok] + htoks)
                        dve("tensor_tensor", X[:, m, c0:c0 + n], X[:, m, c0:c0 + n], bank[:, 0:n], ALU.add,
                            reads=["X", btok], writes=["X"])

        def ple(sts, l):
            rmsnorm(Xc, "X", 8, D, lambda c: C("nple")[:, l * 8 + c:l * 8 + c + 1], XNc, "XN", sts)
            o = 11 * W * 2
            SG = [P.carve("SG%d" % i, o + i * 2048, [512], F32) for i in range(2)]
            wp, wptok = wget("pp%d" % l)
            wpv = wp[:, 0:2048].rearrange("p (k m) -> p k m", k=2)
            cnt = 0
            for j in range(2):
                wg, wgtok = wget("pg%d%d" % (l, j))
                wgv = wg[:, 0:4096].rearrange("p (k m) -> p k m", k=8)
                for mi in range(4):
                    m = j * 4 + mi
                    for (c0, n) in sts:
                        bg, bgtok = nb()
                        mmgroup(bg, bgtok, 128, n, [(wgv[:, k, mi * 128:(mi + 1) * 128], XNc(k, c0, n)) for k in range(8)],
                                [wgtok, "XN"])
                        bp, bptok = nb()
                        mmgroup(bp, bptok, 128, n, [(wpv[:, k, m * 128:(m + 1) * 128], PT2[:, l, k, c0:c0 + n]) for k in range(2)],
                                [wptok, "PT2"])
                        sg, sgtok = SG[cnt % 2], "SG%d" % (cnt % 2)
                        cnt += 1
                        act(sg[:, 0:n], bg[:, 0:n], AF.Sigmoid, [bgtok], [sgtok])
                        dve("tensor_tensor", sg[:, 0:n], sg[:, 0:n], bp[:, 0:n], ALU.mult, reads=[sgtok, bptok], writes=[sgtok])
                        dve("tensor_tensor", X[:, m, c0:c0 + n], X[:, m, c0:c0 + n], sg[:, 0:n], ALU.add,
                            reads=["X", sgtok], writes=["X"])

        def router(sts):
            o = 11 * W * 2 + 8192
            CALL = P.carve("CALL", o, [8, W], BF16)
            o += 8 * W * 2
            LG = P.carve("LG", o, [8], F32)
            TOP = P.carve("TOP", o + 32, [8], F32)
            CB = P.carve("CB", o + 64, [8], F32)
            CB2 = P.carve("CB2", o + 96, [8], F32)
            WV = P.carve("WV", o + 128, [8], F32)
            o += 160
            DG = [P.carve("DG%d" % i, o + i * 512, [128], F32) for i in range(2)]
            cnt = 0
            for (c0, n) in sts:
                for tb in range((n + 127) // 128):
                    nt = min(128, n - tb * 128)
                    tc0 = c0 + tb * 128
                    bank, btok = nb()
                    for k in range(8):
                        mm(bank[0:nt, 0:8], X[:, k, tc0:tc0 + nt], GWR[:, k, :], k == 0, k == 7, ["X", "GWR"], [btok])
                    b2, b2tok = nb()
                    tr(b2[0:nt, 0:128], RSTD[:, tc0:tc0 + nt], identf, ["RSTD", "CST"], [b2tok])
                    dve_copy(WV[0:nt, 3:4], b2[0:nt, 0:1], [b2tok], ["WV"])
                    dve("tensor_scalar", LG[0:nt, :], bank[0:nt, 0:8], WV[0:nt, 3:4], None, ALU.mult,
                        reads=[btok, "WV"], writes=["LG"])
                    dve("max", TOP[0:nt, :], LG[0:nt, :], reads=["LG"], writes=["TOP"])
                    dve("tensor_tensor", WV[0:nt, 0:1], TOP[0:nt, 1:2], TOP[0:nt, 0:1], ALU.subtract,
                        reads=["TOP", "WV"], writes=["WV"])
                    act(WV[0:nt, 1:2], WV[0:nt, 0:1], AF.Sigmoid, ["WV"], ["WV"])
                    act(WV[0:nt, 2:3], WV[0:nt, 0:1], AF.Sigmoid, ["WV"], ["WV"], scale=-1.0)
                    dve("tensor_scalar", CB[0:nt, :], LG[0:nt, :], TOP[0:nt, 0:1], WV[0:nt, 2:3], ALU.is_equal, ALU.mult,
                        reads=["LG", "TOP", "WV"], writes=["CB"])
                    dve("tensor_scalar", CB2[0:nt, :], LG[0:nt, :], TOP[0:nt, 1:2], WV[0:nt, 1:2], ALU.is_equal, ALU.mult,
                        reads=["LG", "TOP", "WV"], writes=["CB2"])
                    dve("tensor_tensor", CB[0:nt, :], CB[0:nt, :], CB2[0:nt, :], ALU.add, reads=["CB", "CB2"], writes=["CB"])
                    for e_ in range(NEXP):
                        dg, dgtok = DG[cnt % 2], "DG%d" % (cnt % 2)
                        cnt += 1
                        dve("tensor_scalar", dg[0:nt, 0:nt], identf[0:nt, 0:nt], CB[0:nt, e_:e_ + 1], None, ALU.mult,
                            reads=["CST", "CB"], writes=[dgtok])
                        bc, bctok = nb()
                        mm(bc[:, 0:nt], onesf[0:nt, :], dg[0:nt, 0:nt], True, True, ["onesf", dgtok], [bctok])
                        act_copy(CALL[:, e_, tc0:tc0 + nt], bc[:, 0:nt], [bctok], ["CALL"])
            return CALL

        def rglru(t, sts):
            YL = P.carve("YL", 0, [8, W], BF16)
            o = 8 * W * 2
            RGIG = P.carve("RGIG", o, [8, 2, 128], BF16)
            o += 4096
            off, nel = woffs["rgig"]
            dma("pool", RGIG.rearrange("p a b c -> p (a b c)"), wflat[:, off:off + nel], [], ["RGIG"], "RGIG")
            names = ["XB", "GG", "TT", "XC", "RR", "IG", "AA", "BT", "HH"]
            tmp = []
            for par in range(2):
                d = {}
                for nm in names:
                    wd_ = (4 + W) if nm == "XB" else W
                    d[nm] = (P.carve("%s%d" % (nm, par), o, [wd_], F32), "%s%d" % (nm, par))
                    o += wd_ * 4
                d["XCB"] = (P.carve("XCB%d" % par, o, [W], BF16), "XCB%d" % par)
                o += W * 2
                tmp.append(d)
            def rg_a(c):
                T_ = tmp[c % 2]
                XB, xbt = T_["XB"]
                GG, ggt = T_["GG"]
                TT, ttt = T_["TT"]
                XC, xct = T_["XC"]
                RR, rrt = T_["RR"]
                IG, igt = T_["IG"]
                AA, aat = T_["AA"]
                BT, btt = T_["BT"]
                HH, hht = T_["HH"]
                XCB, xcbt = T_["XCB"]

                wt, wtok = wget("in1%d" % c)
                wv = wt[:, 0:2048].rearrange("p (k m) -> p k m", k=8)
                for (c0, n) in sts:
                    is_s = c0 >= TW
                    bx, bxtok = nb()
                    mmgroup(bx, bxtok, 128, n, [(wv[:, k, 0:128], XNc(k, c0, n)) for k in range(8)], [wtok, "XN"])
                    bg, bgtok = nb()
                    mmgroup(bg, bgtok, 128, n, [(wv[:, k, 128:256], XNc(k, c0, n)) for k in range(8)], [wtok, "XN"])
                    act_copy(GG[:, c0:c0 + n], bg[:, 0:n], [bgtok], [ggt])
                    act(TT[:, c0:c0 + n], GG[:, c0:c0 + n], AF.Square, [ggt], [ttt])
                    dve("tensor_scalar", TT[:, c0:c0 + n], TT[:, c0:c0 + n], 0.044715, 1.0, ALU.mult, ALU.add,
                        reads=[ttt], writes=[ttt])
                    dve("tensor_tensor", TT[:, c0:c0 + n], TT[:, c0:c0 + n], GG[:, c0:c0 + n], ALU.mult, reads=[ttt, ggt], writes=[ttt])
                    act(TT[:, c0:c0 + n], TT[:, c0:c0 + n], AF.Sigmoid, [ttt], [ttt], scale=1.5957691216057308)
                    dve("tensor_tensor", GG[:, c0:c0 + n], TT[:, c0:c0 + n], GG[:, c0:c0 + n], ALU.mult, reads=[ttt, ggt], writes=[ggt])
                    cw = C("convw")
                    if not is_s:
                        act_copy(XB[:, 0:4], CH[:, c, :], ["CH"], [xbt])
                        act_copy(XB[:, 4:4 + n], bx[:, 0:n], [bxtok], [xbt])
                        dve_copy(CH[:, c, :], XB[:, n:n + 4], [xbt], ["CH"])
                        dve("tensor_scalar", XC[:, 0:n], XB[:, 1:1 + n], cw[:, c:c + 1], C("convb")[:, c:c + 1],
                            ALU.mult, ALU.add, reads=[xbt, "CST"], writes=[xct])
                        for k in range(1, 4):
                            dve("scalar_tensor_tensor", XC[:, 0:n], XB[:, 1 + k:1 + k + n], cw[:, k * 8 + c:k * 8 + c + 1],
                                XC[:, 0:n], ALU.mult, ALU.add, reads=[xbt, xct, "CST"], writes=[xct])
                    else:
                        act_copy(CEX[:, c, :, 3], bx[:, 0:n], [bxtok], ["CEX"])
                        act_copy(NXB[:, c, :], bx[:, 0:n], [bxtok], ["NXB"])
                        dve("tensor_scalar", XC[:, c0:c0 + n], CEX[:, c, :, 0], cw[:, c:c + 1], C("convb")[:, c:c + 1],
                            ALU.mult, ALU.add, reads=["CEX", "CST"], writes=[xct])
                        for k in range(1, 4):
                            dve("scalar_tensor_tensor", XC[:, c0:c0 + n], CEX[:, c, :, k], cw[:, k * 8 + c:k * 8 + c + 1],
                                XC[:, c0:c0 + n], ALU.mult, ALU.add, reads=["CEX", xct, "CST"], writes=[xct])
                    act_copy(XCB[:, c0:c0 + n], XC[:, c0:c0 + n], [xct], [xcbt])

            def rg_b(c):
                T_ = tmp[c % 2]
                XB, xbt = T_["XB"]
                GG, ggt = T_["GG"]
                TT, ttt = T_["TT"]
                XC, xct = T_["XC"]
                RR, rrt = T_["RR"]
                IG, igt = T_["IG"]
                AA, aat = T_["AA"]
                BT, btt = T_["BT"]
                HH, hht = T_["HH"]
                XCB, xcbt = T_["XCB"]

                for (c0, n) in sts:
                    is_s = c0 >= TW
                    br, brtok = nb()
                    mm(br[:, 0:n], RGIG[:, c, 0, :], XCB[:, c0:c0 + n], True, True, ["RGIG", xcbt], [brtok])
                    bi_, bitok = nb()
                    mm(bi_[:, 0:n], RGIG[:, c, 1, :], XCB[:, c0:c0 + n], True, True, ["RGIG", xcbt], [bitok])
                    act(RR[:, c0:c0 + n], br[:, 0:n], AF.Sigmoid, [brtok, "CST"], [rrt], bias=C("brg")[:, c:c + 1])
                    act(IG[:, c0:c0 + n], bi_[:, 0:n], AF.Sigmoid, [bitok, "CST"], [igt], bias=C("big")[:, c:c + 1])
                    act(AA[:, c0:c0 + n], RR[:, c0:c0 + n], AF.Exp, [rrt, "NSP"], [aat], scale=NSP[:, c:c + 1])
                    act(BT[:, c0:c0 + n], RR[:, c0:c0 + n], AF.Exp, [rrt, "NSP"], [btt], scale=NSP[:, 8 + c:9 + c])
                    act(BT[:, c0:c0 + n], BT[:, c0:c0 + n], AF.Sqrt, [btt], [btt], scale=-1.0, bias=1.0)
                    dve("tensor_tensor", IG[:, c0:c0 + n], IG[:, c0:c0 + n], XC[:, c0:c0 + n], ALU.mult, reads=[igt, xct], writes=[igt])
                    dve("tensor_tensor", BT[:, c0:c0 + n], BT[:, c0:c0 + n], IG[:, c0:c0 + n], ALU.mult, reads=[btt, igt], writes=[btt])
                    if not is_s:
                        dve("tensor_tensor_scan", HH[:, 0:n], AA[:, 0:n], BT[:, 0:n], HS[:, c:c + 1], ALU.mult, ALU.add,
                            reads=[aat, btt, "HS"], writes=[hht])
                        dve_copy(HS[:, c:c + 1], HH[:, n - 1:n], [hht], ["HS"])
                    else:
                        dve("tensor_tensor", HH[:, c0:c0 + n], AA[:, c0:c0 + n], H0S[:, c, :], ALU.mult, reads=[aat, "H0S"], writes=[hht])
                        dve("tensor_tensor", HH[:, c0:c0 + n], HH[:, c0:c0 + n], BT[:, c0:c0 + n], ALU.add, reads=[hht, btt], writes=[hht])
                        dve_copy(H0S[:, c, :], HH[:, c0:c0 + n], [hht], ["H0S"])
                    dve("tensor_tensor", YL[:, c, c0:c0 + n], HH[:, c0:c0 + n], GG[:, c0:c0 + n], ALU.mult, reads=[hht, ggt], writes=["YL"])

            rg_a(0)
            for c in range(8):
                if c + 1 < 8:
                    rg_a(c + 1)
                rg_b(c)
            for j in range(2):
                wt, wtok = wget("out1%d" % j)
                wv = wt[:, 0:4096].rearrange("p (k m) -> p k m", k=8)
                for mi in range(4):
                    m = j * 4 + mi
                    for (c0, n) in sts:
                        bank, btok = nb()
                        mmgroup(bank, btok, 128, n, [(wv[:, k, mi * 128:(mi + 1) * 128], YL[:, k, c0:c0 + n]) for k in range(8)],
                                [wtok, "YL"])
                        dve("tensor_tensor", X[:, m, c0:c0 + n], X[:, m, c0:c0 + n], bank[:, 0:n], ALU.add,
                            reads=["X", btok], writes=["X"])
            if t == last_tile:
                for c in range(8):
                    dma("sp", conv_p[:, c * 128:(c + 1) * 128].rearrange("r f -> f r"), CH[:, c, 1:4], ["CH"], [], "conv_p",
                        allow_slow_non_contiguous=True)
                dma("sp", h_p.rearrange("o (c f) -> f (o c)", f=128), HS[:], ["HS"], [], "h_p", allow_slow_non_contiguous=True)
                if do_s:
                    out_tok16([(NXB[:, c, :], "NXB") for c in range(8)], conv_s[:, 2, :], "cs")
                    out_tok16([(H0S[:, c, :], "H0S") for c in range(8)], h_s, "hs")

        def decode_attention(QL, QR, RL, KRO, OT, wuv, wuvtok):
            o = A_F
            NG = 2
            G = [P.carve("G%d" % i, o + i * GCH * ROWW * 4, [GCH, ROWW], F32) for i in range(NG)]
            o += NG * GCH * ROWW * 4
            GBW = 162
            NGB = 3
            GB = [P.carve("GB%d" % i, o + i * GCH * GBW * 2, [GCH, GBW], BF16) for i in range(NGB)]
            o += NGB * GCH * GBW * 2
            KTL = [P.carve("KTL%d" % i, o + i * GCH * 256, [GCH, 128], BF16) for i in range(2)]
            o += 2 * GCH * 256
            KTR = [P.carve("KTR%d" % i, o + i * GCH * 256, [GCH, 128], BF16, 0, 32) for i in range(2)]
            o += 2 * GCH * 256
            STB = P.carve("STB", o, [GCH, 8], F32)
            o += GCH * 32
            PSB = P.carve("PSB", o, [GCH, 8], BF16)
            o += GCH * 32
            MXJ = P.carve("MXJ", o, [8], F32)
            MBC = P.carve("MBC", o + 32, [8], F32)
            o += 64
            QLS = P.carve("QLS", o, [NSAMP, 8], BF16)
            o += NSAMP * 16
            QRS = P.carve("QRS", o, [NSAMP, 8], BF16, 0, 32)
            o += NSAMP * 16
            MOLD = P.carve("MOLD", o, [NSAMP], F32, 0, 8)
            o += 64
            SMALL = P.carve("SMALL", o, [8], F32, 0, 8)
            o += 32
            DIAG = P.carve("DIAG", o, [8], F32, 0, 8)
            o += 32
            OACC = P.carve("OACC", o, [132], F32, 0, 8)
            o += 528
            OL1 = P.carve("OL1", o, [128], F32, 0, 8)
            o += 512
            OLST = P.carve("OLST", o, [8, NSAMP], BF16)
            o += 256
            OTOKS = P.carve("OTOKS", o, [512], BF16, 0, NSAMP)
            o += 1024
            dve_copy(QLS, QL[:, :, TW:W].rearrange("p h b -> p b h"), ["QL"], ["QLS"])
            dve_copy(QRS, QR[:, :, TW:W].rearrange("p h b -> p b h"), ["QR"], ["QRS"])
            bn, bntok = nb()
            for b in range(NSAMP):
                mm(bn[0:8, b:b + 1], QLS[:, b, :], RLB[:, b:b + 1], True, False, ["QLS", "RLB"], [bntok])
                mm(bn[0:8, b:b + 1], QRS[:, b, :], KROB[:, b:b + 1], False, True, ["QRS", "KROB"], [bntok])
            dve_copy(MOLD, bn[0:8, 0:NSAMP], [bntok], ["MOLD"])
            chunks = [(b, rc) for b in range(NSAMP) for rc in range(NGCH)]
            for i in range(NGB):
                dve("memset", GB[i][:, :, 128:130], 1.0, reads=[], writes=["GB%d" % i])
            P.rr = [0, 1, 2, 3, 4, 5]
            st = {}

            def s_init(b):
                bv0, bv0tok = nb()
                mm(bv0[0:8, 0:128], SEL[:, b, :], ROWS_S[:, 0:128], True, True, ["SEL", "ROWS_S"], [bv0tok])
                dve_copy(OACC[:, 0:128], bv0[0:8, 0:128], [bv0tok], ["OACC"])
                dve("memset", OACC[:, 128:129], 1.0, reads=[], writes=["OACC"])

            def s_fin(b):
                dve("reciprocal", SMALL[:, 3:4], OACC[:, 128:129], reads=["OACC", "SMALL"], writes=["SMALL"])
                dve("tensor_scalar", OL1, OACC[:, 0:128], SMALL[:, 3:4], None, ALU.mult, reads=["OACC", "SMALL"], writes=["OL1"])
                bt_, bttok = nb()
                tr(bt_[:, 0:8], OL1, identf[0:8, 0:8], ["OL1", "CST"], [bttok])
                act_copy(OLST[:, :, b], bt_[:, 0:8], [bttok], ["OLST"])

            def s1a(n):
                b, rc = chunks[n]
                gp = n % NG
                g, gtok = G[gp], "G%d" % gp
                P.add("pool", "indirect_dma_start", out=g.rearrange("p r c -> p (r c)"), out_offset=None, in_=cache,
                      in_offset=bass.IndirectOffsetOnAxis(ap=IDX[:, b, rc:rc + 1], axis=0),
                      reads=["IDX"], writes=[gtok], dma=True, key=gtok)
                gb, gbtok = GB[n % NGB], "GB%d" % (n % NGB)
                act_copy(gb[:, :, 0:128], g[:, :, 0:128], [gtok], [gbtok])
                dve_copy(gb[:, :, 130:162], g[:, :, 128:160], [gtok], [gbtok])

            def s1(n):
                b, rc = chunks[n]
                kp = n % 2
                ktl, ktltok = KTL[kp], "KTL%d" % kp
                ktr, ktrtok = KTR[kp], "KTR%d" % kp
                gb, gbtok = GB[n % NGB], "GB%d" % (n % NGB)
                for q8 in range(GCH // 8):
                    ba, batok = nb()
                    bav = bfview(ba)
                    bb, bbtok = nb()
                    bbv = bfview(bb)
                    for i in range(8):
                        r = q8 * 8 + i
                        tr(bav[:, i * 128:(i + 1) * 128], gb[:, r, 0:128], identb[:], [gbtok, "identb"], [batok])
                    for i in range(8):
                        r = q8 * 8 + i
                        tr(bbv[0:32, i * 128:(i + 1) * 128], gb[:, r, 130:162], identb[:], [gbtok, "identb"], [bbtok])
                    act_copy(ktl[:, q8 * 8:(q8 + 1) * 8, :], bav[:, 0:1024].rearrange("p (a b) -> p a b", a=8), [batok], [ktltok])
                    dve_copy(ktr[:, q8 * 8:(q8 + 1) * 8, :], bbv[0:32, 0:1024].rearrange("p (a b) -> p a b", a=8), [bbtok], [ktrtok])

            def s1s(n):
                b, rc = chunks[n]
                kp = n % 2
                ktl, ktltok = KTL[kp], "KTL%d" % kp
                ktr, ktrtok = KTR[kp], "KTR%d" % kp
                bs, bstok = banks[6 + kp], "bank%d" % (6 + kp)
                for r in range(GCH):
                    mm(bs[:, r * 8:(r + 1) * 8], ktl[:, r, :], QLS[:, b, :], True, False, [ktltok, "QLS"], [bstok])
                    mm(bs[:, r * 8:(r + 1) * 8], ktr[:, r, :], QRS[:, b, :], False, True, [ktrtok, "QRS"], [bstok])

            def s2(n):
                b, rc = chunks[n]
                gp = n % NG
                kp = n % 2
                g, gtok = G[gp], "G%d" % gp
                bs, bstok = banks[6 + kp], "bank%d" % (6 + kp)
                bsv = bs[:, 0:GCH * 8].rearrange("p (r h) -> p r h", h=8)
                dve("tensor_reduce", MXJ, bsv.rearrange("p r h -> p h r"), AX.X, ALU.max, reads=[bstok], writes=["MXJ"])
                bm, bmtok = nb()
                tr(bm[0:8, 0:128], MXJ, identf, ["MXJ", "CST"], [bmtok])
                dve("tensor_reduce", SMALL[:, 0:1], bm[0:8, 0:128], AX.X, ALU.max, reads=[bmtok], writes=["SMALL"])
                dve("tensor_tensor", SMALL[:, 1:2], MOLD[:, b:b + 1], SMALL[:, 0:1], ALU.max, reads=["MOLD", "SMALL"], writes=["SMALL"])
                dve("tensor_tensor", SMALL[:, 2:3], MOLD[:, b:b + 1], SMALL[:, 1:2], ALU.subtract, reads=["MOLD", "SMALL"], writes=["SMALL"])
                act(SMALL[:, 2:3], SMALL[:, 2:3], AF.Exp, ["SMALL"], ["SMALL"])
                dve_copy(MOLD[:, b:b + 1], SMALL[:, 1:2], ["SMALL"], ["MOLD"])
                dve("tensor_scalar", DIAG, identf[0:8, 0:8], SMALL[:, 1:2], None, ALU.mult, reads=["CST", "SMALL"], writes=["DIAG"])

            def s2b(n):
                b, rc = chunks[n]
                kp = n % 2
                bs, bstok = banks[6 + kp], "bank%d" % (6 + kp)
                bsv = bs[:, 0:GCH * 8].rearrange("p (r h) -> p r h", h=8)
                bd, bdtok = nb()
                mm(bd[:, 0:8], onesf[0:8, :], DIAG, True, True, ["onesf", "DIAG"], [bdtok])
                act_copy(MBC, bd[:, 0:8], [bdtok], ["MBC"])
                dve("tensor_tensor", STB, bsv, MBC.unsqueeze(1).broadcast_to([128, GCH, 8]), ALU.subtract,
                    reads=[bstok, "MBC"], writes=["STB"])
                act(PSB, STB, AF.Exp, ["STB"], ["PSB"])

            def s3(n):
                b, rc = chunks[n]
                gb, gbtok = GB[n % NGB], "GB%d" % (n % NGB)
                bo, botok = nb()
                for r in range(GCH):
                    mm(bo[0:8, 0:129], PSB[:, r, :], gb[:, r, 0:129], r == 0, r == GCH - 1, ["PSB", gbtok], [botok])
                dve("scalar_tensor_tensor", OACC[:, 0:129], OACC[:, 0:129], SMALL[:, 2:3], bo[0:8, 0:129], ALU.mult, ALU.add,
                    reads=["OACC", "SMALL", botok], writes=["OACC"])

            s1a(0)
            s1a(1)
            s1(0)
            s1s(0)
            for n in range(len(chunks)):
                b, rc = chunks[n]
                if n + 2 < len(chunks):
                    s1a(n + 2)
                if rc == 0:
                    s_init(b)
                s2(n)
                if n + 1 < len(chunks):
                    s1(n + 1)
                s2b(n)
                if n + 1 < len(chunks):
                    s1s(n + 1)
                s3(n)
                if rc == NGCH - 1:
                    s_fin(b)
            P.rr = list(range(8))
            bv, bvtok = nb()
            for h in range(8):
                mm(bv[0:NSAMP, h * 64:(h + 1) * 64], OLST[:, h, :], wuv[:, h * 64:(h + 1) * 64], True, True, ["OLST", wuvtok], [bvtok])
            dve_copy(OTOKS, bv[0:NSAMP, 0:512], [bvtok], ["OTOKS"])
            b2, b2tok = nb()
            b2v = bfview(b2)
            for j in range(4):
                tr(b2v[:, j * 16:(j + 1) * 16], OTOKS[:, j * 128:(j + 1) * 128], identb[0:NSAMP, 0:NSAMP], ["OTOKS", "identb"], [b2tok])
            act_copy(OT[:, :, TW:W], b2v[:, 0:64].rearrange("p (j n) -> p j n", j=4), [b2tok], ["OT"])

        try:
          for t in range(n_tiles):
              sts = [(0, TW)]
              if do_s and t == last_tile:
                  sts.append((TW, NSAMP))
              g0 = t * TW
              XS = P.carve("XS", A_F, [4, D], F32)
              PS_ = P.carve("PS_", A_F + 16384, [2, 4, 256], F32)
              for b in range(4):
                  dma("sp", XS[:, b, :], xp[g0 + b * 128:g0 + (b + 1) * 128, :], [], ["XS"], "XS%d" % b)
              for l in range(2):
                  dma("sp", PS_[:, l, :, :], pp[l, g0:g0 + TW, :].rearrange("(b p) f -> p b f", p=128), [], ["PS_"], "PS_%d" % l)
              dma("sp", ROPE[:, :, 0:TW], rope[:, :, g0:g0 + TW], [], ["ROPE"], "ROPEa")
              if do_s and t == last_tile:
                  dma("sp", ROPE[:, :, TW:W], rope[:, :, SEQ:SEQ + NSAMP], [], ["ROPE"], "ROPEb")
              for c in range(8):
                  bank, btok = nb()
                  for b in range(4):
                      tr(bank[:, b * 128:(b + 1) * 128], XS[:, b, c * 128:(c + 1) * 128], identf, ["XS", "CST"], [btok])
                  if c % 2 == 0:
                      act_copy(X[:, c, 0:TW], bank[:, 0:TW], [btok], ["X"])
                  else:
                      dve_copy(X[:, c, 0:TW], bank[:, 0:TW], [btok], ["X"])
              for l in range(2):
                  for k in range(2):
                      bank, btok = nb()
                      for b in range(4):
                          tr(bank[:, b * 128:(b + 1) * 128], PS_[:, l, b, k * 128:(k + 1) * 128], identf, ["PS_", "CST"], [btok])
                      act_copy(PT2[:, l, k, 0:TW], bank[:, 0:TW], [btok], ["PT2"])
              if do_s and t == last_tile:
                  XSS = P.carve("XSS", A_F + 24576, [D], F32, 0, NSAMP)
                  PSS = P.carve("PSS", A_F + 28672, [2, 256], F32, 0, NSAMP)
                  dma("sp", XSS, xs, [], ["XSS"], "XSS")
                  dma("sp", PSS, psm.rearrange("l b f -> b l f"), [], ["PSS"], "PSS")
                  bank, btok = nb()
                  for c in range(8):
                      tr(bank[:, c * 16:(c + 1) * 16], XSS[:, c * 128:(c + 1) * 128], identf[0:NSAMP, 0:NSAMP], ["XSS", "CST"], [btok])
                  dve_copy(X[:, :, TW:W], bank[:, 0:128].rearrange("p (c n) -> p c n", c=8), [btok], ["X"])
                  bank, btok = nb()
                  for l in range(2):
                      for k in range(2):
                          j = l * 2 + k
                          tr(bank[:, j * 16:(j + 1) * 16], PSS[:, l, k * 128:(k + 1) * 128], identf[0:NSAMP, 0:NSAMP], ["PSS", "CST"], [btok])
                  dve_copy(PT2[:, :, :, TW:W], bank[:, 0:64].rearrange("p (l k n) -> p l k n", l=2, k=2), [btok], ["PT2"])

              chk(1)
              rmsnorm(Xc, "X", 8, D, lambda c: C("nmix")[:, c:c + 1], XNc, "XN", sts)
              chk(2)
              CQ = P.carve("CQ", A_F, [2, W], F32)
              CKV = P.carve("CKV", A_F + 2 * W * 4, [W], F32)
              KR = P.carve("KR", A_F + 3 * W * 4, [W], F32, 0, 32)
              RL = P.carve("RL", A_F + 4 * W * 4, [W], F32)
              KRO = P.carve("KRO", A_F + 5 * W * 4, [W], F32, 0, 32)
              o1 = A_F + 6 * W * 4
              CQN = P.carve("CQN", o1, [2, W], BF16)
              o1 += 2 * W * 2
              PL = P.carve("PL", o1, [4, W], BF16)
              o1 += 4 * W * 2
              TA = P.carve("TA", o1, [16 + TW], F32)
              o1 += (16 + TW) * 4
              TB = P.carve("TB", o1, [16 + TW], F32)
              o1 += (16 + TW) * 4
              QN = [P.carve("QN%d" % i, o1 + i * W * 2, [W], BF16, 0, 64) for i in range(2)]
              o1 += 2 * W * 2
              QRR = P.carve("QRR", o1, [W], F32, 0, 32)
              o1 += W * 4
              T1 = P.carve("T1", o1, [TW], F32, 0, 32)
              o1 += TW * 4
              T2 = P.carve("T2", o1, [TW], F32, 0, 32)
              o1 += TW * 4
              ROWST = P.carve("ROWST", o1, [4, ROWW], F32)
              o1 += 4 * ROWW * 4
              QR = P.carve("QR", A_QR, [8, W], BF16, 0, 32)
              QL = P.carve("QL", A_QL, [8, W], BF16)
              PY = P.carve("PY", A_PY, [4, W], BF16)
              OT = P.carve("OT", A_OT, [4, W], BF16)

              wt, wtok = wget("in0a")
              wv = wt[:, 0:4096].rearrange("p (k m) -> p k m", k=8)
              for g in range(4):
                  for (c0, n) in sts:
                      bank, btok = nb()
                      mmgroup(bank, btok, 128, n, [(wv[:, k, g * 128:(g + 1) * 128], XNc(k, c0, n)) for k in range(8)], [wtok, "XN"])
                      act_copy(UB[:, g, 16 + c0:16 + c0 + n], bank[:, 0:n], [btok], ["UB"])
              wt, wtok = wget("in0b")
              wv = wt[:, 0:8 * 416].rearrange("p (k m) -> p k m", k=8)
              for (c0, n) in sts:
                  for j in range(2):
                      bank, btok = nb()
                      mmgroup(bank, btok, 128, n, [(wv[:, k, j * 128:(j + 1) * 128], XNc(k, c0, n)) for k in range(8)], [wtok, "XN"])
                      dve_copy(CQ[:, j, c0:c0 + n], bank[:, 0:n], [btok], ["CQ"])
                  bank, btok = nb()
                  mmgroup(bank, btok, 128, n, [(wv[:, k, 256:384], XNc(k, c0, n)) for k in range(8)], [wtok, "XN"])
                  act_copy(CKV[:, c0:c0 + n], bank[:, 0:n], [btok], ["CKV"])
                  bank, btok = nb()
                  mmgroup(bank, btok, 32, n, [(wv[:, k, 384:416], XNc(k, c0, n)) for k in range(8)], [wtok, "XN"])
                  dve_copy(KR[:, c0:c0 + n], bank[0:32, 0:n], [btok], ["KR"])

              chk(3)
              for g, wdw in enumerate((2, 4, 8, 16)):
                  U = UB[:, g, :]
                  cur, curtok = U, "UB"
                  valid = 1
                  d = 1
                  bufs = [(TA, "TA"), (TB, "TB")]
                  bi = 0
                  while d < wdw:
                      dst, dtok = bufs[bi]
                      bi ^= 1
                      lo = valid + d
                      dve("tensor_tensor", dst[:, lo:16 + TW], cur[:, lo:16 + TW], cur[:, lo - d:16 + TW - d], ALU.add,
                          reads=[curtok], writes=[dtok])
                      cur, curtok = dst, dtok
                      valid = lo
                      d *= 2
                  dve("scalar_tensor_tensor", PL[:, g, 0:TW], cur[:, 16:16 + TW], 1.0 / wdw, U[:, 16:16 + TW], ALU.mult, ALU.subtract,
                      reads=[curtok, "UB"], writes=["PL"])
                  if t == 0:
                      dve("tensor_tensor", NT1[:, 0:16], cur[:, 16:32], C("invc")[:, g * 16:(g + 1) * 16], ALU.mult,
                          reads=[curtok, "CST"], writes=["NT1"])
                      dve("tensor_tensor", PL[:, g, 0:16], NT1[:, 0:16], U[:, 16:32], ALU.subtract, reads=["NT1", "UB"], writes=["PL"])
              chk(31)
              if do_s and t == last_tile:
                  for g in range(4):
                      dve_copy(EXT[:, g, :, 15], UB[:, g, 16 + TW:16 + W], ["UB"], ["EXT"])
                      dve_copy(NU[:, g, :], UB[:, g, 16 + TW:16 + W], ["UB"], ["NU"])
                  for g, wdw in enumerate((2, 4, 8, 16)):
                      dve("tensor_reduce", NT1[:, 0:NSAMP], EXT[:, g, :, 16 - wdw:16], AX.X, ALU.add, reads=["EXT"], writes=["NT1"])
                      dve("scalar_tensor_tensor", PL[:, g, TW:W], NT1[:, 0:NSAMP], 1.0 / wdw, UB[:, g, 16 + TW:16 + W],
                          ALU.mult, ALU.subtract, reads=["NT1", "UB"], writes=["PL"])
              if t == last_tile:
                  for g in range(4):
                      dma("sp", pool_p[:, g * 128:(g + 1) * 128].rearrange("r f -> f r"), UB[:, g, 16 + TW - 15:16 + TW],
                          ["UB"], [], "pool_p", allow_slow_non_contiguous=True)
                  if do_s:
                      out_tok16([(NU[:, g, :], "NU") for g in range(4)], pool_s[:, 14, :], "ps")
              else:
                  dve_copy(UB[:, :, 0:16], UB[:, :, TW:TW + 16], ["UB"], ["UB"])
              chk(32)
              wt, wtok = wget("pool")
              wv = wt[:, 0:512].rearrange("p (g m) -> p g m", g=4)
              for g in range(4):
                  for (c0, n) in sts:
                      bank, btok = nb()
                      mmgroup(bank, btok, 128, n, [(wv[:, g, :], PL[:, g, c0:c0 + n])], [wtok, "PL"])
                      dve("tensor_scalar", PY[:, g, c0:c0 + n], bank[:, 0:n], C("pscale")[:, g:g + 1], None, ALU.mult, reads=[btok, "CST"], writes=["PY"])

              chk(4)
              rmsnorm(lambda c, c0, n: CQ[:, c, c0:c0 + n], "CQ", 2, 256, lambda c: C("qnorm")[:, c:c + 1],
                      lambda c, c0, n: CQN[:, c, c0:c0 + n], "CQN", sts)
              wq, wqtok = wget("uq")
              wqv = wq[:, 0:1536].rearrange("p (k m) -> p k m", k=2)
              wk, wktok = wget("uk")
              wkv = wk[:, 0:1024].rearrange("p (h c) -> p h c", h=8)
              r32t = C("r32t")[0:32, :]
              for h in range(8):
                  for (c0, n) in sts:
                      qn, qntok = QN[h % 2], "QN%d" % (h % 2)
                      bank, btok = nb()
                      mmgroup(bank, btok, 64, n, [(wqv[:, k, h * 64:(h + 1) * 64], CQN[:, k, c0:c0 + n]) for k in range(2)], [wqtok, "CQN"])
                      act_copy(qn[:, c0:c0 + n], bank[0:64, 0:n], [btok], [qntok])
                      bank, btok = nb()
                      mmgroup(bank, btok, 128, n, [(wkv[0:64, h, :], qn[:, c0:c0 + n])], [wktok, qntok])
                      act_copy(QL[:, h, c0:c0 + n], bank[:, 0:n], [btok], ["QL"], scale=SCALE)
                      bank, btok = nb()
                      mmgroup(bank, btok, 32, n, [(wqv[:, k, 512 + h * 32:512 + (h + 1) * 32], CQN[:, k, c0:c0 + n]) for k in range(2)],
                              [wqtok, "CQN"])
                      dve_copy(QRR[:, c0:c0 + n], bank[0:32, 0:n], [btok], ["QRR"])
                      bank, btok = nb()
                      mmgroup(bank, btok, 32, n, [(r32t, QRR[:, c0:c0 + n])], ["CST", "QRR"])
                      dve("scalar_tensor_tensor", T1[:, 0:n], QRR[:, c0:c0 + n], SCALE, ROPE[:, 0, c0:c0 + n], ALU.mult, ALU.mult,
                          reads=["QRR", "ROPE"], writes=["T1"])
                      dve("scalar_tensor_tensor", T2[:, 0:n], bank[0:32, 0:n], SCALE, ROPE[:, 1, c0:c0 + n], ALU.mult, ALU.mult,
                          reads=[btok, "ROPE"], writes=["T2"])
                      dve("tensor_tensor", QR[:, h, c0:c0 + n], T1[:, 0:n], T2[:, 0:n], ALU.add, reads=["T1", "T2"], writes=["QR"])

              chk(5)
              rmsnorm(lambda c, c0, n: CKV[:, c0:c0 + n], "CKV", 1, 128, lambda c: C("kvnorm")[:, 0:1],
                      lambda c, c0, n: RL[:, c0:c0 + n], "RL", sts)
              for (c0, n) in sts:
                  bank, btok = nb()
                  mmgroup(bank, btok, 32, n, [(r32t, KR[:, c0:c0 + n])], ["CST", "KR"])
                  dve("tensor_tensor", T1[:, 0:n], KR[:, c0:c0 + n], ROPE[:, 0, c0:c0 + n], ALU.mult, reads=["KR", "ROPE"], writes=["T1"])
                  dve("tensor_tensor", T2[:, 0:n], bank[0:32, 0:n], ROPE[:, 1, c0:c0 + n], ALU.mult, reads=[btok, "ROPE"], writes=["T2"])
                  dve("tensor_tensor", KRO[:, c0:c0 + n], T1[:, 0:n], T2[:, 0:n], ALU.add, reads=["T1", "T2"], writes=["KRO"])
              chk(51)
              act_copy(KLT[:, g0:g0 + TW], RL[:, 0:TW], ["RL"], ["KLT"])
              act_copy(KRT[:, g0:g0 + TW], KRO[:, 0:TW], ["KRO"], ["KRT"])
              chk(52)
              for b in range(4):
                  bank, btok = nb()
                  tr(bank[:, 0:128], RL[:, b * 128:(b + 1) * 128], identf, ["RL", "CST"], [btok])
                  tr(bank[:, 128:160], KRO[:, b * 128:(b + 1) * 128], identf[0:32, 0:32], ["KRO", "CST"], [btok])
                  dve_copy(ROWST[:, b, :], bank[:, 0:ROWW], [btok], ["ROWST"])
                  act_copy(VT[:, t * 4 + b, :], ROWST[:, b, 0:128], ["ROWST"], ["VT"])
              chk(53)
              dma("sp", rows_p[g0:g0 + TW, :].rearrange("(b p) c -> p b c", p=128), ROWST, ["ROWST"], [], "rows_p")
              if do_s and t == last_tile:
                  bank, btok = nb()
                  tr(bank[0:NSAMP, 0:128], RL[:, TW:W], identf, ["RL", "CST"], [btok])
                  tr(bank[0:NSAMP, 128:160], KRO[:, TW:W], identf[0:32, 0:32], ["KRO", "CST"], [btok])
                  dve_copy(ROWS_S[:], bank[0:NSAMP, 0:ROWW], [btok], ["ROWS_S"])
                  dve_copy(RLB[:], RL[:, TW:W], ["RL"], ["RLB"])
                  dve_copy(KROB[:], KRO[:, TW:W], ["KRO"], ["KROB"])
                  dma("sp", rows_s, ROWS_S[:], ["ROWS_S"], [], "rows_s")

              chk(6)
              o2 = A_F
              S_ = [P.carve("S%d" % i, o2 + i * SEQ * 4, [SEQ], F32) for i in range(2)]
              o2 += 2 * SEQ * 4
              PM = [P.carve("PM%d" % i, o2 + i * 2048, [1024], BF16) for i in range(2)]
              o2 += 4096
              PTS = [P.carve("PTS%d" % i, o2 + i * 2048, [8, 128], BF16) for i in range(2)]
              o2 += 4096
              OLT = [P.carve("OLT%d" % i, o2 + i * 256, [128], BF16) for i in range(2)]
              o2 += 512
              OTOK = P.carve("OTOK", o2, [512], BF16)
              o2 += 1024
              SM = [P.carve("SM%d" % i, o2 + i * 64, [16], F32) for i in range(2)]
              o2 += 128
              wuv, wuvtok = wget("uv")
              segc = 0
              items = [(qb, h) for qb in range(4) for h in range(8)]
              segc_box = [0]

              def att_a(idx):
                  qb, h = items[idx]
                  nk = (t * 4 + qb + 1) * 128
                  qc0 = qb * 128
                  par = idx % 2
                  S, stok = S_[par], "S%d" % par
                  sm, smtok = SM[par], "SM%d" % par
                  nchk = (nk + 511) // 512
                  for ck in range(nchk):
                      k0 = ck * 512
                      kw = min(512, nk - k0)
                      last = ck == nchk - 1
                      bank, btok = nb()
                      mm(bank[:, 0:kw], QL[:, h, qc0:qc0 + 128], KLT[:, k0:k0 + kw], True, False, ["QL", "KLT"], [btok])
                      mm(bank[:, 0:kw], QR[:, h, qc0:qc0 + 128], KRT[:, k0:k0 + kw], False, not last, ["QR", "KRT"], [btok])
                      if last:
                          mm(bank[:, kw - 128:kw], identb[:], maskb[:], False, True, ["identb", "maskb"], [btok])
                      dve("tensor_scalar", S[:, k0:k0 + kw], bank[:, 0:kw], 1.0, None, ALU.mult, ALU.max,
                          accum_out=sm[:, ck:ck + 1], reads=[btok], writes=[stok, smtok])
                  dve("tensor_reduce", sm[:, 8:9], sm[:, 0:nchk], AX.X, ALU.max, negate=True, reads=[smtok], writes=[smtok])

              def att_b(idx):
                  qb, h = items[idx]
                  nk = (t * 4 + qb + 1) * 128
                  qc0 = qb * 128
                  par = idx % 2
                  S, stok = S_[par], "S%d" % par
                  sm, smtok = SM[par], "SM%d" % par
                  nseg = (nk + 1023) // 1024
                  bo, botok = nb()
                  nkb = nk // 128
                  for sg in range(nseg):
                      s0 = sg * 1024
                      sw = min(1024, nk - s0)
                      sp_ = segc_box[0] % 2
                      segc_box[0] += 1
                      pm, pmtok = PM[sp_], "PM%d" % sp_
                      pts, ptstok = PTS[sp_], "PTS%d" % sp_
                      act(pm[:, 0:sw], S[:, s0:s0 + sw], AF.Exp, [stok, smtok], [pmtok, smtok],
                          bias=sm[:, 8:9], accum_out=sm[:, 9 + sg:10 + sg])
                      bt_, bttok = nb()
                      btv = bfview(bt_)
                      nbl = sw // 128
                      for j in range(nbl):
                          tr(btv[:, j * 128:(j + 1) * 128], pm[:, j * 128:(j + 1) * 128], identb[:], [pmtok, "identb"], [bttok])
                      ptsf = pts.rearrange("p a b -> p (a b)")
                      if sp_ == 0:
                          act_copy(ptsf[:, 0:sw], btv[:, 0:sw], [bttok], [ptstok])
                      else:
                          dve_copy(ptsf[:, 0:sw], btv[:, 0:sw], [bttok], [ptstok])
                      for j in range(nbl):
                          kb = sg * 8 + j
                          mm(bo[:, 0:128], VT[:, kb, :], pts[:, j, :], kb == 0, kb == nkb - 1, ["VT", ptstok], [botok])
                  olt, olttok = OLT[par], "OLT%d" % par
                  act_copy(olt, bo[:, 0:128], [botok], [olttok])
                  bv, bvtok = nb()
                  mm(bv[:, 0:64], olt, wuv[:, h * 64:(h + 1) * 64], True, True, [olttok, wuvtok], [bvtok])
                  dve("tensor_reduce", sm[:, 13:14], sm[:, 9:9 + nseg], AX.X, ALU.add, reads=[smtok], writes=[smtok])
                  dve("reciprocal", sm[:, 14:15], sm[:, 13:14], reads=[smtok], writes=[smtok])
                  dve("tensor_scalar", OTOK[:, h * 64:(h + 1) * 64], bv[:, 0:64], sm[:, 14:15], None, ALU.mult,
                      reads=[bvtok, smtok], writes=["OTOK"])
                  if h == 7:
                      bt_, bttok = nb()
                      btv = bfview(bt_)
                      for j in range(4):
                          tr(btv[:, j * 128:(j + 1) * 128], OTOK[:, j * 128:(j + 1) * 128], identb[:], ["OTOK", "identb"], [bttok])
                      act_copy(OT[:, :, qc0:qc0 + 128], btv[:, 0:512].rearrange("p (j n) -> p j n", j=4), [bttok], ["OT"])

              att_a(0)
              for idx in range(len(items)):
                  if idx + 1 < len(items):
                      att_a(idx + 1)
                  att_b(idx)

              if do_s and t == last_tile:
                  decode_attention(QL, QR, RL, KRO, OT, wuv, wuvtok)

              chk(7)
              for j in range(2):
                  wt, wtok = wget("out0%d" % j)
                  wv = wt[:, 0:4096].rearrange("p (k m) -> p k m", k=8)
                  for mi in range(4):
                      m = j * 4 + mi
                      for (c0, n) in sts:
                          bank, btok = nb()
                          pairs = [(wv[:, k, mi * 128:(mi + 1) * 128], PY[:, k, c0:c0 + n]) for k in range(4)]
                          pairs += [(wv[:, 4 + k, mi * 128:(mi + 1) * 128], OT[:, k, c0:c0 + n]) for k in range(4)]
                          mmgroup(bank, btok, 128, n, pairs, [wtok, "PY", "OT"])
                          dve("tensor_tensor", X[:, m, c0:c0 + n], X[:, m, c0:c0 + n], bank[:, 0:n], ALU.add,
                              reads=["X", btok], writes=["X"])

              chk(8)
              rmsnorm(Xc, "X", 8, D, lambda c: C("nffn")[:, c:c + 1], XNc, "XN", sts)
              for pe_ in range(2):
                  swiglu_expert(sts, "fg%d" % pe_, "fu%d" % pe_, "fd%d" % pe_, None, None)
              chk(9)
              ple(sts, 0)
              chk(10)
              rmsnorm(Xc, "X", 8, D, lambda c: C("nmix")[:, 8 + c:9 + c], XNc, "XN", sts)
              rglru(t, sts)
              chk(11)
              rmsnorm(Xc, "X", 8, D, lambda c: C("nffn")[:, 8 + c:9 + c], XNc, "XN", sts)
              chk(12)
              CALL = router(sts)
              chk(13)
              for e_ in range(NEXP):
                  swiglu_expert(sts, "eg%d" % e_, "eu%d" % e_, "ed%d" % e_, e_, CALL)
              chk(14)
              ple(sts, 1)
              chk(15)
              rmsnorm(Xc, "X", 8, D, lambda c: C("nfin")[:, c:c + 1], Xc, "X", sts)
              YS = [P.carve("YS%d" % i, i * 4096, [D], F32) for i in range(2)]
              for b in range(4):
                  ys, ystok = YS[b % 2], "YS%d" % (b % 2)
                  for hf in range(2):
                      bank, btok = nb()
                      for ci in range(4):
                          c = hf * 4 + ci
                          tr(bank[:, ci * 128:(ci + 1) * 128], X[:, c, b * 128:(b + 1) * 128], identf, ["X", "CST"], [btok])
                      if hf == 0:
                          act_copy(ys[:, 0:512], bank[:, 0:512], [btok], [ystok])
                      else:
                          dve_copy(ys[:, 512:1024], bank[:, 0:512], [btok], [ystok])
                  dma("sp", y_p[g0 + b * 128:g0 + (b + 1) * 128, :], ys, [ystok], [], ystok)
              if do_s and t == last_tile:
                  out_tok16([(X[:, c, TW:W], "X") for c in range(8)], y_s, "ys")

        except _Stop:
            pass
        print('sbuf bytes remaining', nc.sbuf_bytes_remaining)
        P.emit()
    return nc, P


_CACHE = {}


def _prep_common(inp):
    units, woffs, wtot = weight_units()
    wflat = np.empty((128, wtot), np.float32)
    for name, nel, fn in units:
        off, _ = woffs[name]
        wflat[:, off:off + nel] = fn(inp)
    return wflat, build_cst(inp), build_rope()


def kernel(**inp):
    inp = {k: np.asarray(v) for k, v in inp.items()}
    n_tiles = int(inp.pop("_n_tiles", NT))
    samples = bool(inp.pop("_samples", True))
    key = (n_tiles, samples)
    if key not in _CACHE:
        _CACHE[key] = build_program(n_tiles, samples)
    nc, P = _CACHE[key]
    wflat, cstv, ropev = _prep_common(inp)
    in_maps = []
    for c in range(NCORE):
        m = {
            "xp": np.ascontiguousarray(inp["x_prompt"][c]),
            "pp": np.ascontiguousarray(inp["p_prompt"][:, c]),
            "wflat": wflat, "cst": cstv, "rope": ropev,
        }
        if samples:
            sl = slice(c * NSAMP, (c + 1) * NSAMP)
            m.update({
                "xs": np.ascontiguousarray(inp["x_sample"][sl, 0]),
                "psm": np.ascontiguousarray(inp["p_sample"][:, sl, 0]),
                "cache": inp["cache_mla"][0].reshape(NPOOLPG * NGCH, GCH * ROWW),
                "spool": np.ascontiguousarray(inp["state_pool"][0, sl]),
                "sconv": np.ascontiguousarray(inp["state_conv"][0, sl]),
                "slru": np.ascontiguousarray(inp["state_lru"][0, sl]),
                "ptab": np.ascontiguousarray(inp["page_table"][sl]).astype(np.int32),
            })
        in_maps.append(m)
    res = run_bass_kernel_spmd(nc, in_maps, core_ids=list(range(NCORE)))
    r = res.results
    f32 = np.float32
    y_prompt = np.stack([r[c]["y_p"] for c in range(NCORE)]).astype(f32)
    rows_prompt = np.stack([r[c]["rows_p"] for c in range(NCORE)])[None].astype(f32)
    pool_prompt = np.stack([r[c]["pool_p"] for c in range(NCORE)])[None].astype(f32)
    conv_prompt = np.stack([r[c]["conv_p"] for c in range(NCORE)])[None].astype(f32)
    h_prompt = np.stack([r[c]["h_p"][0] for c in range(NCORE)])[None].astype(f32)
    if samples:
        y_sample = np.concatenate([r[c]["y_s"] for c in range(NCORE)])[:, None, :].astype(f32)
        rows_sample = np.concatenate([r[c]["rows_s"] for c in range(NCORE)])[None, :, None, :].astype(f32)
        pool_sample = np.concatenate([r[c]["pool_s"] for c in range(NCORE)])[None].astype(f32)
        conv_sample = np.concatenate([r[c]["conv_s"] for c in range(NCORE)])[None].astype(f32)
        h_sample = np.concatenate([r[c]["h_s"] for c in range(NCORE)])[None].astype(f32)
    else:
        y_sample = np.zeros((128, 1, D), f32)
        rows_sample = np.zeros((1, 128, 1, ROWW), f32)
        pool_sample = np.zeros((1, 128, 15, 512), f32)
        conv_sample = np.zeros((1, 128, 3, D), f32)
        h_sample = np.zeros((1, 128, D), f32)
    return (y_prompt, y_sample, rows_prompt, rows_sample, pool_prompt, pool_sample,
            conv_prompt, conv_sample, h_prompt, h_sample)
```

```python
import contextlib
import math
import numpy as np
import concourse.bass as bass
import concourse.mybir as mybir
from concourse.bass_utils import run_bass_kernel_spmd

F32 = mybir.dt.float32
BF16 = mybir.dt.bfloat16
I32 = mybir.dt.int32
AF = mybir.ActivationFunctionType
ALU = mybir.AluOpType
AX = mybir.AxisListType

D = 1024
SEQ = 4096
NCORE = 8
NSAMP = 16
TW = 512
W = TW + NSAMP
NT = SEQ // TW
PAST_PAGES = 128
PAGE = 128
ROWW = 160
NPOOLPG = 20480
SCALE = float((64 + 32) ** -0.5)
EPS = 1e-6
DEXP = 1408
NEXP = 8
GCH = 16
NGCH = PAGE // GCH
SLOT = 4096
NSLOT = 4
ENGS = ("pe", "act", "dve", "pool", "sp")


class Op:
    __slots__ = ("idx", "eng", "fn", "dma", "key", "deps", "inc", "cnt")

    def __init__(self, idx, eng, fn, dma, key):
        self.idx = idx
        self.eng = eng
        self.fn = fn
        self.dma = dma
        self.key = key
        self.deps = {}
        self.inc = False
        self.cnt = 0


class Prog:
    def __init__(self, nc):
        self.nc = nc
        self.ops = []
        self.lastw = {}
        self.readers = {}
        self.stack = contextlib.ExitStack()
        self.nbank = 0
        self.live = []
        self.scr = None

    def sb(self, name, shape, dtype):
        return self.stack.enter_context(self.nc.sbuf_tensor(name, list(shape), dtype))

    def ps(self, name, shape, dtype):
        return self.stack.enter_context(self.nc.psum_tensor(name, list(shape), dtype))

    def alias(self, new, olds):
        lst = self.readers.setdefault(new, [])
        for o in olds:
            lst.extend(self.readers.get(o, []))
            if o in self.lastw:
                lst.append(self.lastw[o])

    def carve(self, tok, off, shape, dtype, p0=0, p1=128):
        esz = 4 if dtype in (F32, I32) else 2
        nel = 1
        for s in shape:
            nel *= s
        nbytes = nel * esz
        assert off % 4 == 0 and nbytes % 4 == 0, (tok, off, nbytes)
        assert off + nbytes <= self.scr_bytes, (tok, off, nbytes, self.scr_bytes)
        ap = self.scr[p0:p1, off // 4:(off + nbytes) // 4]
        if dtype != F32:
            ap = ap.bitcast(dtype)
        if len(shape) == 2:
            ap = ap.rearrange("p (a b) -> p a b", a=shape[0])
        elif len(shape) == 3:
            ap = ap.rearrange("p (a b c) -> p a b c", a=shape[0], b=shape[1])
        lo, hi = off, off + nbytes
        same = [e for e in self.live if e[2] == tok]
        if same and same[0][0] == lo and same[0][1] == hi:
            return ap
        olds = [e for e in self.live if e[0] < hi and lo < e[1]]
        if olds:
            self.alias(tok, [e[2] for e in olds if e[2] != tok])
            keep = [e for e in self.live if not (e[0] < hi and lo < e[1])]
            for (lo_o, hi_o, tok_o) in olds:
                if tok_o == tok:
                    continue
                if lo_o < lo:
                    keep.append((lo_o, lo, tok_o))
                if hi_o > hi:
                    keep.append((hi, hi_o, tok_o))
            self.live = keep
        self.live.append((lo, hi, tok))
        return ap

    def add(self, eng, meth, *args, reads=(), writes=(), dma=False, key=None, **kw):
        op = Op(len(self.ops), eng, (meth, args, kw), dma, key)
        for t in reads:
            w = self.lastw.get(t)
            if w is not None:
                op.deps[w] = True
            if t.startswith("bank"):
                for r in self.readers.get(t, ()):
                    if self.ops[r].eng != eng:
                        op.deps[r] = True
                        self.rar = getattr(self, "rar", 0) + 1
        for t in writes:
            w = self.lastw.get(t)
            if w is not None and w not in op.deps:
                op.deps[w] = False
            for r in self.readers.get(t, ()):
                if r not in op.deps:
                    op.deps[r] = False
        for t in reads:
            lst = self.readers.setdefault(t, [])
            if not dma:
                lst[:] = [r for r in lst if self.ops[r].dma or self.ops[r].eng != eng]
            lst.append(op.idx)
        for t in writes:
            self.lastw[t] = op.idx
            self.readers[t] = []
        self.ops.append(op)
        return op

    def emit(self):
        nc = self.nc
        ops = self.ops
        for op in ops:
            for d, raw in op.deps.items():
                dop = ops[d]
                if dop.dma:
                    continue
                if dop.eng == op.eng and not op.dma:
                    if dop.eng == "pe":
                        continue
                dop.inc = True
        esem = {e: self.stack.enter_context(nc.semaphore("sem_" + e)) for e in ENGS}
        ecnt = {e: 0 for e in ENGS}
        ksem = {}
        kcnt = {}
        for op in ops:
            if op.dma:
                k = op.key
                if k not in ksem:
                    ksem[k] = self.stack.enter_context(nc.semaphore("dk%d" % len(ksem)))
                    kcnt[k] = 0
                kcnt[k] += 16
                op.cnt = kcnt[k]
            elif op.inc:
                ecnt[op.eng] += 1
                op.cnt = ecnt[op.eng]
        self.stats = dict(nops=len(ops), nsem=len(ksem) + len(ENGS), ecnt=dict(ecnt))
        per_eng = {e: [op for op in ops if op.eng == e] for e in ENGS}

        def run(eng_name, eng):
            waited = {}
            for op in per_eng[eng_name]:
                for d, raw in op.deps.items():
                    dop = ops[d]
                    if dop.dma:
                        s = ksem[dop.key]
                    else:
                        if dop.eng == op.eng and not op.dma:
                            if dop.eng == "pe":
                                continue
                        s = esem[dop.eng]
                    v = dop.cnt
                    if waited.get(s.name, 0) >= v:
                        continue
                    eng.wait_ge(s, v)
                    waited[s.name] = v
                meth, args, kw = op.fn
                ins = getattr(eng, meth)(*args, **kw)
                if op.dma:
                    ins.then_inc(ksem[op.key], 16)
                elif op.inc:
                    ins.then_inc(esem[op.eng], 1)
            if eng_name == "sp":
                for k, v in kcnt.items():
                    if waited.get(ksem[k].name, 0) >= v:
                        continue
                    eng.wait_ge(ksem[k], v)

        with nc.Block() as block:
            @block.tensor
            def _(e):
                run("pe", e)

            @block.scalar
            def _(e):
                run("act", e)

            @block.vector
            def _(e):
                run("dve", e)

            @block.gpsimd
            def _(e):
                run("pool", e)

            @block.sync
            def _(e):
                run("sp", e)


def _blk(Wm, k0, nk, m0, mw):
    a = Wm[k0 * 128:(k0 + nk) * 128, m0:m0 + mw]
    return np.ascontiguousarray(a.reshape(nk, 128, mw).transpose(1, 0, 2)).reshape(128, nk * mw)


def _split3(i):
    return i * 512, min(512, DEXP - i * 512)


def weight_units(inp=None):
    u = []

    def add(name, nel, fn):
        u.append((name, nel, fn))

    add("in0a", 8 * 512, lambda i: _blk(i["w_in0"][0], 0, 8, 0, 512))
    add("in0b", 8 * 416, lambda i: _blk(i["w_in0"][0], 0, 8, 512, 416))
    add("pool", 512, lambda i: np.ascontiguousarray(i["pool_w"][0].transpose(1, 0, 2)).reshape(128, 512))

    def uq(i):
        w = i["w_uq"][0].reshape(256, 8, 96)
        w2 = np.concatenate([w[:, :, :64].reshape(256, 512), w[:, :, 64:].reshape(256, 256)], axis=1)
        return _blk(w2, 0, 2, 0, 768)
    add("uq", 2 * 768, uq)

    def uk(i):
        w = np.ascontiguousarray(i["w_uk"][0].transpose(2, 1, 0)).reshape(64, 1024)
        return np.concatenate([w, np.zeros((64, 1024), np.float32)], axis=0)
    add("uk", 1024, uk)
    add("uv", 512, lambda i: np.ascontiguousarray(i["w_uv"][0].reshape(128, 512)))
    for j in range(2):
        add("out0%d" % j, 4096, lambda i, j=j: _blk(i["w_out0"][0], 0, 8, j * 512, 512))
    for pe in range(2):
        for j in range(3):
            m0, mw = _split3(j)
            add("fg%d%d" % (pe, j), 8 * mw, lambda i, pe=pe, m0=m0, mw=mw: _blk(i["w_ffn_gate"][0], 0, 8, pe * DEXP + m0, mw))
            add("fu%d%d" % (pe, j), 8 * mw, lambda i, pe=pe, m0=m0, mw=mw: _blk(i["w_ffn_up"][0], 0, 8, pe * DEXP + m0, mw))
        for j in range(4):
            add("fd%d%d" % (pe, j), 11 * 256, lambda i, pe=pe, j=j: _blk(i["w_ffn_down"][0], pe * 11, 11, j * 256, 256))
    for l in range(2):
        for j in range(2):
            add("pg%d%d" % (l, j), 4096, lambda i, l=l, j=j: _blk(i["w_ple_gate"][l], 0, 8, j * 512, 512))
        add("pp%d" % l, 2048, lambda i, l=l: _blk(i["w_ple_proj"][l], 0, 2, 0, 1024))
    for c in range(8):
        def in1(i, c=c):
            w = i["w_in1"][0]
            w2 = np.concatenate([w[:, c * 128:(c + 1) * 128], w[:, 1024 + c * 128:1024 + (c + 1) * 128]], axis=1)
            return _blk(w2, 0, 8, 0, 256)
        add("in1%d" % c, 2048, in1)

    def rgig(i):
        a = i["w_rg"][0].transpose(1, 0, 2)
        b = i["w_ig"][0].transpose(1, 0, 2)
        return np.ascontiguousarray(np.stack([a, b], axis=2)).reshape(128, 2048)
    add("rgig", 2048, rgig)
    for j in range(2):
        add("out1%d" % j, 4096, lambda i, j=j: _blk(i["w_out1"][0], 0, 8, j * 512, 512))
    for e in range(NEXP):
        for j in range(3):
            m0, mw = _split3(j)
            add("eg%d%d" % (e, j), 8 * mw, lambda i, e=e, m0=m0, mw=mw: _blk(i["w_exp_gate"][0, e], 0, 8, m0, mw))
            add("eu%d%d" % (e, j), 8 * mw, lambda i, e=e, m0=m0, mw=mw: _blk(i["w_exp_up"][0, e], 0, 8, m0, mw))
        for j in range(4):
            add("ed%d%d" % (e, j), 11 * 256, lambda i, e=e, j=j: _blk(i["w_exp_down"][0, e], 0, 11, j * 256, 256))
    offs = {}
    off = 0
    for name, nel, fn in u:
        offs[name] = (off, nel)
        off += nel
    return u, offs, off


def cst_layout():
    names = [("identf", 128), ("mask", 128), ("nmix", 16), ("nffn", 16), ("nple", 16), ("nfin", 8),
             ("pscale", 4), ("qnorm", 2), ("kvnorm", 1), ("convw", 32), ("convb", 8), ("brg", 8), ("big", 8),
             ("lam", 8), ("wr", 64), ("r32t", 32), ("invc", 64)]
    offs = {}
    off = 0
    for n, c in names:
        offs[n] = (off, c)
        off += c
    return offs, off


def _fm(v):
    return np.ascontiguousarray(np.asarray(v, np.float32).reshape(-1, 128).T)


def build_cst(inp):
    offs, tot = cst_layout()
    c = np.zeros((128, tot), np.float32)

    def put(name, arr):
        o, n = offs[name]
        arr = np.asarray(arr, np.float32)
        c[:arr.shape[0], o:o + arr.shape[1]] = arr
    put("identf", np.eye(128, dtype=np.float32))
    q = np.arange(128)
    put("mask", np.where(q[None, :] <= q[:, None], 0.0, -30000.0).astype(np.float32))
    put("nmix", np.concatenate([_fm(inp["norm_mix"][0]), _fm(inp["norm_mix"][1])], axis=1))
    put("nffn", np.concatenate([_fm(inp["norm_ffn"][0]), _fm(inp["norm_ffn"][1])], axis=1))
    put("nple", np.concatenate([_fm(inp["norm_ple"][0]), _fm(inp["norm_ple"][1])], axis=1))
    put("nfin", _fm(inp["norm_final"]))
    put("pscale", _fm(inp["pool_scale"][0]))
    put("qnorm", _fm(inp["q_norm"][0]))
    put("kvnorm", _fm(inp["kv_norm"][0]))
    put("convw", np.concatenate([_fm(inp["conv_w"][0][k]) for k in range(4)], axis=1))
    put("convb", _fm(inp["conv_b"][0]))
    put("brg", _fm(inp["b_rg"][0]))
    put("big", _fm(inp["b_ig"][0]))
    put("lam", _fm(inp["lru_lambda"][0]))
    put("wr", np.ascontiguousarray(inp["w_router"][0].reshape(8, 128, 8).transpose(1, 0, 2)).reshape(128, 64))
    R = np.zeros((32, 32), np.float32)
    for i in range(16):
        R[i, 16 + i] = -1.0
        R[16 + i, i] = 1.0
    put("r32t", R.T)
    invc = np.zeros((128, 64), np.float32)
    for g, w in enumerate((2, 4, 8, 16)):
        for t in range(16):
            invc[:, g * 16 + t] = 1.0 / min(t + 1, w)
    put("invc", invc)
    return c


def build_rope():
    inv = (np.float32(10000.0) ** (-np.arange(0, 32, 2, dtype=np.float32) / np.float32(32))).astype(np.float32)
    pos = np.concatenate([np.arange(SEQ, dtype=np.float32), np.full(NSAMP, PAST_PAGES * PAGE, np.float32)])
    ang = (pos[:, None] * inv[None, :]).astype(np.float32)
    cos = np.cos(ang).astype(np.float32).T
    sin = np.sin(ang).astype(np.float32).T
    r = np.zeros((32, 2, SEQ + NSAMP), np.float32)
    r[0:16, 0] = cos
    r[16:32, 0] = cos
    r[0:16, 1] = sin
    r[16:32, 1] = sin
    return r


class _Stop(Exception):
    pass


def build_program(n_tiles=NT, samples=True, stop=0):
    def chk(stage):
        if stage == stop:
            raise _Stop()

    nc = bass.Bass("TRN2", target_bir_lowering=False)
    units, woffs, wtot = weight_units()
    coffs, ctot = cst_layout()
    last_tile = n_tiles - 1
    do_s = samples

    def din(name, shape, dt=F32):
        return nc.dram_tensor(name, list(shape), dt, kind="ExternalInput").ap()

    def dout(name, shape, dt=F32):
        return nc.dram_tensor(name, list(shape), dt, kind="ExternalOutput").ap()

    xp = din("xp", [SEQ, D])
    pp = din("pp", [2, SEQ, 256])
    wflat = din("wflat", [128, wtot])
    cst = din("cst", [128, ctot])
    rope = din("rope", [32, 2, SEQ + NSAMP])
    y_p = dout("y_p", [SEQ, D])
    rows_p = dout("rows_p", [SEQ, ROWW])
    pool_p = dout("pool_p", [15, 512])
    conv_p = dout("conv_p", [3, D])
    h_p = dout("h_p", [1, D])
    if do_s:
        xs = din("xs", [NSAMP, D])
        psm = din("psm", [2, NSAMP, 256])
        cache = din("cache", [NPOOLPG * NGCH, GCH * ROWW])
        spool = din("spool", [NSAMP, 15, 512])
        sconv = din("sconv", [NSAMP, 3, D])
        slru = din("slru", [NSAMP, D])
        ptab = din("ptab", [NSAMP, PAST_PAGES], I32)
        y_s = dout("y_s", [NSAMP, D])
        rows_s = dout("rows_s", [NSAMP, ROWW])
        pool_s = dout("pool_s", [NSAMP, 15, 512])
        conv_s = dout("conv_s", [NSAMP, 3, D])
        h_s = dout("h_s", [NSAMP, D])

    P = Prog(nc)
    with P.stack:
        X = P.sb("X", [128, 8, W], F32)
        XN = P.sb("XN", [128, 8, W], BF16)
        KLT = P.sb("KLT", [128, SEQ], BF16)
        KRT = P.sb("KRT", [32, SEQ], BF16)
        VT = P.sb("VT", [128, SEQ // 128, 128], BF16)
        UB = P.sb("UB", [128, 4, 16 + W], F32)
        CH = P.sb("CH", [128, 8, 4], F32)
        HS = P.sb("HS", [128, 8], F32)
        PT2 = P.sb("PT2", [128, 2, 2, W], BF16)
        CST = P.sb("CST", [128, ctot], F32)
        ROPE = P.sb("ROPE", [32, 2, W], F32)
        identb = P.sb("identb", [128, 128], BF16)
        maskb = P.sb("maskb", [128, 128], BF16)
        onesb = P.sb("onesb", [128, 128], BF16)
        onesf = P.sb("onesf", [128, 128], F32)
        GWR = P.sb("GWR", [128, 8, 8], F32)
        NSP = P.sb("NSP", [128, 16], F32)
        SQ = [P.sb("SQ%d" % i, [128, 512], BF16) for i in range(2)]
        NT1 = P.sb("NT1", [128, 512], F32)
        RSTD = P.sb("RSTD", [128, W], F32)
        slots = [P.sb("wslot%d" % i, [128, SLOT], BF16) for i in range(NSLOT)]
        SCR_BYTES = 88 * 1024
        P.scr = P.sb("SCR", [128, SCR_BYTES // 4], F32)
        P.scr_bytes = SCR_BYTES
        banks = [P.ps("bank%d" % i, [128, 512], F32) for i in range(8)]
        if do_s:
            IDX = P.sb("IDX", [128, NSAMP, NGCH], I32)
            PTT = P.sb("PTT", [128, NSAMP], I32)
            EXT = P.sb("EXT", [128, 4, NSAMP, 16], F32)
            CEX = P.sb("CEX", [128, 8, NSAMP, 4], F32)
            H0S = P.sb("H0S", [128, 8, NSAMP], F32)
            NXB = P.sb("NXB", [128, 8, NSAMP], F32)
            NU = P.sb("NU", [128, 4, NSAMP], F32)
            SEL = P.sb("SEL", [16, NSAMP, 8], F32)
            ROWS_S = P.sb("ROWS_S", [16, ROWW], F32)
            RLB = P.sb("RLB", [128, NSAMP], BF16)
            KROB = P.sb("KROB", [32, NSAMP], BF16)

        def C(name):
            o, c = coffs[name]
            return CST[:, o:o + c]

        identf = C("identf")

        P.rr = list(range(8))

        def nb():
            i = P.rr[P.nbank % len(P.rr)]
            P.nbank += 1
            return banks[i], "bank%d" % i

        def bfview(bank):
            return bank[:, 0:512].bitcast(BF16)

        def mm(out, lhsT, rhs, start, stop, reads, writes):
            P.add("pe", "matmul", out, lhsT, rhs, start=start, stop=stop, reads=reads, writes=writes)

        def tr(out, in_, ident, reads, writes):
            P.add("pe", "transpose", out, in_, ident, reads=reads, writes=writes)

        def act(out, in_, func, reads, writes, **kw):
            P.add("act", "activation", out, in_, func, reads=reads, writes=writes, **kw)

        def dve(meth, *args, reads, writes, **kw):
            P.add("dve", meth, *args, reads=reads, writes=writes, **kw)

        def dma(eng, out, in_, reads, writes, key, **kw):
            P.add(eng, "dma_start", out=out, in_=in_, reads=reads, writes=writes, dma=True, key=key, **kw)

        def act_copy(out, in_, reads, writes, scale=None):
            if scale is None:
                act(out, in_, AF.Copy, reads, writes)
            else:
                act(out, in_, AF.Copy, reads, writes, scale=scale)

        def dve_copy(out, in_, reads, writes):
            dve("tensor_copy", out, in_, reads=reads, writes=writes)

        def mmgroup(bank, btok, msz, n, pairs, reads):
            k = len(pairs)
            for i, (l, r) in enumerate(pairs):
                mm(bank[0:msz, 0:n], l, r, i == 0, i == k - 1, reads, [btok])

        wstate = {"u": 0}

        def wget(name):
            off, nel = woffs[name]
            s = wstate["u"] % NSLOT
            wstate["u"] += 1
            tok = "wslot%d" % s
            dma("pool", slots[s][:, 0:nel], wflat[:, off:off + nel], [], [tok], tok)
            return slots[s], tok

        def rmsnorm(src, src_tok, nch, dfeat, gain, dst, dst_tok, sts):
            for (c0, n) in sts:
                bank, btok = nb()
                for c in range(nch):
                    sq, sqt = SQ[c % 2], "SQ%d" % (c % 2)
                    if c % 2 == 0:
                        act(sq[:, 0:n], src(c, c0, n), AF.Square, [src_tok], [sqt])
                    else:
                        dve("tensor_tensor", sq[:, 0:n], src(c, c0, n), src(c, c0, n), ALU.mult, reads=[src_tok], writes=[sqt])
                    mm(bank[:, 0:n], onesb[:], sq[:, 0:n], c == 0, c == nch - 1, [sqt, "onesb"], [btok])
                dve("tensor_scalar", NT1[:, 0:n], bank[:, 0:n], 1.0 / dfeat, EPS, ALU.mult, ALU.add,
                    reads=[btok], writes=["NT1"])
                act(NT1[:, 0:n], NT1[:, 0:n], AF.Sqrt, ["NT1"], ["NT1"])
                dve("reciprocal", RSTD[:, c0:c0 + n], NT1[:, 0:n], reads=["NT1"], writes=["RSTD"])
                for c in range(nch):
                    dve("scalar_tensor_tensor", dst(c, c0, n), src(c, c0, n), gain(c), RSTD[:, c0:c0 + n],
                        ALU.mult, ALU.mult, reads=[src_tok, "RSTD", "CST"], writes=[dst_tok])

        def Xc(c, c0, n):
            return X[:, c, c0:c0 + n]

        def XNc(c, c0, n):
            return XN[:, c, c0:c0 + n]

        def out_tok16(srcs, dst_ap, tag):
            nch = len(srcs)
            stg = P.carve("OTS_" + tag, SCR_BYTES - 4096, [D], F32, 0, NSAMP)
            for h0 in range(0, nch, 4):
                bank, btok = nb()
                cnt = min(4, nch - h0)
                for ci in range(cnt):
                    tr(bank[0:NSAMP, ci * 128:(ci + 1) * 128], srcs[h0 + ci][0], identf, [srcs[h0 + ci][1], "CST"], [btok])
                dve_copy(stg[:, h0 * 128:(h0 + cnt) * 128], bank[0:NSAMP, 0:cnt * 128], [btok], ["OTS_" + tag])
            dma("sp", dst_ap, stg[:, 0:nch * 128], ["OTS_" + tag], [], "OTS_" + tag)

        dma("sp", CST[:], cst, [], ["CST"], "CST")
        dve_copy(identb[:], identf, ["CST"], ["identb"])
        dve_copy(maskb[:], C("mask"), ["CST"], ["maskb"])
        dve("memset", onesb[:], 1.0, reads=[], writes=["onesb"])
        dve("memset", onesf[:], 1.0, reads=[], writes=["onesf"])
        dve("memset", UB[:], 0.0, reads=[], writes=["UB"])
        dve("memset", CH[:], 0.0, reads=[], writes=["CH"])
        dve("memset", HS[:], 0.0, reads=[], writes=["HS"])
        for k in range(8):
            dve("tensor_scalar", GWR[:, k, :], C("wr")[:, k * 8:(k + 1) * 8], C("nffn")[:, 8 + k:9 + k], None, ALU.mult,
                reads=["CST"], writes=["GWR"])
        act(NSP[:, 0:8], C("lam"), AF.Exp, ["CST"], ["NSP"], scale=-1.0)
        act(NSP[:, 0:8], NSP[:, 0:8], AF.Ln, ["NSP"], ["NSP"], bias=1.0)
        dve("tensor_scalar", NSP[:, 8:16], NSP[:, 0:8], -16.0, None, ALU.mult, reads=["NSP"], writes=["NSP"])
        dve("tensor_scalar", NSP[:, 0:8], NSP[:, 0:8], -8.0, None, ALU.mult, reads=["NSP"], writes=["NSP"])

        if do_s:
            ST0 = P.carve("ST0", 0, [512], F32, 0, 120)
            ST1 = P.carve("ST1", 2048, [512], F32, 0, 120)
            spv = spool.rearrange("b r f -> (b r) f")
            dma("sp", ST0, spv[0:120, :], [], ["ST0"], "ST0")
            dma("sp", ST1, spv[120:240, :], [], ["ST1"], "ST1")
            for g in range(4):
                bank, btok = nb()
                tr(bank[:, 0:120], ST0[:, g * 128:(g + 1) * 128], identf[0:120, 0:120], ["ST0", "CST"], [btok])
                tr(bank[:, 120:240], ST1[:, g * 128:(g + 1) * 128], identf[0:120, 0:120], ["ST1", "CST"], [btok])
                dve_copy(EXT[:, g, :, 0:15], bank[:, 0:240].rearrange("p (b r) -> p b r", b=NSAMP), [btok], ["EXT"])
            ST2 = P.carve("ST2", 4096, [D], F32, 0, 48)
            dma("sp", ST2, sconv.rearrange("b r f -> (b r) f"), [], ["ST2"], "ST2")
            for c in range(8):
                bank, btok = nb()
                tr(bank[:, 0:48], ST2[:, c * 128:(c + 1) * 128], identf[0:48, 0:48], ["ST2", "CST"], [btok])
                dve_copy(CEX[:, c, :, 0:3], bank[:, 0:48].rearrange("p (b r) -> p b r", b=NSAMP), [btok], ["CEX"])
            ST3 = P.carve("ST3", 8192, [D], F32, 0, NSAMP)
            dma("sp", ST3, slru, [], ["ST3"], "ST3")
            bank, btok = nb()
            for c in range(8):
                tr(bank[:, c * 16:(c + 1) * 16], ST3[:, c * 128:(c + 1) * 128], identf[0:NSAMP, 0:NSAMP], ["ST3", "CST"], [btok])
            dve_copy(H0S[:], bank[:, 0:128].rearrange("p (c b) -> p c b", c=8), [btok], ["H0S"])
            dma("sp", PTT[:], ptab.rearrange("b j -> j b"), [], ["PTT"], "PTT", allow_slow_non_contiguous=True)
            for rc in range(NGCH):
                dve("tensor_scalar", IDX[:, :, rc], PTT[:], NGCH, rc, ALU.mult, ALU.add, reads=["PTT"], writes=["IDX"])
            dve_copy(SEL[:], identf[0:16, 0:16].unsqueeze(2).broadcast_to([16, NSAMP, 8]), ["CST"], ["SEL"])
            dma("sp", pool_s[:, 0:14, :], spool[:, 1:15, :], [], [], "pool_s_cp")
            dma("sp", conv_s[:, 0:2, :], sconv[:, 1:3, :], [], [], "conv_s_cp")

        A_QR = 0
        A_QL = A_QR + 8 * W * 2
        A_PY = A_QL + 8 * W * 2
        A_OT = A_PY + 4 * W * 2
        A_F = A_OT + 4 * W * 2

        def swiglu_expert(sts, gname, uname, dname, e_idx, CALL):
            H = [P.carve("H%d" % j, j * W * 2, [W], BF16) for j in range(11)]
            o = 11 * W * 2
            SG = [P.carve("SG%d" % i, o + i * 2048, [512], F32) for i in range(2)]
            o += 4096
            SG2 = [P.carve("SGB%d" % i, o + i * 2048, [512], F32) for i in range(2)]
            cnt = 0
            for j3 in range(3):
                m0, mw = _split3(j3)
                wg, wgtok = wget(gname + str(j3))
                wu, wutok = wget(uname + str(j3))
                wgv = wg[:, 0:8 * mw].rearrange("p (k m) -> p k m", k=8)
                wuv_ = wu[:, 0:8 * mw].rearrange("p (k m) -> p k m", k=8)
                for ci in range(mw // 128):
                    jc = j3 * 4 + ci
                    for (c0, n) in sts:
                        bg, bgtok = nb()
                        mmgroup(bg, bgtok, 128, n, [(wgv[:, k, ci * 128:(ci + 1) * 128], XNc(k, c0, n)) for k in range(8)],
                                [wgtok, "XN"])
                        bu, butok = nb()
                        mmgroup(bu, butok, 128, n, [(wuv_[:, k, ci * 128:(ci + 1) * 128], XNc(k, c0, n)) for k in range(8)],
                                [wutok, "XN"])
                        sg, sgtok = SG[cnt % 2], "SG%d" % (cnt % 2)
                        sg2, sg2tok = SG2[cnt % 2], "SGB%d" % (cnt % 2)
                        cnt += 1
                        act(sg[:, 0:n], bg[:, 0:n], AF.Silu, [bgtok], [sgtok])
                        if e_idx is None:
                            dve("tensor_tensor", H[jc][:, c0:c0 + n], sg[:, 0:n], bu[:, 0:n], ALU.mult,
                                reads=[sgtok, butok], writes=["H%d" % jc])
                        else:
                            dve("tensor_tensor", sg2[:, 0:n], sg[:, 0:n], bu[:, 0:n], ALU.mult,
                                reads=[sgtok, butok], writes=[sg2tok])
                            dve("tensor_tensor", H[jc][:, c0:c0 + n], sg2[:, 0:n], CALL[:, e_idx, c0:c0 + n], ALU.mult,
                                reads=[sg2tok, "CALL"], writes=["H%d" % jc])
            htoks = ["H%d" % j for j in range(11)]
            for j4 in range(4):
                wd, wdtok = wget(dname + str(j4))
                wdv = wd[:, 0:11 * 256].rearrange("p (k m) -> p k m", k=11)
                for mi in range(2):
                    m = j4 * 2 + mi
                    for (c0, n) in sts:
                        bank, btok = nb()
                        mmgroup(bank, btok, 128, n, [(wdv[:, k, mi * 128:(mi + 1) * 128], H[k][:, c0:c0 + n]) for k in range(11)],
                                [wdtok] + htoks)
                        dve("tensor_tensor", X[:, m, c0:c0 + n], X[:, m, c0:c0 + n], bank[:, 0:n], ALU.add,
                            reads=["X", btok], writes=["X"])

        def ple(sts, l):
            rmsnorm(Xc, "X", 8, D, lambda c: C("nple")[:, l * 8 + c:l * 8 + c + 1], XNc, "XN", sts)
            o = 11 * W * 2
            SG = [P.carve("SG%d" % i, o + i * 2048, [512], F32) for i in range(2)]
            wp, wptok = wget("pp%d" % l)
            wpv = wp[:, 0:2048].rearrange("p (k m) -> p k m", k=2)
            cnt = 0
            for j in range(2):
                wg, wgtok = wget("pg%d%d" % (l, j))
                wgv = wg[:, 0:4096].rearrange("p (k m) -> p k m", k=8)
                for mi in range(4):
                    m = j * 4 + mi
                    for (c0, n) in sts:
                        bg, bgtok = nb()
                        mmgroup(bg, bgtok, 128, n, [(wgv[:, k, mi * 128:(mi + 1) * 128], XNc(k, c0, n)) for k in range(8)],
                                [wgtok, "XN"])
                        bp, bptok = nb()
                        mmgroup(bp, bptok, 128, n, [(wpv[:, k, m * 128:(m + 1) * 128], PT2[:, l, k, c0:c0 + n]) for k in range(2)],
                                [wptok, "PT2"])
                        sg, sgtok = SG[cnt % 2], "SG%d" % (cnt % 2)
                        cnt += 1
                        act(sg[:, 0:n], bg[:, 0:n], AF.Sigmoid, [bgtok], [sgtok])
                        dve("tensor_tensor", sg[:, 0:n], sg[:, 0:n], bp[:, 0:n], ALU.mult, reads=[sgtok, bptok], writes=[sgtok])
                        dve("tensor_tensor", X[:, m, c0:c0 + n], X[:, m, c0:c0 + n], sg[:, 0:n], ALU.add,
                            reads=["X", sgtok], writes=["X"])

        def router(sts):
            o = 11 * W * 2 + 8192
            CALL = P.carve("CALL", o, [8, W], BF16)
            o += 8 * W * 2
            LG = P.carve("LG", o, [8], F32)
            TOP = P.carve("TOP", o + 32, [8], F32)
            CB = P.carve("CB", o + 64, [8], F32)
            CB2 = P.carve("CB2", o + 96, [8], F32)
            WV = P.carve("WV", o + 128, [8], F32)
            o += 160
            DG = [P.carve("DG%d" % i, o + i * 512, [128], F32) for i in range(2)]
            cnt = 0
            for (c0, n) in sts:
                for tb in range((n + 127) // 128):
                    nt = min(128, n - tb * 128)
                    tc0 = c0 + tb * 128
                    bank, btok = nb()
                    for k in range(8):
                        mm(bank[0:nt, 0:8], X[:, k, tc0:tc0 + nt], GWR[:, k, :], k == 0, k == 7, ["X", "GWR"], [btok])
                    b2, b2tok = nb()
                    tr(b2[0:nt, 0:128], RSTD[:, tc0:tc0 + nt], identf, ["RSTD", "CST"], [b2tok])
                    dve_copy(WV[0:nt, 3:4], b2[0:nt, 0:1], [b2tok], ["WV"])
                    dve("tensor_scalar", LG[0:nt, :], bank[0:nt, 0:8], WV[0:nt, 3:4], None, ALU.mult,
                        reads=[btok, "WV"], writes=["LG"])
                    dve("max", TOP[0:nt, :], LG[0:nt, :], reads=["LG"], writes=["TOP"])
                    dve("tensor_tensor", WV[0:nt, 0:1], TOP[0:nt, 1:2], TOP[0:nt, 0:1], ALU.subtract,
                        reads=["TOP", "WV"], writes=["WV"])
                    act(WV[0:nt, 1:2], WV[0:nt, 0:1], AF.Sigmoid, ["WV"], ["WV"])
                    act(WV[0:nt, 2:3], WV[0:nt, 0:1], AF.Sigmoid, ["WV"], ["WV"], scale=-1.0)
                    dve("tensor_scalar", CB[0:nt, :], LG[0:nt, :], TOP[0:nt, 0:1], WV[0:nt, 2:3], ALU.is_equal, ALU.mult,
                        reads=["LG", "TOP", "WV"], writes=["CB"])
                    dve("tensor_scalar", CB2[0:nt, :], LG[0:nt, :], TOP[0:nt, 1:2], WV[0:nt, 1:2], ALU.is_equal, ALU.mult,
                        reads=["LG", "TOP", "WV"], writes=["CB2"])
                    dve("tensor_tensor", CB[0:nt, :], CB[0:nt, :], CB2[0:nt, :], ALU.add, reads=["CB", "CB2"], writes=["CB"])
                    for e_ in range(NEXP):
                        dg, dgtok = DG[cnt % 2], "DG%d" % (cnt % 2)
                        cnt += 1
                        dve("tensor_scalar", dg[0:nt, 0:nt], identf[0:nt, 0:nt], CB[0:nt, e_:e_ + 1], None, ALU.mult,
                            reads=["CST", "CB"], writes=[dgtok])
                        bc, bctok = nb()
                        mm(bc[:, 0:nt], onesf[0:nt, :], dg[0:nt, 0:nt], True, True, ["onesf", dgtok], [bctok])
                        act_copy(CALL[:, e_, tc0:tc0 + nt], bc[:, 0:nt], [bctok], ["CALL"])
            return CALL

        def rglru(t, sts):
            YL = P.carve("YL", 0, [8, W], BF16)
            o = 8 * W * 2
            RGIG = P.carve("RGIG", o, [8, 2, 128], BF16)
            o += 4096
            off, nel = woffs["rgig"]
            dma("pool", RGIG.rearrange("p a b c -> p (a b c)"), wflat[:, off:off + nel], [], ["RGIG"], "RGIG")
            names = ["XB", "GG", "TT", "XC", "RR", "IG", "AA", "BT", "HH"]
            tmp = []
            for par in range(2):
                d = {}
                for nm in names:
                    wd_ = (4 + W) if nm == "XB" else W
                    d[nm] = (P.carve("%s%d" % (nm, par), o, [wd_], F32), "%s%d" % (nm, par))
                    o += wd_ * 4
                d["XCB"] = (P.carve("XCB%d" % par, o, [W], BF16), "XCB%d" % par)
                o += W * 2
                tmp.append(d)
            def rg_a(c):
                T_ = tmp[c % 2]
                XB, xbt = T_["XB"]
                GG, ggt = T_["GG"]
                TT, ttt = T_["TT"]
                XC, xct = T_["XC"]
                RR, rrt = T_["RR"]
                IG, igt = T_["IG"]
                AA, aat = T_["AA"]
                BT, btt = T_["BT"]
                HH, hht = T_["HH"]
                XCB, xcbt = T_["XCB"]

                wt, wtok = wget("in1%d" % c)
                wv = wt[:, 0:2048].rearrange("p (k m) -> p k m", k=8)
                for (c0, n) in sts:
                    is_s = c0 >= TW
                    bx, bxtok = nb()
                    mmgroup(bx, bxtok, 128, n, [(wv[:, k, 0:128], XNc(k, c0, n)) for k in range(8)], [wtok, "XN"])
                    bg, bgtok = nb()
                    mmgroup(bg, bgtok, 128, n, [(wv[:, k, 128:256], XNc(k, c0, n)) for k in range(8)], [wtok, "XN"])
                    act_copy(GG[:, c0:c0 + n], bg[:, 0:n], [bgtok], [ggt])
                    act(TT[:, c0:c0 + n], GG[:, c0:c0 + n], AF.Square, [ggt], [ttt])
                    dve("tensor_scalar", TT[:, c0:c0 + n], TT[:, c0:c0 + n], 0.044715, 1.0, ALU.mult, ALU.add,
                        reads=[ttt], writes=[ttt])
                    dve("tensor_tensor", TT[:, c0:c0 + n], TT[:, c0:c0 + n], GG[:, c0:c0 + n], ALU.mult, reads=[ttt, ggt], writes=[ttt])
                    act(TT[:, c0:c0 + n], TT[:, c0:c0 + n], AF.Sigmoid, [ttt], [ttt], scale=1.5957691216057308)
                    dve("tensor_tensor", GG[:, c0:c0 + n], TT[:, c0:c0 + n], GG[:, c0:c0 + n], ALU.mult, reads=[ttt, ggt], writes=[ggt])
                    cw = C("convw")
                    if not is_s:
                        act_copy(XB[:, 0:4], CH[:, c, :], ["CH"], [xbt])
                        act_copy(XB[:, 4:4 + n], bx[:, 0:n], [bxtok], [xbt])
                        dve_copy(CH[:, c, :], XB[:, n:n + 4], [xbt], ["CH"])
                        dve("tensor_scalar", XC[:, 0:n], XB[:, 1:1 + n], cw[:, c:c + 1], C("convb")[:, c:c + 1],
                            ALU.mult, ALU.add, reads=[xbt, "CST"], writes=[xct])
                        for k in range(1, 4):
                            dve("scalar_tensor_tensor", XC[:, 0:n], XB[:, 1 + k:1 + k + n], cw[:, k * 8 + c:k * 8 + c + 1],
                                XC[:, 0:n], ALU.mult, ALU.add, reads=[xbt, xct, "CST"], writes=[xct])
                    else:
                        act_copy(CEX[:, c, :, 3], bx[:, 0:n], [bxtok], ["CEX"])
                        act_copy(NXB[:, c, :], bx[:, 0:n], [bxtok], ["NXB"])
                        dve("tensor_scalar", XC[:, c0:c0 + n], CEX[:, c, :, 0], cw[:, c:c + 1], C("convb")[:, c:c + 1],
                            ALU.mult, ALU.add, reads=["CEX", "CST"], writes=[xct])
                        for k in range(1, 4):
                            dve("scalar_tensor_tensor", XC[:, c0:c0 + n], CEX[:, c, :, k], cw[:, k * 8 + c:k * 8 + c + 1],
                                XC[:, c0:c0 + n], ALU.mult, ALU.add, reads=["CEX", xct, "CST"], writes=[xct])
                    act_copy(XCB[:, c0:c0 + n], XC[:, c0:c0 + n], [xct], [xcbt])

            def rg_b(c):
                T_ = tmp[c % 2]
                XB, xbt = T_["XB"]
                GG, ggt = T_["GG"]
                TT, ttt = T_["TT"]
                XC, xct = T_["XC"]
                RR, rrt = T_["RR"]
                IG, igt = T_["IG"]
                AA, aat = T_["AA"]
                BT, btt = T_["BT"]
                HH, hht = T_["HH"]
                XCB, xcbt = T_["XCB"]

                for (c0, n) in sts:
                    is_s = c0 >= TW
                    br, brtok = nb()
                    mm(br[:, 0:n], RGIG[:, c, 0, :], XCB[:, c0:c0 + n], True, True, ["RGIG", xcbt], [brtok])
                    bi_, bitok = nb()
                    mm(bi_[:, 0:n], RGIG[:, c, 1, :], XCB[:, c0:c0 + n], True, True, ["RGIG", xcbt], [bitok])
                    act(RR[:, c0:c0 + n], br[:, 0:n], AF.Sigmoid, [brtok, "CST"], [rrt], bias=C("brg")[:, c:c + 1])
                    act(IG[:, c0:c0 + n], bi_[:, 0:n], AF.Sigmoid, [bitok, "CST"], [igt], bias=C("big")[:, c:c + 1])
                    act(AA[:, c0:c0 + n], RR[:, c0:c0 + n], AF.Exp, [rrt, "NSP"], [aat], scale=NSP[:, c:c + 1])
                    act(BT[:, c0:c0 + n], RR[:, c0:c0 + n], AF.Exp, [rrt, "NSP"], [btt], scale=NSP[:, 8 + c:9 + c])
                    act(BT[:, c0:c0 + n], BT[:, c0:c0 + n], AF.Sqrt, [btt], [btt], scale=-1.0, bias=1.0)
                    dve("tensor_tensor", IG[:, c0:c0 + n], IG[:, c0:c0 + n], XC[:, c0:c0 + n], ALU.mult, reads=[igt, xct], writes=[igt])
                    dve("tensor_tensor", BT[:, c0:c0 + n], BT[:, c0:c0 + n], IG[:, c0:c0 + n], ALU.mult, reads=[btt, igt], writes=[btt])
                    if not is_s:
                        dve("tensor_tensor_scan", HH[:, 0:n], AA[:, 0:n], BT[:, 0:n], HS[:, c:c + 1], ALU.mult, ALU.add,
                            reads=[aat, btt, "HS"], writes=[hht])
                        dve_copy(HS[:, c:c + 1], HH[:, n - 1:n], [hht], ["HS"])
                    else:
                        dve("tensor_tensor", HH[:, c0:c0 + n], AA[:, c0:c0 + n], H0S[:, c, :], ALU.mult, reads=[aat, "H0S"], writes=[hht])
                        dve("tensor_tensor", HH[:, c0:c0 + n], HH[:, c0:c0 + n], BT[:, c0:c0 + n], ALU.add, reads=[hht, btt], writes=[hht])
                        dve_copy(H0S[:, c, :], HH[:, c0:c0 + n], [hht], ["H0S"])
                    dve("tensor_tensor", YL[:, c, c0:c0 + n], HH[:, c0:c0 + n], GG[:, c0:c0 + n], ALU.mult, reads=[hht, ggt], writes=["YL"])

            rg_a(0)
            for c in range(8):
                if c + 1 < 8:
                    rg_a(c + 1)
                rg_b(c)
            for j in range(2):
                wt, wtok = wget("out1%d" % j)
                wv = wt[:, 0:4096].rearrange("p (k m) -> p k m", k=8)
                for mi in range(4):
                    m = j * 4 + mi
                    for (c0, n) in sts:
                        bank, btok = nb()
                        mmgroup(bank, btok, 128, n, [(wv[:, k, mi * 128:(mi + 1) * 128], YL[:, k, c0:c0 + n]) for k in range(8)],
                                [wtok, "YL"])
                        dve("tensor_tensor", X[:, m, c0:c0 + n], X[:, m, c0:c0 + n], bank[:, 0:n], ALU.add,
                            reads=["X", btok], writes=["X"])
            if t == last_tile:
                for c in range(8):
                    dma("sp", conv_p[:, c * 128:(c + 1) * 128].rearrange("r f -> f r"), CH[:, c, 1:4], ["CH"], [], "conv_p",
                        allow_slow_non_contiguous=True)
                dma("sp", h_p.rearrange("o (c f) -> f (o c)", f=128), HS[:], ["HS"], [], "h_p", allow_slow_non_contiguous=True)
                if do_s:
                    out_tok16([(NXB[:, c, :], "NXB") for c in range(8)], conv_s[:, 2, :], "cs")
                    out_tok16([(H0S[:, c, :], "H0S") for c in range(8)], h_s, "hs")

        def decode_attention(QL, QR, RL, KRO, OT, wuv, wuvtok):
            o = A_F
            NG = 2
            G = [P.carve("G%d" % i, o + i * GCH * ROWW * 4, [GCH, ROWW], F32) for i in range(NG)]
            o += NG * GCH * ROWW * 4
            GBW = 162
            NGB = 3
            GB = [P.carve("GB%d" % i, o + i * GCH * GBW * 2, [GCH, GBW], BF16) for i in range(NGB)]
            o += NGB * GCH * GBW * 2
            KTL = [P.carve("KTL%d" % i, o + i * GCH * 256, [GCH, 128], BF16) for i in range(2)]
            o += 2 * GCH * 256
            KTR = [P.carve("KTR%d" % i, o + i * GCH * 256, [GCH, 128], BF16, 0, 32) for i in range(2)]
            o += 2 * GCH * 256
            STB = P.carve("STB", o, [GCH, 8], F32)
            o += GCH * 32
            PSB = P.carve("PSB", o, [GCH, 8], BF16)
            o += GCH * 32
            MXJ = P.carve("MXJ", o, [8], F32)
            MBC = P.carve("MBC", o + 32, [8], F32)
            o += 64
            QLS = P.carve("QLS", o, [NSAMP, 8], BF16)
            o += NSAMP * 16
            QRS = P.carve("QRS", o, [NSAMP, 8], BF16, 0, 32)
            o += NSAMP * 16
            MOLD = P.carve("MOLD", o, [NSAMP], F32, 0, 8)
            o += 64
            SMALL = P.carve("SMALL", o, [8], F32, 0, 8)
            o += 32
            DIAG = P.carve("DIAG", o, [8], F32, 0, 8)
            o += 32
            OACC = P.carve("OACC", o, [132], F32, 0, 8)
            o += 528
            OL1 = P.carve("OL1", o, [128], F32, 0, 8)
            o += 512
            OLST = P.carve("OLST", o, [8, NSAMP], BF16)
            o += 256
            OTOKS = P.carve("OTOKS", o, [512], BF16, 0, NSAMP)
            o += 1024
            dve_copy(QLS, QL[:, :, TW:W].rearrange("p h b -> p b h"), ["QL"], ["QLS"])
            dve_copy(QRS, QR[:, :, TW:W].rearrange("p h b -> p b h"), ["QR"], ["QRS"])
            bn, bntok = nb()
            for b in range(NSAMP):
                mm(bn[0:8, b:b + 1], QLS[:, b, :], RLB[:, b:b + 1], True, False, ["QLS", "RLB"], [bntok])
                mm(bn[0:8, b:b + 1], QRS[:, b, :], KROB[:, b:b + 1], False, True, ["QRS", "KROB"], [bntok])
            dve_copy(MOLD, bn[0:8, 0:NSAMP], [bntok], ["MOLD"])
            chunks = [(b, rc) for b in range(NSAMP) for rc in range(NGCH)]
            for i in range(NGB):
                dve("memset", GB[i][:, :, 128:130], 1.0, reads=[], writes=["GB%d" % i])
            P.rr = [0, 1, 2, 3, 4, 5]
            st = {}

            def s_init(b):
                bv0, bv0tok = nb()
                mm(bv0[0:8, 0:128], SEL[:, b, :], ROWS_S[:, 0:128], True, True, ["SEL", "ROWS_S"], [bv0tok])
                dve_copy(OACC[:, 0:128], bv0[0:8, 0:128], [bv0tok], ["OACC"])
                dve("memset", OACC[:, 128:129], 1.0, reads=[], writes=["OACC"])

            def s_fin(b):
                dve("reciprocal", SMALL[:, 3:4], OACC[:, 128:129], reads=["OACC", "SMALL"], writes=["SMALL"])
                dve("tensor_scalar", OL1, OACC[:, 0:128], SMALL[:, 3:4], None, ALU.mult, reads=["OACC", "SMALL"], writes=["OL1"])
                bt_, bttok = nb()
                tr(bt_[:, 0:8], OL1, identf[0:8, 0:8], ["OL1", "CST"], [bttok])
                act_copy(OLST[:, :, b], bt_[:, 0:8], [bttok], ["OLST"])

            def s1a(n):
                b, rc = chunks[n]
                gp = n % NG
                g, gtok = G[gp], "G%d" % gp
                P.add("pool", "indirect_dma_start", out=g.rearrange("p r c -> p (r c)"), out_offset=None, in_=cache,
                      in_offset=bass.IndirectOffsetOnAxis(ap=IDX[:, b, rc:rc + 1], axis=0),
                      reads=["IDX"], writes=[gtok], dma=True, key=gtok)
                gb, gbtok = GB[n % NGB], "GB%d" % (n % NGB)
                act_copy(gb[:, :, 0:128], g[:, :, 0:128], [gtok], [gbtok])
                dve_copy(gb[:, :, 130:162], g[:, :, 128:160], [gtok], [gbtok])

            def s1(n):
                b, rc = chunks[n]
                kp = n % 2
                ktl, ktltok = KTL[kp], "KTL%d" % kp
                ktr, ktrtok = KTR[kp], "KTR%d" % kp
                gb, gbtok = GB[n % NGB], "GB%d" % (n % NGB)
                for q8 in range(GCH // 8):
                    ba, batok = nb()
                    bav = bfview(ba)
                    bb, bbtok = nb()
                    bbv = bfview(bb)
                    for i in range(8):
                        r = q8 * 8 + i
                        tr(bav[:, i * 128:(i + 1) * 128], gb[:, r, 0:128], identb[:], [gbtok, "identb"], [batok])
                    for i in range(8):
                        r = q8 * 8 + i
                        tr(bbv[0:32, i * 128:(i + 1) * 128], gb[:, r, 130:162], identb[:], [gbtok, "identb"], [bbtok])
                    act_copy(ktl[:, q8 * 8:(q8 + 1) * 8, :], bav[:, 0:1024].rearrange("p (a b) -> p a b", a=8), [batok], [ktltok])
                    dve_copy(ktr[:, q8 * 8:(q8 + 1) * 8, :], bbv[0:32, 0:1024].rearrange("p (a b) -> p a b", a=8), [bbtok], [ktrtok])

            def s1s(n):
                b, rc = chunks[n]
                kp = n % 2
                ktl, ktltok = KTL[kp], "KTL%d" % kp
                ktr, ktrtok = KTR[kp], "KTR%d" % kp
                bs, bstok = banks[6 + kp], "bank%d" % (6 + kp)
                for r in range(GCH):
                    mm(bs[:, r * 8:(r + 1) * 8], ktl[:, r, :], QLS[:, b, :], True, False, [ktltok, "QLS"], [bstok])
                    mm(bs[:, r * 8:(r + 1) * 8], ktr[:, r, :], QRS[:, b, :], False, True, [ktrtok, "QRS"], [bstok])

            def s2(n):
                b, rc = chunks[n]
                gp = n % NG
                kp = n % 2
                g, gtok = G[gp], "G%d" % gp
                bs, bstok = banks[6 + kp], "bank%d" % (6 + kp)
                bsv = bs[:, 0:GCH * 8].rearrange("p (r h) -> p r h", h=8)
                dve("tensor_reduce", MXJ, bsv.rearrange("p r h -> p h r"), AX.X, ALU.max, reads=[bstok], writes=["MXJ"])
                bm, bmtok = nb()
                tr(bm[0:8, 0:128], MXJ, identf, ["MXJ", "CST"], [bmtok])
                dve("tensor_reduce", SMALL[:, 0:1], bm[0:8, 0:128], AX.X, ALU.max, reads=[bmtok], writes=["SMALL"])
                dve("tensor_tensor", SMALL[:, 1:2], MOLD[:, b:b + 1], SMALL[:, 0:1], ALU.max, reads=["MOLD", "SMALL"], writes=["SMALL"])
                dve("tensor_tensor", SMALL[:, 2:3], MOLD[:, b:b + 1], SMALL[:, 1:2], ALU.subtract, reads=["MOLD", "SMALL"], writes=["SMALL"])
                act(SMALL[:, 2:3], SMALL[:, 2:3], AF.Exp, ["SMALL"], ["SMALL"])
                dve_copy(MOLD[:, b:b + 1], SMALL[:, 1:2], ["SMALL"], ["MOLD"])
                dve("tensor_scalar", DIAG, identf[0:8, 0:8], SMALL[:, 1:2], None, ALU.mult, reads=["CST", "SMALL"], writes=["DIAG"])

            def s2b(n):
                b, rc = chunks[n]
                kp = n % 2
                bs, bstok = banks[6 + kp], "bank%d" % (6 + kp)
                bsv = bs[:, 0:GCH * 8].rearrange("p (r h) -> p r h", h=8)
                bd, bdtok = nb()
                mm(bd[:, 0:8], onesf[0:8, :], DIAG, True, True, ["onesf", "DIAG"], [bdtok])
                act_copy(MBC, bd[:, 0:8], [bdtok], ["MBC"])
                dve("tensor_tensor", STB, bsv, MBC.unsqueeze(1).broadcast_to([128, GCH, 8]), ALU.subtract,
                    reads=[bstok, "MBC"], writes=["STB"])
                act(PSB, STB, AF.Exp, ["STB"], ["PSB"])

            def s3(n):
                b, rc = chunks[n]
                gb, gbtok = GB[n % NGB], "GB%d" % (n % NGB)
                bo, botok = nb()
                for r in range(GCH):
                    mm(bo[0:8, 0:129], PSB[:, r, :], gb[:, r, 0:129], r == 0, r == GCH - 1, ["PSB", gbtok], [botok])
                dve("scalar_tensor_tensor", OACC[:, 0:129], OACC[:, 0:129], SMALL[:, 2:3], bo[0:8, 0:129], ALU.mult, ALU.add,
                    reads=["OACC", "SMALL", botok], writes=["OACC"])

            s1a(0)
            s1a(1)
            s1(0)
            s1s(0)
            for n in range(len(chunks)):
                b, rc = chunks[n]
                if n + 2 < len(chunks):
                    s1a(n + 2)
                if rc == 0:
                    s_init(b)
                s2(n)
                if n + 1 < len(chunks):
                    s1(n + 1)
                s2b(n)
                if n + 1 < len(chunks):
                    s1s(n + 1)
                s3(n)
                if rc == NGCH - 1:
                    s_fin(b)
            P.rr = list(range(8))
            bv, bvtok = nb()
            for h in range(8):
                mm(bv[0:NSAMP, h * 64:(h + 1) * 64], OLST[:, h, :], wuv[:, h * 64:(h + 1) * 64], True, True, ["OLST", wuvtok], [bvtok])
            dve_copy(OTOKS, bv[0:NSAMP, 0:512], [bvtok], ["OTOKS"])
            b2, b2tok = nb()
            b2v = bfview(b2)
            for j in range(4):
                tr(b2v[:, j * 16:(j + 1) * 16], OTOKS[:, j * 128:(j + 1) * 128], identb[0:NSAMP, 0:NSAMP], ["OTOKS", "identb"], [b2tok])
            act_copy(OT[:, :, TW:W], b2v[:, 0:64].rearrange("p (j n) -> p j n", j=4), [b2tok], ["OT"])

        try:
          for t in range(n_tiles):
              sts = [(0, TW)]
              if do_s and t == last_tile:
                  sts.append((TW, NSAMP))
              g0 = t * TW
              XS = P.carve("XS", A_F, [4, D], F32)
              PS_ = P.carve("PS_", A_F + 16384, [2, 4, 256], F32)
              for b in range(4):
                  dma("sp", XS[:, b, :], xp[g0 + b * 128:g0 + (b + 1) * 128, :], [], ["XS"], "XS%d" % b)
              for l in range(2):
                  dma("sp", PS_[:, l, :, :], pp[l, g0:g0 + TW, :].rearrange("(b p) f -> p b f", p=128), [], ["PS_"], "PS_%d" % l)
              dma("sp", ROPE[:, :, 0:TW], rope[:, :, g0:g0 + TW], [], ["ROPE"], "ROPEa")
              if do_s and t == last_tile:
                  dma("sp", ROPE[:, :, TW:W], rope[:, :, SEQ:SEQ + NSAMP], [], ["ROPE"], "ROPEb")
              for c in range(8):
                  bank, btok = nb()
                  for b in range(4):
                      tr(bank[:, b * 128:(b + 1) * 128], XS[:, b, c * 128:(c + 1) * 128], identf, ["XS", "CST"], [btok])
                  if c % 2 == 0:
                      act_copy(X[:, c, 0:TW], bank[:, 0:TW], [btok], ["X"])
                  else:
                      dve_copy(X[:, c, 0:TW], bank[:, 0:TW], [btok], ["X"])
              for l in range(2):
                  for k in range(2):
                      bank, btok = nb()
                      for b in range(4):
                          tr(bank[:, b * 128:(b + 1) * 128], PS_[:, l, b, k * 128:(k + 1) * 128], identf, ["PS_", "CST"], [btok])
                      act_copy(PT2[:, l, k, 0:TW], bank[:, 0:TW], [btok], ["PT2"])
              if do_s and t == last_tile:
                  XSS = P.carve("XSS", A_F + 24576, [D], F32, 0, NSAMP)
                  PSS = P.carve("PSS", A_F + 28672, [2, 256], F32, 0, NSAMP)
                  dma("sp", XSS, xs, [], ["XSS"], "XSS")
                  dma("sp", PSS, psm.rearrange("l b f -> b l f"), [], ["PSS"], "PSS")
                  bank, btok = nb()
                  for c in range(8):
                      tr(bank[:, c * 16:(c + 1) * 16], XSS[:, c * 128:(c + 1) * 128], identf[0:NSAMP, 0:NSAMP], ["XSS", "CST"], [btok])
                  dve_copy(X[:, :, TW:W], bank[:, 0:128].rearrange("p (c n) -> p c n", c=8), [btok], ["X"])
                  bank, btok = nb()
                  for l in range(2):
                      for k in range(2):
                          j = l * 2 + k
                          tr(bank[:, j * 16:(j + 1) * 16], PSS[:, l, k * 128:(k + 1) * 128], identf[0:NSAMP, 0:NSAMP], ["PSS", "CST"], [btok])
                  dve_copy(PT2[:, :, :, TW:W], bank[:, 0:64].rearrange("p (l k n) -> p l k n", l=2, k=2), [btok], ["PT2"])

              chk(1)
              rmsnorm(Xc, "X", 8, D, lambda c: C("nmix")[:, c:c + 1], XNc, "XN", sts)
              chk(2)
              CQ = P.carve("CQ", A_F, [2, W], F32)
              CKV = P.carve("CKV", A_F + 2 * W * 4, [W], F32)
              KR = P.carve("KR", A_F + 3 * W * 4, [W], F32, 0, 32)
              RL = P.carve("RL", A_F + 4 * W * 4, [W], F32)
              KRO = P.carve("KRO", A_F + 5 * W * 4, [W], F32, 0, 32)
              o1 = A_F + 6 * W * 4
              CQN = P.carve("CQN", o1, [2, W], BF16)
              o1 += 2 * W * 2
              PL = P.carve("PL", o1, [4, W], BF16)
              o1 += 4 * W * 2
              TA = P.carve("TA", o1, [16 + TW], F32)
              o1 += (16 + TW) * 4
              TB = P.carve("TB", o1, [16 + TW], F32)
              o1 += (16 + TW) * 4
              QN = [P.carve("QN%d" % i, o1 + i * W * 2, [W], BF16, 0, 64) for i in range(2)]
              o1 += 2 * W * 2
              QRR = P.carve("QRR", o1, [W], F32, 0, 32)
              o1 += W * 4
              T1 = P.carve("T1", o1, [TW], F32, 0, 32)
              o1 += TW * 4
              T2 = P.carve("T2", o1, [TW], F32, 0, 32)
              o1 += TW * 4
              ROWST = P.carve("ROWST", o1, [4, ROWW], F32)
              o1 += 4 * ROWW * 4
              QR = P.carve("QR", A_QR, [8, W], BF16, 0, 32)
              QL = P.carve("QL", A_QL, [8, W], BF16)
              PY = P.carve("PY", A_PY, [4, W], BF16)
              OT = P.carve("OT", A_OT, [4, W], BF16)

              wt, wtok = wget("in0a")
              wv = wt[:, 0:4096].rearrange("p (k m) -> p k m", k=8)
              for g in range(4):
                  for (c0, n) in sts:
                      bank, btok = nb()
                      mmgroup(bank, btok, 128, n, [(wv[:, k, g * 128:(g + 1) * 128], XNc(k, c0, n)) for k in range(8)], [wtok, "XN"])
                      act_copy(UB[:, g, 16 + c0:16 + c0 + n], bank[:, 0:n], [btok], ["UB"])
              wt, wtok = wget("in0b")
              wv = wt[:, 0:8 * 416].rearrange("p (k m) -> p k m", k=8)
              for (c0, n) in sts:
                  for j in range(2):
                      bank, btok = nb()
                      mmgroup(bank, btok, 128, n, [(wv[:, k, j * 128:(j + 1) * 128], XNc(k, c0, n)) for k in range(8)], [wtok, "XN"])
                      dve_copy(CQ[:, j, c0:c0 + n], bank[:, 0:n], [btok], ["CQ"])
                  bank, btok = nb()
                  mmgroup(bank, btok, 128, n, [(wv[:, k, 256:384], XNc(k, c0, n)) for k in range(8)], [wtok, "XN"])
                  act_copy(CKV[:, c0:c0 + n], bank[:, 0:n], [btok], ["CKV"])
                  bank, btok = nb()
                  mmgroup(bank, btok, 32, n, [(wv[:, k, 384:416], XNc(k, c0, n)) for k in range(8)], [wtok, "XN"])
                  dve_copy(KR[:, c0:c0 + n], bank[0:32, 0:n], [btok], ["KR"])

              chk(3)
              for g, wdw in enumerate((2, 4, 8, 16)):
                  U = UB[:, g, :]
                  cur, curtok = U, "UB"
                  valid = 1
                  d = 1
                  bufs = [(TA, "TA"), (TB, "TB")]
                  bi = 0
                  while d < wdw:
                      dst, dtok = bufs[bi]
                      bi ^= 1
                      lo = valid + d
                      dve("tensor_tensor", dst[:, lo:16 + TW], cur[:, lo:16 + TW], cur[:, lo - d:16 + TW - d], ALU.add,
                          reads=[curtok], writes=[dtok])
                      cur, curtok = dst, dtok
                      valid = lo
                      d *= 2
                  dve("scalar_tensor_tensor", PL[:, g, 0:TW], cur[:, 16:16 + TW], 1.0 / wdw, U[:, 16:16 + TW], ALU.mult, ALU.subtract,
                      reads=[curtok, "UB"], writes=["PL"])
                  if t == 0:
                      dve("tensor_tensor", NT1[:, 0:16], cur[:, 16:32], C("invc")[:, g * 16:(g + 1) * 16], ALU.mult,
                          reads=[curtok, "CST"], writes=["NT1"])
                      dve("tensor_tensor", PL[:, g, 0:16], NT1[:, 0:16], U[:, 16:32], ALU.subtract, reads=["NT1", "UB"], writes=["PL"])
              chk(31)
              if do_s and t == last_tile:
                  for g in range(4):
                      dve_copy(EXT[:, g, :, 15], UB[:, g, 16 + TW:16 + W], ["UB"], ["EXT"])
                      dve_copy(NU[:, g, :], UB[:, g, 16 + TW:16 + W], ["UB"], ["NU"])
                  for g, wdw in enumerate((2, 4, 8, 16)):
                      dve("tensor_reduce", NT1[:, 0:NSAMP], EXT[:, g, :, 16 - wdw:16], AX.X, ALU.add, reads=["EXT"], writes=["NT1"])
                      dve("scalar_tensor_tensor", PL[:, g, TW:W], NT1[:, 0:NSAMP], 1.0 / wdw, UB[:, g, 16 + TW:16 + W],
                          ALU.mult, ALU.subtract, reads=["NT1", "UB"], writes=["PL"])
              if t == last_tile:
                  for g in range(4):
                      dma("sp", pool_p[:, g * 128:(g + 1) * 128].rearrange("r f -> f r"), UB[:, g, 16 + TW - 15:16 + TW],
                          ["UB"], [], "pool_p", allow_slow_non_contiguous=True)
                  if do_s:
                      out_tok16([(NU[:, g, :], "NU") for g in range(4)], pool_s[:, 14, :], "ps")
              else:
                  dve_copy(UB[:, :, 0:16], UB[:, :, TW:TW + 16], ["UB"], ["UB"])
              chk(32)
              wt, wtok = wget("pool")
              wv = wt[:, 0:512].rearrange("p (g m) -> p g m", g=4)
              for g in range(4):
                  for (c0, n) in sts:
                      bank, btok = nb()
                      mmgroup(bank, btok, 128, n, [(wv[:, g, :], PL[:, g, c0:c0 + n])], [wtok, "PL"])
                      dve("tensor_scalar", PY[:, g, c0:c0 + n], bank[:, 0:n], C("pscale")[:, g:g + 1], None, ALU.mult, reads=[btok, "CST"], writes=["PY"])

              chk(4)
              rmsnorm(lambda c, c0, n: CQ[:, c, c0:c0 + n], "CQ", 2, 256, lambda c: C("qnorm")[:, c:c + 1],
                      lambda c, c0, n: CQN[:, c, c0:c0 + n], "CQN", sts)
              wq, wqtok = wget("uq")
              wqv = wq[:, 0:1536].rearrange("p (k m) -> p k m", k=2)
              wk, wktok = wget("uk")
              wkv = wk[:, 0:1024].rearrange("p (h c) -> p h c", h=8)
              r32t = C("r32t")[0:32, :]
              for h in range(8):
                  for (c0, n) in sts:
                      qn, qntok = QN[h % 2], "QN%d" % (h % 2)
                      bank, btok = nb()
                      mmgroup(bank, btok, 64, n, [(wqv[:, k, h * 64:(h + 1) * 64], CQN[:, k, c0:c0 + n]) for k in range(2)], [wqtok, "CQN"])
                      act_copy(qn[:, c0:c0 + n], bank[0:64, 0:n], [btok], [qntok])
                      bank, btok = nb()
                      mmgroup(bank, btok, 128, n, [(wkv[0:64, h, :], qn[:, c0:c0 + n])], [wktok, qntok])
                      act_copy(QL[:, h, c0:c0 + n], bank[:, 0:n], [btok], ["QL"], scale=SCALE)
                      bank, btok = nb()
                      mmgroup(bank, btok, 32, n, [(wqv[:, k, 512 + h * 32:512 + (h + 1) * 32], CQN[:, k, c0:c0 + n]) for k in range(2)],
                              [wqtok, "CQN"])
                      dve_copy(QRR[:, c0:c0 + n], bank[0:32, 0:n], [btok], ["QRR"])
                      bank, btok = nb()
                      mmgroup(bank, btok, 32, n, [(r32t, QRR[:, c0:c0 + n])], ["CST", "QRR"])
                      dve("scalar_tensor_tensor", T1[:, 0:n], QRR[:, c0:c0 + n], SCALE, ROPE[:, 0, c0:c0 + n], ALU.mult, ALU.mult,
                          reads=["QRR", "ROPE"], writes=["T1"])
                      dve("scalar_tensor_tensor", T2[:, 0:n], bank[0:32, 0:n], SCALE, ROPE[:, 1, c0:c0 + n], ALU.mult, ALU.mult,
                          reads=[btok, "ROPE"], writes=["T2"])
                      dve("tensor_tensor", QR[:, h, c0:c0 + n], T1[:, 0:n], T2[:, 0:n], ALU.add, reads=["T1", "T2"], writes=["QR"])

              chk(5)
              rmsnorm(lambda c, c0, n: CKV[:, c0:c0 + n], "CKV", 1, 128, lambda c: C("kvnorm")[:, 0:1],
                      lambda c, c0, n: RL[:, c0:c0 + n], "RL", sts)
              for (c0, n) in sts:
                  bank, btok = nb()
                  mmgroup(bank, btok, 32, n, [(r32t, KR[:, c0:c0 + n])], ["CST", "KR"])
                  dve("tensor_tensor", T1[:, 0:n], KR[:, c0:c0 + n], ROPE[:, 0, c0:c0 + n], ALU.mult, reads=["KR", "ROPE"], writes=["T1"])
                  dve("tensor_tensor", T2[:, 0:n], bank[0:32, 0:n], ROPE[:, 1, c0:c0 + n], ALU.mult, reads=[btok, "ROPE"], writes=["T2"])
                  dve("tensor_tensor", KRO[:, c0:c0 + n], T1[:, 0:n], T2[:, 0:n], ALU.add, reads=["T1", "T2"], writes=["KRO"])
              chk(51)
              act_copy(KLT[:, g0:g0 + TW], RL[:, 0:TW], ["RL"], ["KLT"])
              act_copy(KRT[:, g0:g0 + TW], KRO[:, 0:TW], ["KRO"], ["KRT"])
              chk(52)
              for b in range(4):
                  bank, btok = nb()
                  tr(bank[:, 0:128], RL[:, b * 128:(b + 1) * 128], identf, ["RL", "CST"], [btok])
                  tr(bank[:, 128:160], KRO[:, b * 128:(b + 1) * 128], identf[0:32, 0:32], ["KRO", "CST"], [btok])
                  dve_copy(ROWST[:, b, :], bank[:, 0:ROWW], [btok], ["ROWST"])
                  act_copy(VT[:, t * 4 + b, :], ROWST[:, b, 0:128], ["ROWST"], ["VT"])
              chk(53)
              dma("sp", rows_p[g0:g0 + TW, :].rearrange("(b p) c -> p b c", p=128), ROWST, ["ROWST"], [], "rows_p")
              if do_s and t == last_tile:
                  bank, btok = nb()
                  tr(bank[0:NSAMP, 0:128], RL[:, TW:W], identf, ["RL", "CST"], [btok])
                  tr(bank[0:NSAMP, 128:160], KRO[:, TW:W], identf[0:32, 0:32], ["KRO", "CST"], [btok])
                  dve_copy(ROWS_S[:], bank[0:NSAMP, 0:ROWW], [btok], ["ROWS_S"])
                  dve_copy(RLB[:], RL[:, TW:W], ["RL"], ["RLB"])
                  dve_copy(KROB[:], KRO[:, TW:W], ["KRO"], ["KROB"])
                  dma("sp", rows_s, ROWS_S[:], ["ROWS_S"], [], "rows_s")

              chk(6)
              o2 = A_F
              S_ = [P.carve("S%d" % i, o2 + i * SEQ * 4, [SEQ], F32) for i in range(2)]
              o2 += 2 * SEQ * 4
              PM = [P.carve("PM%d" % i, o2 + i * 2048, [1024], BF16) for i in range(2)]
              o2 += 4096
              PTS = [P.carve("PTS%d" % i, o2 + i * 2048, [8, 128], BF16) for i in range(2)]
              o2 += 4096
              OLT = [P.carve("OLT%d" % i, o2 + i * 256, [128], BF16) for i in range(2)]
              o2 += 512
              OTOK = P.carve("OTOK", o2, [512], BF16)
              o2 += 1024
              SM = [P.carve("SM%d" % i, o2 + i * 64, [16], F32) for i in range(2)]
              o2 += 128
              wuv, wuvtok = wget("uv")
              segc = 0
              items = [(qb, h) for qb in range(4) for h in range(8)]
              segc_box = [0]

              def att_a(idx):
                  qb, h = items[idx]
                  nk = (t * 4 + qb + 1) * 128
                  qc0 = qb * 128
                  par = idx % 2
                  S, stok = S_[par], "S%d" % par
                  sm, smtok = SM[par], "SM%d" % par
                  nchk = (nk + 511) // 512
                  for ck in range(nchk):
                      k0 = ck * 512
                      kw = min(512, nk - k0)
                      last = ck == nchk - 1
                      bank, btok = nb()
                      mm(bank[:, 0:kw], QL[:, h, qc0:qc0 + 128], KLT[:, k0:k0 + kw], True, False, ["QL", "KLT"], [btok])
                      mm(bank[:, 0:kw], QR[:, h, qc0:qc0 + 128], KRT[:, k0:k0 + kw], False, not last, ["QR", "KRT"], [btok])
                      if last:
                          mm(bank[:, kw - 128:kw], identb[:], maskb[:], False, True, ["identb", "maskb"], [btok])
                      dve("tensor_scalar", S[:, k0:k0 + kw], bank[:, 0:kw], 1.0, None, ALU.mult, ALU.max,
                          accum_out=sm[:, ck:ck + 1], reads=[btok], writes=[stok, smtok])
                  dve("tensor_reduce", sm[:, 8:9], sm[:, 0:nchk], AX.X, ALU.max, negate=True, reads=[smtok], writes=[smtok])

              def att_b(idx):
                  qb, h = items[idx]
                  nk = (t * 4 + qb + 1) * 128
                  qc0 = qb * 128
                  par = idx % 2
                  S, stok = S_[par], "S%d" % par
                  sm, smtok = SM[par], "SM%d" % par
                  nseg = (nk + 1023) // 1024
                  bo, botok = nb()
                  nkb = nk // 128
                  for sg in range(nseg):
                      s0 = sg * 1024
                      sw = min(1024, nk - s0)
                      sp_ = segc_box[0] % 2
                      segc_box[0] += 1
                      pm, pmtok = PM[sp_], "PM%d" % sp_
                      pts, ptstok = PTS[sp_], "PTS%d" % sp_
                      act(pm[:, 0:sw], S[:, s0:s0 + sw], AF.Exp, [stok, smtok], [pmtok, smtok],
                          bias=sm[:, 8:9], accum_out=sm[:, 9 + sg:10 + sg])
                      bt_, bttok = nb()
                      btv = bfview(bt_)
                      nbl = sw // 128
                      for j in range(nbl):
                          tr(btv[:, j * 128:(j + 1) * 128], pm[:, j * 128:(j + 1) * 128], identb[:], [pmtok, "identb"], [bttok])
                      ptsf = pts.rearrange("p a b -> p (a b)")
                      if sp_ == 0:
                          act_copy(ptsf[:, 0:sw], btv[:, 0:sw], [bttok], [ptstok])
                      else:
                          dve_copy(ptsf[:, 0:sw], btv[:, 0:sw], [bttok], [ptstok])
                      for j in range(nbl):
                          kb = sg * 8 + j
                          mm(bo[:, 0:128], VT[:, kb, :], pts[:, j, :], kb == 0, kb == nkb - 1, ["VT", ptstok], [botok])
                  olt, olttok = OLT[par], "OLT%d" % par
                  act_copy(olt, bo[:, 0:128], [botok], [olttok])
                  bv, bvtok = nb()
                  mm(bv[:, 0:64], olt, wuv[:, h * 64:(h + 1) * 64], True, True, [olttok, wuvtok], [bvtok])
                  dve("tensor_reduce", sm[:, 13:14], sm[:, 9:9 + nseg], AX.X, ALU.add, reads=[smtok], writes=[smtok])
                  dve("reciprocal", sm[:, 14:15], sm[:, 13:14], reads=[smtok], writes=[smtok])
                  dve("tensor_scalar", OTOK[:, h * 64:(h + 1) * 64], bv[:, 0:64], sm[:, 14:15], None, ALU.mult,
                      reads=[bvtok, smtok], writes=["OTOK"])
                  if h == 7:
                      bt_, bttok = nb()
                      btv = bfview(bt_)
                      for j in range(4):
                          tr(btv[:, j * 128:(j + 1) * 128], OTOK[:, j * 128:(j + 1) * 128], identb[:], ["OTOK", "identb"], [bttok])
                      act_copy(OT[:, :, qc0:qc0 + 128], btv[:, 0:512].rearrange("p (j n) -> p j n", j=4), [bttok], ["OT"])

              att_a(0)
              for idx in range(len(items)):
                  if idx + 1 < len(items):
                      att_a(idx + 1)
                  att_b(idx)

              if do_s and t == last_tile:
                  decode_attention(QL, QR, RL, KRO, OT, wuv, wuvtok)

              chk(7)
              for j in range(2):
                  wt, wtok = wget("out0%d" % j)
                  wv = wt[:, 0:4096].rearrange("p (k m) -> p k m", k=8)
                  for mi in range(4):
                      m = j * 4 + mi
                      for (c0, n) in sts:
                          bank, btok = nb()
                          pairs = [(wv[:, k, mi * 128:(mi + 1) * 128], PY[:, k, c0:c0 + n]) for k in range(4)]
                          pairs += [(wv[:, 4 + k, mi * 128:(mi + 1) * 128], OT[:, k, c0:c0 + n]) for k in range(4)]
                          mmgroup(bank, btok, 128, n, pairs, [wtok, "PY", "OT"])
                          dve("tensor_tensor", X[:, m, c0:c0 + n], X[:, m, c0:c0 + n], bank[:, 0:n], ALU.add,
                              reads=["X", btok], writes=["X"])

              chk(8)
              rmsnorm(Xc, "X", 8, D, lambda c: C("nffn")[:, c:c + 1], XNc, "XN", sts)
              for pe_ in range(2):
                  swiglu_expert(sts, "fg%d" % pe_, "fu%d" % pe_, "fd%d" % pe_, None, None)
              chk(9)
              ple(sts, 0)
              chk(10)
              rmsnorm(Xc, "X", 8, D, lambda c: C("nmix")[:, 8 + c:9 + c], XNc, "XN", sts)
              rglru(t, sts)
              chk(11)
              rmsnorm(Xc, "X", 8, D, lambda c: C("nffn")[:, 8 + c:9 + c], XNc, "XN", sts)
              chk(12)
              CALL = router(sts)
              chk(13)
              for e_ in range(NEXP):
                  swiglu_expert(sts, "eg%d" % e_, "eu%d" % e_, "ed%d" % e_, e_, CALL)
              chk(14)
              ple(sts, 1)
              chk(15)
              rmsnorm(Xc, "X", 8, D, lambda c: C("nfin")[:, c:c + 1], Xc, "X", sts)
              YS = [P.carve("YS%d" % i, i * 4096, [D], F32) for i in range(2)]
              for b in range(4):
                  ys, ystok = YS[b % 2], "YS%d" % (b % 2)
                  for hf in range(2):
                      bank, btok = nb()
                      for ci in range(4):
                          c = hf * 4 + ci
                          tr(bank[:, ci * 128:(ci + 1) * 128], X[:, c, b * 128:(b + 1) * 128], identf, ["X", "CST"], [btok])
                      if hf == 0:
                          act_copy(ys[:, 0:512], bank[:, 0:512], [btok], [ystok])
                      else:
                          dve_copy(ys[:, 512:1024], bank[:, 0:512], [btok], [ystok])
                  dma("sp", y_p[g0 + b * 128:g0 + (b + 1) * 128, :], ys, [ystok], [], ystok)
              if do_s and t == last_tile:
                  out_tok16([(X[:, c, TW:W], "X") for c in range(8)], y_s, "ys")

        except _Stop:
            pass
        print('sbuf bytes remaining', nc.sbuf_bytes_remaining)
        P.emit()
    return nc, P


_CACHE = {}


def _prep_common(inp):
    units, woffs, wtot = weight_units()
    wflat = np.empty((128, wtot), np.float32)
    for name, nel, fn in units:
        off, _ = woffs[name]
        wflat[:, off:off + nel] = fn(inp)
    return wflat, build_cst(inp), build_rope()


def kernel(**inp):
    inp = {k: np.asarray(v) for k, v in inp.items()}
    n_tiles = int(inp.pop("_n_tiles", NT))
    samples = bool(inp.pop("_samples", True))
    key = (n_tiles, samples)
    if key not in _CACHE:
        _CACHE[key] = build_program(n_tiles, samples)
    nc, P = _CACHE[key]
    wflat, cstv, ropev = _prep_common(inp)
    in_maps = []
    for c in range(NCORE):
        m = {
            "xp": np.ascontiguousarray(inp["x_prompt"][c]),
            "pp": np.ascontiguousarray(inp["p_prompt"][:, c]),
            "wflat": wflat, "cst": cstv, "rope": ropev,
        }
        if samples:
            sl = slice(c * NSAMP, (c + 1) * NSAMP)
            m.update({
                "xs": np.ascontiguousarray(inp["x_sample"][sl, 0]),
                "psm": np.ascontiguousarray(inp["p_sample"][:, sl, 0]),
                "cache": inp["cache_mla"][0].reshape(NPOOLPG * NGCH, GCH * ROWW),
                "spool": np.ascontiguousarray(inp["state_pool"][0, sl]),
                "sconv": np.ascontiguousarray(inp["state_conv"][0, sl]),
                "slru": np.ascontiguousarray(inp["state_lru"][0, sl]),
                "ptab": np.ascontiguousarray(inp["page_table"][sl]).astype(np.int32),
            })
        in_maps.append(m)
    res = run_bass_kernel_spmd(nc, in_maps, core_ids=list(range(NCORE)))
    r = res.results
    f32 = np.float32
    y_prompt = np.stack([r[c]["y_p"] for c in range(NCORE)]).astype(f32)
    rows_prompt = np.stack([r[c]["rows_p"] for c in range(NCORE)])[None].astype(f32)
    pool_prompt = np.stack([r[c]["pool_p"] for c in range(NCORE)])[None].astype(f32)
    conv_prompt = np.stack([r[c]["conv_p"] for c in range(NCORE)])[None].astype(f32)
    h_prompt = np.stack([r[c]["h_p"][0] for c in range(NCORE)])[None].astype(f32)
    if samples:
        y_sample = np.concatenate([r[c]["y_s"] for c in range(NCORE)])[:, None, :].astype(f32)
        rows_sample = np.concatenate([r[c]["rows_s"] for c in range(NCORE)])[None, :, None, :].astype(f32)
        pool_sample = np.concatenate([r[c]["pool_s"] for c in range(NCORE)])[None].astype(f32)
        conv_sample = np.concatenate([r[c]["conv_s"] for c in range(NCORE)])[None].astype(f32)
        h_sample = np.concatenate([r[c]["h_s"] for c in range(NCORE)])[None].astype(f32)
    else:
        y_sample = np.zeros((128, 1, D), f32)
        rows_sample = np.zeros((1, 128, 1, ROWW), f32)
        pool_sample = np.zeros((1, 128, 15, 512), f32)
        conv_sample = np.zeros((1, 128, 3, D), f32)
        h_sample = np.zeros((1, 128, D), f32)
    return (y_prompt, y_sample, rows_prompt, rows_sample, pool_prompt, pool_sample,
            conv_prompt, conv_sample, h_prompt, h_sample)
```
